# Optimizing a Trainium2 kernel written in Bass

```python
import jax, jax.numpy as jnp
from jax import lax
import numpy as np


D_MODEL = 1024
BATCH = 8
SEQ = 2048
DEPTH = 2

GRID_W = 64
CTX_LEN = 256
N_MIXERS = 2
EPS = 1e-6

CHUNK = 128
A_GROUPS = 8
A_WIDTH = 2 * D_MODEL
A_GW = A_WIDTH // A_GROUPS

HEAD_DIM = 64
B_Q_HEADS = D_MODEL // HEAD_DIM
B_KV_HEADS = 4
B_GROUP = B_Q_HEADS // B_KV_HEADS
Q_DIM = B_Q_HEADS * HEAD_DIM
KV_DIM = B_KV_HEADS * HEAD_DIM
WINDOW = 128
BLOCK = 128
ROPE_BASE = 10000.0
ROPE_PAIRS_AXIS = HEAD_DIM // 4

N_EXPERTS = 16
N_EXPERT_GROUPS = 4
EXPERTS_PER_GROUP = N_EXPERTS // N_EXPERT_GROUPS
TOP_K = 2
D_EXPERT = D_MODEL // 2

kernel_name = 'hybrid_dit_gmlp_swa_moe'


def rmsnorm(x, g):
    xf = x.astype(jnp.float32)
    y = xf * lax.rsqrt(jnp.mean(xf * xf, axis=-1, keepdims=True) + EPS)
    return (y * g.astype(jnp.float32)).astype(x.dtype)


def modulate(h, shift, scale):
    return h * (1 + scale) + shift


def adaln(cvec, w, b):
    m = jax.nn.silu(cvec) @ w + b
    return jnp.split(m[..., None, :], 6, axis=-1)


def chunk_gmlp(h, w_in, g_v, w_s, b_s, w_out):
    bsz, n, _ = h.shape
    uv = jax.nn.gelu(h @ w_in)
    u, v = jnp.split(uv, 2, axis=-1)
    v = rmsnorm(v, g_v).reshape(bsz, n // CHUNK, CHUNK, A_GROUPS, A_GW)
    v = jnp.einsum('gpq,bcqgd->bcpgd', w_s, v) + b_s.T[None, None, :, :, None]
    return (u * v.reshape(bsz, n, A_WIDTH)) @ w_out


def axial_angles(n):
    rows = n // GRID_W
    row = jnp.repeat(jnp.arange(rows, dtype=jnp.float32), GRID_W)
    col = jnp.tile(jnp.arange(GRID_W, dtype=jnp.float32), rows)
    inv = ROPE_BASE ** (-jnp.arange(ROPE_PAIRS_AXIS, dtype=jnp.float32) / ROPE_PAIRS_AXIS)
    return jnp.stack([row[:, None] * inv, col[:, None] * inv], axis=1)


def apply_rope(x, ang):
    b, n, hh, d = x.shape
    xa = x.reshape(b, n, hh, 2, 2, ROPE_PAIRS_AXIS)
    cos = jnp.cos(ang)[None, :, None].astype(x.dtype)
    sin = jnp.sin(ang)[None, :, None].astype(x.dtype)
    x1, x2 = xa[..., 0, :], xa[..., 1, :]
    out = jnp.stack([x1 * cos - x2 * sin, x2 * cos + x1 * sin], axis=-2)
    return out.reshape(b, n, hh, d)


def qkv_split(qkv):
    b, n, _ = qkv.shape
    q, k, v = jnp.split(qkv, [Q_DIM, Q_DIM + KV_DIM], axis=-1)
    return (q.reshape(b, n, B_Q_HEADS, HEAD_DIM),
            k.reshape(b, n, B_KV_HEADS, HEAD_DIM),
            v.reshape(b, n, B_KV_HEADS, HEAD_DIM))


def window_gqa(h, hc, w_qkv, sink, w_o, need_ctx):
    bsz, n, _ = h.shape
    nb = n // BLOCK
    scale = HEAD_DIM ** -0.5
    q, k, v = qkv_split(h @ w_qkv)
    qc, kc, vc = qkv_split(hc @ w_qkv)
    ang = axial_angles(n)
    q = apply_rope(q, ang)
    k = apply_rope(k, ang)
    kc32 = kc.astype(jnp.float32)
    sink32 = sink.astype(jnp.float32).reshape(B_KV_HEADS, B_GROUP)
    pad = ((0, 0), (BLOCK, BLOCK), (0, 0), (0, 0))
    k_pad = jnp.pad(k, pad)
    v_pad = jnp.pad(v, pad)
    qb = (q.astype(jnp.float32) * scale).reshape(bsz, nb, BLOCK, B_KV_HEADS, B_GROUP, HEAD_DIM)
    qb = jnp.moveaxis(qb, 1, 0)

    def block(args):
        qi, start = args
        kw = lax.dynamic_slice_in_dim(k_pad, start, 3 * BLOCK, axis=1)
        vw = lax.dynamic_slice_in_dim(v_pad, start, 3 * BLOCK, axis=1)
        qpos = start + jnp.arange(BLOCK)
        kpos = start - BLOCK + jnp.arange(3 * BLOCK)
        valid = ((jnp.abs(kpos[None, :] - qpos[:, None]) <= WINDOW)
                 & (kpos >= 0)[None, :] & (kpos < n)[None, :])
        s_win = jnp.einsum('bqhgd,bkhd->bhgqk', qi, kw.astype(jnp.float32))
        s_win = jnp.where(valid[None, None, None], s_win, -jnp.inf)
        s_ctx = jnp.einsum('bqhgd,bkhd->bhgqk', qi, kc32)
        s_sink = jnp.broadcast_to(sink32[None, :, :, None, None], s_ctx.shape[:-1] + (1,))
        p = jax.nn.softmax(jnp.concatenate([s_sink, s_ctx, s_win], axis=-1), axis=-1)
        p = p.astype(v.dtype)
        n_ctx = kc.shape[1]
        return (jnp.einsum('bhgqk,bkhd->bqhgd', p[..., 1:1 + n_ctx], vc)
                + jnp.einsum('bhgqk,bkhd->bqhgd', p[..., 1 + n_ctx:], vw))

    o = lax.map(block, (qb, jnp.arange(nb) * BLOCK))
    y = jnp.moveaxis(o, 0, 1).reshape(bsz, n, Q_DIM) @ w_o
    if not need_ctx:
        return y, None
    n_ctx = hc.shape[1]
    qc32 = (qc.astype(jnp.float32) * scale).reshape(bsz, n_ctx, B_KV_HEADS, B_GROUP, HEAD_DIM)
    sc = jnp.einsum('bqhgd,bkhd->bhgqk', qc32, kc32)
    sc_sink = jnp.broadcast_to(sink32[None, :, :, None, None], sc.shape[:-1] + (1,))
    pc = jax.nn.softmax(jnp.concatenate([sc_sink, sc], axis=-1), axis=-1)[..., 1:].astype(vc.dtype)
    oc = jnp.einsum('bhgqk,bkhd->bqhgd', pc, vc).reshape(bsz, n_ctx, Q_DIM)
    return y, oc @ w_o


def moe(h, w_router, b_router, w_gate, w_up, w_down):
    s = jax.nn.sigmoid((h @ w_router).astype(jnp.float32))
    sb = s + b_router.astype(jnp.float32)
    grp = sb.reshape(sb.shape[:-1] + (N_EXPERT_GROUPS, EXPERTS_PER_GROUP))
    grp_score = jnp.sum(lax.top_k(grp, TOP_K)[0], axis=-1)
    best = jnp.argmax(grp_score, axis=-1)
    in_group = (jnp.arange(N_EXPERTS) // EXPERTS_PER_GROUP) == best[..., None]
    _, idx = lax.top_k(jnp.where(in_group, sb, -jnp.inf), TOP_K)
    w = jnp.take_along_axis(s, idx, axis=-1)
    w = w / jnp.sum(w, axis=-1, keepdims=True)
    gates = jnp.sum(jax.nn.one_hot(idx, N_EXPERTS, dtype=jnp.float32) * w[..., None], axis=-2)
    gates = gates.astype(h.dtype)
    out = jnp.zeros_like(h)
    for e in range(N_EXPERTS):
        y = (jax.nn.silu(h @ w_gate[e]) * (h @ w_up[e])) @ w_down[e]
        out = out + gates[..., e:e + 1] * y
    return out


def setup_inputs(seed: int = 0) -> dict:
    key = jax.random.key(seed)
    ks = jax.random.split(key, 24)
    n_a = (DEPTH + N_MIXERS - 1) // N_MIXERS
    n_b = DEPTH // N_MIXERS
    nrm = jax.random.normal
    f32 = jnp.float32
    d = D_MODEL
    return {
        'x': nrm(ks[0], (BATCH, SEQ, d), f32),
        'c': nrm(ks[1], (BATCH, d), f32),
        'ctx': nrm(ks[2], (BATCH, CTX_LEN, d), f32),
        'c_ctx': nrm(ks[3], (d,), f32),
        'ada_w': nrm(ks[4], (DEPTH, d, 6 * d), f32) * (0.3 * d ** -0.5),
        'ada_b': nrm(ks[5], (DEPTH, 6 * d), f32) * 0.02,
        'norm1_g': 1.0 + 0.02 * nrm(ks[6], (DEPTH, d), f32),
        'norm2_g': 1.0 + 0.02 * nrm(ks[7], (DEPTH, d), f32),
        'a_w_in': nrm(ks[8], (n_a, d, 2 * A_WIDTH), f32) * d ** -0.5,
        'a_g_v': 1.0 + 0.02 * nrm(ks[9], (n_a, A_WIDTH), f32),
        'a_w_s': nrm(ks[10], (n_a, A_GROUPS, CHUNK, CHUNK), f32) * CHUNK ** -0.5,
        'a_b_s': nrm(ks[11], (n_a, A_GROUPS, CHUNK), f32) * 0.02,
        'a_w_out': nrm(ks[12], (n_a, A_WIDTH, d), f32) * A_WIDTH ** -0.5,
        'b_w_qkv': nrm(ks[13], (n_b, d, Q_DIM + 2 * KV_DIM), f32) * d ** -0.5,
        'b_sink': nrm(ks[14], (n_b, B_Q_HEADS), f32),
        'b_w_o': nrm(ks[15], (n_b, Q_DIM, d), f32) * Q_DIM ** -0.5,
        'router_w': nrm(ks[16], (d, N_EXPERTS), f32) * d ** -0.5,
        'router_b': nrm(ks[17], (N_EXPERTS,), f32) * 0.01,
        'moe_w_gate': nrm(ks[18], (DEPTH, N_EXPERTS, d, D_EXPERT), f32) * d ** -0.5,
        'moe_w_up': nrm(ks[19], (DEPTH, N_EXPERTS, d, D_EXPERT), f32) * d ** -0.5,
        'moe_w_down': nrm(ks[20], (DEPTH, N_EXPERTS, D_EXPERT, d), f32) * D_EXPERT ** -0.5,
        'final_g': 1.0 + 0.02 * nrm(ks[21], (d,), f32),
    }


def reference(x, c, ctx, c_ctx, ada_w, ada_b, norm1_g, norm2_g, a_w_in, a_g_v, a_w_s, a_b_s, a_w_out,
              b_w_qkv, b_sink, b_w_o, router_w, router_b, moe_w_gate, moe_w_up, moe_w_down, final_g):
    cx = ctx
    n_ctx = ctx.shape[1]
    for i in range(DEPTH):
        last = i == DEPTH - 1
        sh1, sc1, g1, sh2, sc2, g2 = adaln(c, ada_w[i], ada_b[i])
        csh1, csc1, cg1, csh2, csc2, cg2 = adaln(c_ctx, ada_w[i], ada_b[i])
        h = modulate(rmsnorm(x, norm1_g[i]), sh1, sc1)
        hc = modulate(rmsnorm(cx, norm1_g[i]), csh1, csc1)
        j = i // N_MIXERS
        if i % N_MIXERS == 0:
            y = chunk_gmlp(h, a_w_in[j], a_g_v[j], a_w_s[j], a_b_s[j], a_w_out[j])
            yc = None if last else chunk_gmlp(hc, a_w_in[j], a_g_v[j], a_w_s[j], a_b_s[j], a_w_out[j])
        else:
            y, yc = window_gqa(h, hc, b_w_qkv[j], b_sink[j], b_w_o[j], not last)
        x = x + g1 * y
        h2 = modulate(rmsnorm(x, norm2_g[i]), sh2, sc2)
        if last:
            x = x + g2 * moe(h2, router_w, router_b, moe_w_gate[i], moe_w_up[i], moe_w_down[i])
        else:
            cx = cx + cg1 * yc
            hc2 = modulate(rmsnorm(cx, norm2_g[i]), csh2, csc2)
            out = moe(jnp.concatenate([hc2, h2], axis=1), router_w, router_b,
                      moe_w_gate[i], moe_w_up[i], moe_w_down[i])
            cx = cx + cg2 * out[:, :n_ctx]
            x = x + g2 * out[:, n_ctx:]
    return rmsnorm(x, final_g)
```

```python
import numpy as np
import concourse.bass as bass
import concourse.mybir as mybir
from concourse.bass_utils import run_bass_kernel_spmd

F32 = mybir.dt.float32
BF16 = mybir.dt.bfloat16
AF = mybir.ActivationFunctionType
ALU = mybir.AluOpType
AX = mybir.AxisListType


class Buf:
    __slots__ = ("name", "writers", "readers", "excl")

    def __init__(self, name):
        self.name = name
        self.writers = []
        self.readers = []
        self.excl = False


class Op:
    __slots__ = ("eng", "fn", "deps", "dma", "sem", "cnt", "need_inc", "k")

    def __init__(self, eng, fn, dma):
        self.eng = eng
        self.fn = fn
        self.deps = []
        self.dma = dma
        self.sem = None
        self.cnt = 0
        self.need_inc = False
        self.k = -1


class Prog:
    ENGS = ("pe", "act", "dve", "pool", "sp")
    NDMA = 16
    LIMIT = 8000

    def __init__(self, nc):
        self.nc = nc
        self.ops = {e: [] for e in self.ENGS}
        self.ndma = 0
        self.dma_ops = []
        self.dma_q = {}
        self.all_bufs = []
        self.barrier_marks = []

    def buf(self, name):
        b = Buf(name)
        b.writers = list(self.barrier_marks)
        self.all_bufs.append(b)
        return b

    def bufs(self, name, n):
        return [self.buf("%s%d" % (name, i)) for i in range(n)]

    def op(self, eng, fn, reads=(), writes=(), dma=False, extra=()):
        o = Op(eng, fn, dma)
        deps = o.deps
        xr = [b for b in reads if b.excl and b not in writes]
        if xr:
            writes = list(writes) + xr
        for b in reads:
            for w in b.writers:
                deps.append(w)
        for b in writes:
            if b.readers:
                for r in b.readers:
                    if r is o:
                        continue
                    if r.eng == eng and eng == "pe" and not r.dma and not dma:
                        continue
                    deps.append(r)
                b.writers = []
                b.readers = []
            else:
                for w in b.writers:
                    if w.eng != eng or w.dma or dma or eng != "pe":
                        deps.append(w)
        for d in extra:
            deps.append(d)
        for b in reads:
            b.readers.append(o)
        for b in writes:
            b.writers.append(o)
        if dma:
            q = self.dma_q.setdefault(eng, [])
            o.k = len(q)
            if len(q) >= self.NDMA:
                deps.append(q[len(q) - self.NDMA])
            q.append(o)
            self.ndma += 1
            self.dma_ops.append(o)
        self.ops[eng].append(o)
        return o

    def barrier(self):
        marks = []
        for q in self.dma_q.values():
            marks.extend(q[-self.NDMA:])
        for e in self.ENGS:
            for o in reversed(self.ops[e]):
                if not o.dma:
                    marks.append(o)
                    break
        self.barrier_marks = marks
        for b in self.all_bufs:
            b.writers = list(marks)
            b.readers = []

    def emit(self):
        nc = self.nc
        for e in self.ENGS:
            for o in self.ops[e]:
                for d in o.deps:
                    if not d.dma:
                        d.need_inc = True
        self._sem_ctx = []
        for qn, q in self.dma_q.items():
            dsems = []
            for i in range(min(self.NDMA, len(q))):
                c = nc.semaphore("dq_%s_%d" % (qn, i))
                dsems.append(c.__enter__())
                self._sem_ctx.append(c)
            dcount = [0] * self.NDMA
            for o in q:
                s = o.k % self.NDMA
                dcount[s] += 16
                o.sem = dsems[s]
                o.cnt = dcount[s]
        for e in self.ENGS:
            cur = None
            n = self.LIMIT
            si = 0
            for o in self.ops[e]:
                if o.dma or not o.need_inc:
                    continue
                if n >= self.LIMIT:
                    c = nc.semaphore("e_%s_%d" % (e, si))
                    cur = c.__enter__()
                    self._sem_ctx.append(c)
                    si += 1
                    n = 0
                n += 1
                o.sem = cur
                o.cnt = n
        engmap = {"pe": nc.tensor, "act": nc.scalar, "dve": nc.vector, "pool": nc.gpsimd, "sp": nc.sync}

        def run(ename, eng):
            waited = {}
            for o in self.ops[ename]:
                need = {}
                for d in o.deps:
                    if d.sem is None:
                        continue
                    key = d.sem
                    if need.get(key, (0,))[0] < d.cnt:
                        need[key] = (d.cnt, d.sem)
                for key, (cnt, sem) in need.items():
                    if waited.get(key, 0) >= cnt:
                        continue
                    eng.wait_ge(sem, cnt)
                    waited[key] = cnt
                inst = o.fn(eng)
                if o.dma:
                    inst.then_inc(o.sem, 16)
                elif o.need_inc:
                    inst.then_inc(o.sem, 1)

        with nc.Block() as block:
            @block.tensor
            def _(e):
                run("pe", e)

            @block.scalar
            def _(e):
                run("act", e)

            @block.vector
            def _(e):
                run("dve", e)

            @block.gpsimd
            def _(e):
                run("pool", e)

            @block.sync
            def _(e):
                run("sp", e)
        for c in reversed(self._sem_ctx):
            c.__exit__(None, None, None)


def _nop_fn(ename):
    def f(eng):
        return eng.nop()
    return f


D = 1024
NCTX = 256
NX = 2048
NT0 = 18
EPS = 1e-6
NE = 16
DE = 512
BASE = 16640
X_OFF = BASE
HT_OFF = X_OFF + 18 * 4096
GBC_OFF = HT_OFF + 8 * 2304 * 2
SM_OFF = GBC_OFF + 8192
TMP_OFF = SM_OFF + 4096
SB_END = 229376
_DT_SIZE = {F32: 4, BF16: 2}


class Ctx:
    pass


class Region:
    def __init__(self, nc, lo, hi, tag):
        self.nc, self.lo, self.hi, self.cur, self.tag = nc, lo, hi, lo, tag
        self.n = 0

    def t(self, name, shape, dtype):
        nbytes = _DT_SIZE[dtype]
        for s in shape[1:]:
            nbytes *= s
        nbytes = (nbytes + 31) // 32 * 32
        assert self.cur + nbytes <= self.hi, (self.tag, name, self.cur + nbytes - self.hi)
        h = self.nc.alloc_sbuf_tensor_at("%s_%s" % (self.tag, name), list(shape), dtype, offset=self.cur)
        self.cur += nbytes
        return h


def build_program(stop_after=None, dbg=False):
    nc = bass.Bass("TRN2", target_bir_lowering=False)
    P = Prog(nc)
    C = Ctx()
    C.nc, C.P = nc, P

    def din(name, shape):
        return nc.dram_tensor(name, list(shape), F32, kind="ExternalInput").ap()

    I = Ctx()
    I.x = din("x", [NX, D])
    I.ctx = din("ctx", [NCTX, D])
    I.ccol = din("ccol", [128, 8, 2])
    I.ada_w = din("ada_w", [2, 12, D, 512])
    I.adab2 = din("adab2", [2, 2, 6 * D])
    I.ncol = din("ncol", [128, 2, 2, 8])
    I.a_w_in = din("a_w_in", [8, D, 512])
    I.a_w_out = din("a_w_out", [2048, D])
    I.gv_bc = din("gv_bc", [128, 2048])
    I.wsT = din("wsT", [128, 8, 128])
    I.bs_bc = din("bs_bc", [128, 8, 128])
    I.wqk = din("wqk", [12, D, 128])
    I.wvv = din("wvv", [D, 256])
    I.b_w_o = din("b_w_o", [D, D])
    I.sink_bc = din("sink_bc", [128, 16])
    I.wr = din("wr", [128, 8, 16])
    I.rb_bc = din("rb_bc", [128, 16])
    I.w_gate = din("moe_w_gate", [2, NE, D, DE])
    I.w_up = din("moe_w_up", [2, NE, D, DE])
    I.w_down = din("moe_w_down", [2, NE, DE, D])
    I.fg_bc = din("fg_bc", [128, D])
    I.rope = din("rope", [2, 128, NX])
    I.perm = din("perm", [128, 128])
    I.mask = din("mask", [128, 384])
    C.I = I
    C.out = nc.dram_tensor("out", [NX, D], F32, kind="ExternalOutput").ap()
    C.msc = nc.dram_tensor("msc", [2, 2, 6 * D], F32, kind="Internal").ap()
    C.mscB = P.buf("msc")
    C.dbg = dbg
    if dbg:
        C.dbg_x = nc.dram_tensor("dbg_x", [NT0 * 128, D], F32, kind="ExternalOutput").ap()
        C.dbg_m = nc.dram_tensor("dbg_m", [2, 2, 6 * D], F32, kind="ExternalOutput").ap()

    C.x_sb = nc.alloc_sbuf_tensor_at("x_sb", [128, NT0, D], F32, offset=X_OFF)
    C.xB = P.bufs("x", NT0)
    C.hT = nc.alloc_sbuf_tensor_at("hT", [128, 8, 2304], BF16, offset=HT_OFF)
    C.gbc = nc.alloc_sbuf_tensor_at("gbc", [128, 2, D], F32, offset=GBC_OFF)
    C.gbcB = P.bufs("gbc", 2)
    sm = Region(nc, SM_OFF, TMP_OFF, "sm")
    C.cols = sm.t("cols", [128, 2, 4, 8, 2], F32)
    C.colsB = P.buf("cols")
    C.gatesv = sm.t("gatesv", [128, NT0, NE], F32)
    C.gatesB = P.bufs("gates", NT0)
    C.ident_f = sm.t("ident_f", [128, 128], F32)
    C.ident_b = sm.t("ident_b", [128, 128], BF16)
    C.identB = P.buf("ident")
    C.rb = sm.t("rb", [128, NE], F32)
    C.rbB = P.buf("rb")
    C.ncol = sm.t("ncol", [128, 2, 2, 8], F32)
    C.ncolB = P.buf("ncol")
    C.sclhs = sm.t("sclhs", [128, 8, 2], BF16)
    C.scB = P.buf("sclhs")
    C.ps = [nc.alloc_psum_tensor("ps%d" % i, [128, 512], F32) for i in range(8)]
    C.psB = P.bufs("ps", 8)
    for b in C.psB:
        b.excl = True
    C.ntt = 0

    P.op("pool", lambda e: e.memset(C.ident_f[:], 1.0), writes=[C.identB])
    P.op("pool", lambda e: e.affine_select(out=C.ident_f[:], in_=C.ident_f[:], pattern=[[-1, 128]],
                                           compare_op=ALU.is_equal, fill=0.0, base=0, channel_multiplier=1),
         reads=[C.identB], writes=[C.identB])
    P.op("dve", lambda e: e.tensor_copy(out=C.ident_b[:], in_=C.ident_f[:]), reads=[C.identB], writes=[C.identB])
    P.op("sp", lambda e: e.dma_start(out=C.rb[:], in_=I.rb_bc), writes=[C.rbB], dma=True)
    P.op("sp", lambda e: e.dma_start(out=C.ncol[:], in_=I.ncol), writes=[C.ncolB], dma=True)

    phases = stop_after if (stop_after and stop_after.startswith("!")) else None
    if phases is not None:
        for ph in phases[1:]:
            if ph == "A":
                phase_adaln(C)
            elif ph == "L":
                for t in range(NT0):
                    src = C.I.ctx[t * 128:(t + 1) * 128, :] if t < 2 else C.I.x[(t - 2) * 128:(t - 1) * 128, :]
                    P.op("sp", lambda e, t=t, src=src: e.dma_start(out=C.x_sb[:, t, :], in_=src), writes=[C.xB[t]], dma=True)
            elif ph == "B":
                phase_gmlp(C)
            elif ph == "C":
                phase_moe(C, 0)
            elif ph == "D":
                phase_attn(C)
            elif ph == "E":
                phase_moe(C, 1)
            elif ph == "F":
                phase_final(C)
        return finish(C)
    phase_adaln(C)
    if stop_after == "A":
        return finish(C)
    phase_gmlp(C)
    if stop_after == "B":
        return finish(C)
    phase_moe(C, 0)
    if stop_after == "C":
        return finish(C)
    phase_attn(C)
    if stop_after == "D":
        return finish(C)
    phase_moe(C, 1)
    phase_final(C)
    return finish(C)


def finish(C):
    P = C.P
    if C.dbg:
        for t in range(2 if getattr(C, "attn_done", False) else 0, NT0):
            P.op("sp", lambda e, t=t: e.dma_start(out=C.dbg_x[t * 128:(t + 1) * 128, :], in_=C.x_sb[:, t, :]),
                 reads=[C.xB[t]], dma=True)
        P.op("sp", lambda e: e.dma_start(out=C.dbg_m, in_=C.msc), reads=[C.mscB], dma=True)
    P.barrier()
    P.op("sp", lambda e: e.nop(), reads=[C.mscB])
    P.emit()
    return C.nc


def adaln_cols(C, layer):
    P = C.P
    for vi, vec in enumerate((0, 1, 3, 4)):
        for who in range(2):
            P.op("sp", lambda e, vi=vi, vec=vec, who=who: e.dma_start(
                out=C.cols[:, layer, vi, :, who],
                in_=C.msc[layer, who, vec * 1024:(vec + 1) * 1024].rearrange("(k p) -> p k", p=128),
                allow_slow_non_contiguous=True),
                reads=[C.mscB], writes=[C.colsB], dma=True)
    for vi, wn in ((1, 0), (3, 1)):
        for who in range(2):
            P.op("dve", lambda e, vi=vi, wn=wn, who=who: e.scalar_tensor_tensor(
                out=C.cols[:, layer, vi, :, who], in0=C.cols[:, layer, vi, :, who], scalar=1.0,
                in1=C.ncol[:, layer, wn, :], op0=ALU.add, op1=ALU.mult),
                reads=[C.colsB, C.ncolB], writes=[C.colsB])


def phase_adaln(C):
    nc, P, I = C.nc, C.P, C.I
    P.barrier()
    R = Region(nc, TMP_OFF, SB_END, "pa")
    m_sb = R.t("m", [2, 6 * D], F32)
    mB = P.buf("m")
    adab = R.t("adab", [2, 6 * D], F32)
    adabB = P.buf("adab")
    wa = [R.t("wa%d" % i, [128, 8, 512], BF16) for i in range(3)]
    waB = P.bufs("wa", 3)
    ccol = R.t("ccol", [128, 8, 2], F32)
    ccB = P.buf("ccol")
    P.op("sp", lambda e: e.dma_start(out=ccol[:], in_=I.ccol), writes=[ccB], dma=True)
    P.op("act", lambda e: e.activation(out=C.sclhs[:], in_=ccol[:], func=AF.Silu), reads=[ccB], writes=[C.scB])
    layer = 0
    P.op("sp", lambda e: e.dma_start(out=adab[:], in_=I.adab2[layer]), writes=[adabB], dma=True)
    for nb in range(12):
        j = nb % 3
        P.op("pool", lambda e, nb=nb, j=j: e.dma_start(out=wa[j][:], in_=I.ada_w[layer, nb].rearrange("(c p) f -> p c f", p=128)),
             writes=[waB[j]], dma=True)
        pb = nb % 2
        for kk in range(8):
            P.op("pe", lambda e, j=j, kk=kk, pb=pb: e.matmul(C.ps[pb][0:2, :], C.sclhs[:, kk, :], wa[j][:, kk, :],
                                                           start=(kk == 0), stop=(kk == 7)),
                 reads=[waB[j], C.scB], writes=[C.psB[pb]])
        P.op("dve", lambda e, nb=nb, pb=pb: e.tensor_tensor(out=m_sb[:, nb * 512:(nb + 1) * 512], in0=C.ps[pb][0:2, :],
                                                            in1=adab[:, nb * 512:(nb + 1) * 512], op=ALU.add),
             reads=[C.psB[pb], adabB], writes=[mB])
    P.op("sp", lambda e: e.dma_start(out=C.msc[layer], in_=m_sb[:]), reads=[mB], writes=[C.mscB], dma=True)
    adaln_cols(C, layer)


def adaln_piece(C, q, wa2, wa2B, adabp, adabpB, mp, mpB):
    P, I = C.P, C.I
    nb, h = q // 2, q % 2
    c0 = nb * 512 + h * 256
    P.op("pool", lambda e: e.dma_start(out=wa2[:], in_=I.ada_w[1, nb][:, h * 256:(h + 1) * 256].rearrange("(c p) f -> p c f", p=128)),
         writes=[wa2B], dma=True)
    P.op("sp", lambda e: e.dma_start(out=adabp[:], in_=I.adab2[1][:, c0:c0 + 256]), writes=[adabpB], dma=True)
    for kk in range(8):
        P.op("pe", lambda e, kk=kk: e.matmul(C.ps[0][0:2, 0:256], C.sclhs[:, kk, :], wa2[:, kk, :], start=(kk == 0), stop=(kk == 7)),
             reads=[wa2B, C.scB], writes=[C.psB[0]])
    P.op("dve", lambda e: e.tensor_tensor(out=mp[:], in0=C.ps[0][0:2, 0:256], in1=adabp[:], op=ALU.add),
         reads=[C.psB[0], adabpB], writes=[mpB])
    P.op("sp", lambda e: e.dma_start(out=C.msc[1][:, c0:c0 + 256], in_=mp[:]), reads=[mpB], writes=[C.mscB], dma=True)


def load_gate_bc(C, layer, gi, who, slot):
    vec = 2 if gi == 0 else 5
    src = C.msc[layer, who:who + 1, vec * 1024:(vec + 1) * 1024]
    C.P.op("sp", lambda e: e.dma_start(out=C.gbc[:, slot, :], in_=src.partition_broadcast(128)),
           reads=[C.mscB], writes=[C.gbcB[slot]], dma=True)


def ps_bf16(C, b):
    return C.ps[b][:].bitcast(BF16)


def norm_stats(C, tiles, ssall, ssBt, rstd, rstdB, junks, junkBs, eps, epsB):
    P = C.P
    T = len(tiles)
    for i, t in enumerate(tiles):
        P.op("dve", lambda e, i=i: e.memset(ssall[:, i:i + 1], 0.0), writes=[ssBt[i]])
    for i, t in enumerate(tiles):
        j = i % len(junks)
        P.op("act", lambda e, i=i, t=t, j=j: e.activation(out=junks[j][:, 0:D], in_=C.x_sb[:, t, :], func=AF.Square, accum_out=ssall[:, i:i + 1]),
             reads=[C.xB[t], ssBt[i]], writes=[ssBt[i], junkBs[j]])
    P.op("act", lambda e: e.activation(out=rstd[:, 0:T], in_=ssall[:, 0:T], func=AF.Sqrt, scale=1.0 / D, bias=eps[:, 0:1]),
         reads=list(ssBt[:T]) + [epsB], writes=[rstdB])
    P.op("dve", lambda e: e.reciprocal(out=rstd[:, 0:T], in_=rstd[:, 0:T]), reads=[rstdB], writes=[rstdB])


def norm_transpose_tile(C, R, src_ap, srcB, layer, which, who, dstT, dstB, tok0, st, ps_bank, rstd_ap=None, rstdB=None):
    P = C.P
    if isinstance(st, list):
        st = st[C.ntt % len(st)]
    if isinstance(ps_bank, (list, tuple)):
        ps_bank = ps_bank[C.ntt % len(ps_bank)]
    sh_v, gm_v = (0, 1) if which == 0 else (2, 3)
    ss, ssB = st["ss"], st["ssB"]
    if rstd_ap is not None:
        P.op("dve", lambda e: e.tensor_scalar(out=st["xn"][:], in0=src_ap, scalar1=rstd_ap, scalar2=None, op0=ALU.mult),
             reads=[srcB, rstdB], writes=[st["xnB"]])
    else:
        _norm_stats_single(C, src_ap, srcB, st)
    _transpose_evac(C, layer, which, who, dstT, dstB, tok0, st, ps_bank)


def _norm_stats_single(C, src_ap, srcB, st):
    P = C.P
    ss, ssB = st["ss"], st["ssB"]
    P.op("dve", lambda e: e.memset(ss[:], 0.0), writes=[ssB])
    P.op("act", lambda e: e.activation(out=st["junk"][:, 0:D], in_=src_ap, func=AF.Square, accum_out=ss[:, 0:1]),
         reads=[srcB, ssB], writes=[ssB, st["junkB"]])
    P.op("act", lambda e: e.activation(out=ss[:, 0:1], in_=ss[:, 0:1], func=AF.Sqrt, scale=1.0 / D, bias=st["eps"][:, 0:1]),
         reads=[ssB, st["epsB"]], writes=[ssB])
    P.op("dve", lambda e: e.reciprocal(out=ss[:, 0:1], in_=ss[:, 0:1]), reads=[ssB], writes=[ssB])
    P.op("dve", lambda e: e.tensor_scalar(out=st["xn"][:], in0=src_ap, scalar1=ss[:, 0:1], scalar2=None, op0=ALU.mult),
         reads=[srcB, ssB], writes=[st["xnB"]])


def _transpose_evac(C, layer, which, who, dstT, dstB, tok0, st, ps_bank):
    P = C.P
    sh_v, gm_v = (0, 1) if which == 0 else (2, 3)
    pt = ps_bf16(C, ps_bank)
    use_act = (C.ntt % 2 == 0)
    C.ntt += 1
    for k in range(8):
        P.op("pe", lambda e, k=k: e.transpose(out=pt[:, k * 128:(k + 1) * 128], in_=st["xn"][:, k * 128:(k + 1) * 128],
                                              identity=C.ident_b[:]),
             reads=[st["xnB"], C.identB], writes=[C.psB[ps_bank]])
    for k in range(8):
        gm = C.cols[:, layer, gm_v, k, who:who + 1]
        sh = C.cols[:, layer, sh_v, k, who:who + 1]
        if use_act:
            P.op("act", lambda e, k=k, gm=gm, sh=sh: e.activation(out=dstT[:, k, tok0:tok0 + 128], in_=pt[:, k * 128:(k + 1) * 128],
                                                                 func=AF.Identity, scale=gm, bias=sh),
                 reads=[C.psB[ps_bank], C.colsB], writes=[dstB])
        else:
            P.op("dve", lambda e, k=k, gm=gm, sh=sh: e.tensor_scalar(out=dstT[:, k, tok0:tok0 + 128], in0=pt[:, k * 128:(k + 1) * 128],
                                                                    scalar1=gm, scalar2=sh, op0=ALU.mult, op1=ALU.add),
                 reads=[C.psB[ps_bank], C.colsB], writes=[dstB])


def make_norm_scratch(C, R, tag):
    P = C.P
    st = {}
    st["junk"] = R.t(tag + "junk", [128, 2048], BF16)
    st["junkB"] = P.buf(tag + "junk")
    st["ss"] = R.t(tag + "ss", [128, 2], F32)
    st["ssB"] = P.buf(tag + "ss")
    st["xn"] = R.t(tag + "xn", [128, D], BF16)
    st["xnB"] = P.buf(tag + "xn")
    st["eps"] = R.t(tag + "eps", [128, 1], F32)
    epsB = P.buf(tag + "eps")
    P.op("dve", lambda e: e.memset(st["eps"][:], EPS), writes=[epsB])
    st["epsB"] = epsB
    return st


def phase_gmlp(C):
    nc, P, I = C.nc, C.P, C.I
    layer = 0
    P.barrier()
    R = Region(nc, TMP_OFF, SB_END, "pb")
    wi = [R.t("wi%d" % i, [128, 8, 512], BF16) for i in range(2)]
    wiB = P.bufs("wi", 2)
    hTg = R.t("hTg", [128, 8, 512], BF16)
    hTgB = P.buf("hTg")
    uT = R.t("uT", [128, 16, 512], BF16)
    uTB = P.buf("uT")
    vraw4 = R.t("vraw", [128, 4, 2048], BF16)
    vrawB4 = P.bufs("vraw", 4)
    prodT = [R.t("prodT%d" % i, [128, 16, 128], BF16) for i in range(2)]
    prodB = P.bufs("prodT", 2)
    sts = []
    for i in range(2):
        st = {}
        st["xn"] = R.t("xn%d" % i, [128, D], BF16)
        st["xnB"] = P.buf("xn%d" % i)
        st["junk"], st["junkB"] = st["xn"], st["xnB"]
        st["ss"] = R.t("ss%d" % i, [128, 2], F32)
        st["ssB"] = P.buf("ss%d" % i)
        sts.append(st)
    eps = R.t("eps", [128, 1], F32)
    epsB = P.buf("eps")
    ssall = R.t("ssall", [128, 4], F32)
    ssBt = P.bufs("ssall", 4)
    rstd = R.t("rstd", [128, 4], F32)
    rstdB = P.buf("rstd")
    ssv = R.t("ssv", [128, 4, 2], F32)
    ssvBt = P.bufs("ssv", 4)
    rstdv = R.t("rstdv", [128, 4], F32)
    rstdvB = P.buf("rstdv")
    tmpf = [R.t("tmpf%d" % i, [128, D], F32) for i in range(2)]
    tmpfB = P.bufs("tmpf", 2)
    gv = R.t("gv", [128, 2048], BF16)
    gvB = P.buf("gv")
    bsrow = R.t("bsrow", [1, 8, 128], BF16)
    ones1 = R.t("ones1", [1, 128], BF16)
    bsB = P.buf("bs")
    wsT = R.t("wsT", [128, 8, 128], BF16)
    wsB = P.buf("ws")
    w_out = nc.alloc_sbuf_tensor_at("pb_wout", [128, 16, D], BF16, offset=HT_OFF)
    woB = P.buf("wout")

    P.op("dve", lambda e: e.memset(eps[:], EPS), writes=[epsB])
    P.op("dve", lambda e: e.memset(ones1[:], 1.0), writes=[bsB])
    P.op("pool", lambda e: e.dma_start(out=gv[:], in_=I.gv_bc), writes=[gvB], dma=True)
    P.op("pool", lambda e: e.dma_start(out=wsT[:], in_=I.wsT), writes=[wsB], dma=True)
    P.op("pool", lambda e: e.dma_start(out=bsrow[:], in_=I.bs_bc[0:1]), writes=[bsB], dma=True)
    load_gate_bc(C, layer, 0, 0, 0)
    load_gate_bc(C, layer, 0, 1, 1)
    for h in range(2):
        P.op("pool", lambda e, h=h: e.dma_start(out=w_out[:, h * 8:(h + 1) * 8, :],
                                               in_=I.a_w_out[h * 1024:(h + 1) * 1024, :].rearrange("(c p) f -> p c f", p=128)),
             writes=[woB], dma=True)

    groups = [[0, 1, 2, 3], [4, 5, 6, 7], [8, 9, 10, 11], [12, 13, 14, 15], [16, 17]]
    ring = [1, 2, 3]
    rk = 0
    pmk = 0
    wk = 0
    tk = 0
    st8 = {"rk": 0, "pmk": 0, "wk": 0, "tk": 0}
    wsc = nc.dram_tensor("w_in_bf16", [8, 128, 8 * 512], BF16, kind="Internal").ap()
    wscB = P.bufs("wsc", 8)

    def load_piece(pc, s, gi):
        if gi == 0:
            P.op("pool", lambda e: e.dma_start(out=wi[s][:], in_=I.a_w_in[pc].rearrange("(c p) f -> p c f", p=128)),
                 writes=[wiB[s]], dma=True)
            P.op("sp", lambda e: e.dma_start(out=wsc[pc], in_=wi[s][:].rearrange("p c f -> p (c f)")),
                 reads=[wiB[s]], writes=[wscB[pc]], dma=True)
        else:
            P.op("sp", lambda e: e.dma_start(out=wi[s][:].rearrange("p c f -> p (c f)"), in_=wsc[pc]),
                 reads=[wscB[pc]], writes=[wiB[s]], dma=True)

    def stage_a(g):
        n = len(g)
        N = 128 * n
        for i, t in enumerate(g):
            src = I.ctx[t * 128:(t + 1) * 128, :] if t < 2 else I.x[(t - 2) * 128:(t - 1) * 128, :]
            P.op("sp", lambda e, t=t, src=src: e.dma_start(out=C.x_sb[:, t, :], in_=src), writes=[C.xB[t]], dma=True)
        norm_stats(C, g, ssall, ssBt, rstd, rstdB, [sts[0]["xn"], sts[1]["xn"]], [sts[0]["xnB"], sts[1]["xnB"]], eps, epsB)
        for i, t in enumerate(g):
            who = 1 if t < 2 else 0
            norm_transpose_tile(C, R, C.x_sb[:, t, :], C.xB[t], layer, 0, who, hTg, hTgB, i * 128, sts, 0,
                                rstd_ap=rstd[:, i:i + 1], rstdB=rstdB)

    def stage_bc(g, gi):
        n = len(g)
        N = 128 * n
        rk, wk = st8["rk"], st8["wk"]
        for jj in range(4):
            s = wk % 2
            wk += 1
            load_piece(jj, s, gi)
            for j4 in range(4):
                j = jj * 4 + j4
                pb = ring[rk % 3]
                rk += 1
                for k in range(8):
                    P.op("pe", lambda e, s=s, j4=j4, k=k, pb=pb, N=N: e.matmul(
                        C.ps[pb][:, 0:N], wi[s][:, k, j4 * 128:(j4 + 1) * 128], hTg[:, k, 0:N], start=(k == 0), stop=(k == 7)),
                        reads=[wiB[s], hTgB], writes=[C.psB[pb]])
                P.op("act", lambda e, j=j, pb=pb, N=N: e.activation(out=uT[:, j, 0:N], in_=C.ps[pb][:, 0:N], func=AF.Gelu_apprx_tanh),
                     reads=[C.psB[pb]], writes=[uTB])
        for sv in range(4):
            s = wk % 2
            wk += 1
            load_piece(4 + sv, s, gi)
            for i, t in enumerate(g):
                pb = ring[rk % 3]
                rk += 1
                for k in range(8):
                    P.op("pe", lambda e, s=s, k=k, pb=pb, i=i: e.matmul(
                        C.ps[pb][:, :], hTg[:, k, i * 128:(i + 1) * 128], wi[s][:, k, :], start=(k == 0), stop=(k == 7)),
                        reads=[wiB[s], hTgB], writes=[C.psB[pb]])
                P.op("act", lambda e, sv=sv, pb=pb, i=i: e.activation(out=vraw4[:, i, sv * 512:(sv + 1) * 512], in_=C.ps[pb][:, :],
                                                                      func=AF.Gelu_apprx_tanh),
                     reads=[C.psB[pb]], writes=[vrawB4[i]])
        for i in range(n):
            P.op("dve", lambda e, i=i: e.memset(ssv[:, i, :], 0.0), writes=[ssvBt[i]])
        for i in range(n):
            for hf in range(2):
                jb = sts[(2 * i + hf) % 2]
                P.op("act", lambda e, i=i, hf=hf, jb=jb: e.activation(out=jb["xn"][:, 0:1024], in_=vraw4[:, i, hf * 1024:(hf + 1) * 1024],
                                                                    func=AF.Square, accum_out=ssv[:, i, hf:hf + 1]),
                     reads=[vrawB4[i], ssvBt[i]], writes=[ssvBt[i], jb["xnB"]])
        P.op("dve", lambda e, n=n: e.tensor_tensor(out=rstdv[:, 0:n], in0=ssv[:, 0:n, 0], in1=ssv[:, 0:n, 1], op=ALU.add),
             reads=list(ssvBt[:n]), writes=[rstdvB])
        P.op("act", lambda e, n=n: e.activation(out=rstdv[:, 0:n], in_=rstdv[:, 0:n], func=AF.Sqrt, scale=1.0 / 2048, bias=eps[:, 0:1]),
             reads=[rstdvB, epsB], writes=[rstdvB])
        P.op("dve", lambda e, n=n: e.reciprocal(out=rstdv[:, 0:n], in_=rstdv[:, 0:n]), reads=[rstdvB], writes=[rstdvB])
        st8["rk"], st8["wk"] = rk, wk

    def stage_d(g):
        n = len(g)
        pmk, tk = st8["pmk"], st8["tk"]
        pps = {}

        def spatial(i, t):
            nonlocal pmk, tk
            pp = tk % 2
            tk += 1
            pps[i] = pp
            P.op("dve", lambda e, i=i: e.scalar_tensor_tensor(out=vraw4[:, i, :], in0=vraw4[:, i, :], scalar=rstdv[:, i:i + 1], in1=gv[:],
                                                             op0=ALU.mult, op1=ALU.mult),
                 reads=[vrawB4[i], rstdvB, gvB], writes=[vrawB4[i]])
            for b4 in range(4):
                pm = 4 + (pmk % 2)
                pmk += 1
                brow = bsrow[0:1, 2 * b4:2 * b4 + 2, :].unsqueeze(2).to_broadcast([1, 2, 2, 128])
                P.op("pe", lambda e, pm=pm, brow=brow: e.matmul(C.ps[pm][:, :].rearrange("p (a b n) -> p a b n", a=2, b=2), ones1[0:1, :], brow,
                                                                start=True, stop=False),
                     reads=[bsB], writes=[C.psB[pm]])
                for jq in range(4):
                    j = b4 * 4 + jq
                    P.op("pe", lambda e, pm=pm, jq=jq, j=j, i=i: e.matmul(
                        C.ps[pm][:, jq * 128:(jq + 1) * 128], vraw4[:, i, j * 128:(j + 1) * 128], wsT[:, j // 2, :], start=False, stop=(jq == 3)),
                        reads=[vrawB4[i], wsB], writes=[C.psB[pm]])
                P.op("dve", lambda e, pm=pm, b4=b4, i=i, pp=pp: e.tensor_tensor(
                    out=prodT[pp][:, 4 * b4:4 * b4 + 4, :], in0=C.ps[pm][:, :].rearrange("p (a n) -> p a n", a=4),
                    in1=uT[:, 4 * b4:4 * b4 + 4, i * 128:(i + 1) * 128], op=ALU.mult),
                    reads=[C.psB[pm], uTB], writes=[prodB[pp]])

        def outproj(i, t):
            who = 1 if t < 2 else 0
            pp = pps[i]
            for half in range(2):
                pb = 6 + half
                for j in range(16):
                    P.op("pe", lambda e, pb=pb, j=j, half=half, pp=pp: e.matmul(
                        C.ps[pb][:, :], prodT[pp][:, j, :], w_out[:, j, half * 512:(half + 1) * 512], start=(j == 0), stop=(j == 15)),
                        reads=[prodB[pp], woB], writes=[C.psB[pb]])
                P.op("dve", lambda e, pb=pb, half=half, who=who, pp=pp: e.tensor_tensor(
                    out=tmpf[pp][:, half * 512:(half + 1) * 512], in0=C.ps[pb][:, :], in1=C.gbc[:, who, half * 512:(half + 1) * 512], op=ALU.mult),
                    reads=[C.psB[pb], C.gbcB[who]], writes=[tmpfB[pp]])
            P.op("dve", lambda e, t=t, pp=pp: e.tensor_tensor(out=C.x_sb[:, t, :], in0=C.x_sb[:, t, :], in1=tmpf[pp][:], op=ALU.add),
                 reads=[tmpfB[pp], C.xB[t]], writes=[C.xB[t]])

        spatial(0, g[0])
        for i in range(n):
            if i + 1 < n:
                spatial(i + 1, g[i + 1])
            outproj(i, g[i])
        st8["pmk"], st8["tk"] = pmk, tk

    stage_a(groups[0])
    for gi, g in enumerate(groups):
        stage_bc(g, gi)
        if gi + 1 < len(groups):
            stage_a(groups[gi + 1])
        stage_d(g)


def _const_tables():
    t = np.arange(NX)
    row = (t // 64).astype(np.float32)
    col = (t % 64).astype(np.float32)
    inv = (10000.0 ** (-np.arange(16, dtype=np.float32) / 16)).astype(np.float32)
    cosT = np.zeros((128, NX), np.float32)
    sinT = np.zeros((128, NX), np.float32)
    perm = np.zeros((128, 128), np.float32)
    for p in range(128):
        d = p % 64
        axis = d // 32
        half = (d % 32) // 16
        f = d % 16
        ang = (row if axis == 0 else col) * inv[f]
        cosT[p] = np.cos(ang)
        sinT[p] = -np.sin(ang) if half == 0 else np.sin(ang)
        partner = p + 16 if half == 0 else p - 16
        perm[partner, p] = 1.0
    qi = np.arange(128)[:, None]
    kj = np.arange(128)[None, :]
    NEG = -30000.0
    mask = np.zeros((128, 384), np.float32)
    mask[:, 0:128] = np.where(qi >= kj, 0.0, NEG)
    mask[:, 128:256] = np.where(qi <= kj, 0.0, NEG)
    return np.stack([cosT, sinT]), perm, mask


def _wqk_layout(w):
    chunks = [w[:, c * 128:(c + 1) * 128] for c in range(8)]
    for hk in range(4):
        kh = w[:, 1024 + hk * 64:1024 + (hk + 1) * 64]
        chunks.append(np.concatenate([kh, kh], axis=1))
    return np.stack(chunks, axis=0)


def _prep(inputs):
    f = lambda a: np.ascontiguousarray(np.asarray(a, dtype=np.float32))
    g = {k: np.asarray(v) for k, v in inputs.items()}
    rope, perm, mask = _const_tables()
    ncol = np.stack([g["norm1_g"], g["norm2_g"]], axis=0)
    ncol = ncol.reshape(2, 2, 8, 128).transpose(3, 1, 0, 2)
    common = {
        "ada_w": f(g["ada_w"].reshape(2, D, 12, 512).transpose(0, 2, 1, 3)),
        "adab2": f(np.repeat(g["ada_b"][:, None, :], 2, axis=1)),
        "ncol": f(ncol),
        "a_w_in": f(g["a_w_in"][0].reshape(D, 8, 512).transpose(1, 0, 2)),
        "a_w_out": f(g["a_w_out"][0]),
        "gv_bc": f(np.broadcast_to(g["a_g_v"][0][None, :], (128, 2048))),
        "wsT": f(g["a_w_s"][0].transpose(2, 0, 1)),
        "bs_bc": f(np.broadcast_to(g["a_b_s"][0][None, :, :], (128, 8, 128))),
        "wqk": f(_wqk_layout(g["b_w_qkv"][0])),
        "wvv": f(g["b_w_qkv"][0][:, 1280:1536]),
        "b_w_o": f(g["b_w_o"][0]),
        "sink_bc": f(np.broadcast_to(g["b_sink"][0][None, :], (128, 16))),
        "wr": f(g["router_w"].reshape(8, 128, 16).transpose(1, 0, 2)),
        "rb_bc": f(np.broadcast_to(g["router_b"][None, :], (128, 16))),
        "moe_w_gate": f(g["moe_w_gate"]),
        "moe_w_up": f(g["moe_w_up"]),
        "moe_w_down": f(g["moe_w_down"]),
        "fg_bc": f(np.broadcast_to(g["final_g"][None, :], (128, D))),
        "rope": f(rope), "perm": f(perm), "mask": f(mask),
    }
    maps = []
    for b in range(8):
        ccol = np.stack([g["c"][b].reshape(8, 128).T, g["c_ctx"].reshape(8, 128).T], axis=-1)
        m = dict(common)
        m["x"] = f(g["x"][b])
        m["ctx"] = f(g["ctx"][b])
        m["ccol"] = f(ccol)
        maps.append(m)
    return maps


def kernel(**inputs):
    maps = _prep(inputs)
    nc = build_program()
    res = run_bass_kernel_spmd(nc, maps, core_ids=list(range(8)))
    out = np.stack([np.asarray(r["out"]) for r in res.results], axis=0)
    return out.astype(np.float32)


def phase_moe(C, layer):
    nc, P, I = C.nc, C.P, C.I
    tiles = list(range(NT0)) if layer == 0 else list(range(2, NT0))
    P.barrier()
    R = Region(nc, TMP_OFF, SB_END, "pc%d" % layer)
    Wg = [R.t("wg%d" % i, [128, 8, DE], BF16) for i in range(2)]
    Wu = [R.t("wu%d" % i, [128, 8, DE], BF16) for i in range(2)]
    Wd = [R.t("wd%d" % i, [128, 4, D], BF16) for i in range(2)]
    WgB, WuB, WdB = P.bufs("wg", 2), P.bufs("wu", 2), P.bufs("wd", 2)
    A = R.t("A", [128, 4, 2304], BF16)
    sg = [R.t("sg%d" % i, [128, 512], BF16) for i in range(2)]
    sgB = P.bufs("sg", 2)
    tmpf = [R.t("tmpf%d" % i, [128, D], F32) for i in range(2)]
    tmpfB = P.bufs("tmpf", 2)
    wr = R.t("wr", [128, 8, NE], BF16)
    wrB = P.buf("wr")
    ov_off = R.cur
    sts = []
    for i in range(2):
        st = {}
        st["xn"] = R.t("xn%d" % i, [128, D], BF16)
        st["xnB"] = P.buf("xn%d" % i)
        st["junk"], st["junkB"] = st["xn"], st["xnB"]
        st["ss"] = R.t("ss%d" % i, [128, 2], F32)
        st["ssB"] = P.buf("ss%d" % i)
        if i == 0:
            st["eps"] = R.t("eps", [128, 1], F32)
            st["epsB"] = P.buf("eps")
        else:
            st["eps"], st["epsB"] = sts[0]["eps"], sts[0]["epsB"]
        sts.append(st)
    st = sts[0]
    rtB = P.buf("rt")
    hTB = P.bufs("h2T", NT0)
    C.hTB = hTB

    P.op("dve", lambda e: e.memset(st["eps"][:], EPS), writes=[st["epsB"]])
    P.op("pool", lambda e: e.dma_start(out=wr[:], in_=I.wr), writes=[wrB], dma=True)
    load_gate_bc(C, layer, 1, 0, 0)
    if layer == 0:
        load_gate_bc(C, layer, 1, 1, 1)

    def load_expert(e):
        s = e % 2
        P.op("pool", lambda en: en.dma_start(out=Wg[s][:], in_=I.w_gate[layer, e].rearrange("(c p) f -> p c f", p=128)),
             writes=[WgB[s]], dma=True)
        P.op("pool", lambda en: en.dma_start(out=Wu[s][:], in_=I.w_up[layer, e].rearrange("(c p) f -> p c f", p=128)),
             writes=[WuB[s]], dma=True)
        P.op("pool", lambda en: en.dma_start(out=Wd[s][:], in_=I.w_down[layer, e].rearrange("(c p) f -> p c f", p=128)),
             writes=[WdB[s]], dma=True)

    load_expert(0)
    load_expert(1)

    T = len(tiles)
    t0 = tiles[0]
    ssall = R.t("ssall", [128, NT0], F32)
    ssBt = P.bufs("ssall", NT0)
    rstd = R.t("rstd", [128, NT0], F32)
    rstdB = P.buf("rstd")
    norm_stats(C, tiles, ssall, ssBt, rstd, rstdB, [sts[0]["xn"], sts[1]["xn"]], [sts[0]["xnB"], sts[1]["xnB"]], st["eps"], st["epsB"])
    RB = 5
    for i, t in enumerate(tiles):
        who = 1 if t < 2 else 0
        norm_transpose_tile(C, R, C.x_sb[:, t, :], C.xB[t], layer, 1, who, C.hT, hTB[t], t * 128, sts, (6, 7),
                            rstd_ap=rstd[:, i:i + 1], rstdB=rstdB)
        for k in range(8):
            P.op("pe", lambda e, k=k, t=t, i=i: e.matmul(C.ps[RB][:, i * NE:(i + 1) * NE], C.hT[:, k, t * 128:(t + 1) * 128], wr[:, k, :],
                                                         start=(k == 0), stop=(k == 7)),
                 reads=[hTB[t], wrB], writes=[C.psB[RB]])
    s_all = R.t("s_all", [128, NT0, NE], F32)
    sbb = R.t("sbb", [128, NT0, NE], F32)
    t6 = R.t("t6", [128, NT0 * 4, 6], F32)
    gsc = R.t("gsc", [128, NT0 * 4], F32)
    gmx = R.t("gmx", [128, NT0], F32)
    pen = R.t("pen", [128, NT0 * 4], F32)
    msk = R.t("msk", [128, NT0, NE], F32)
    eq = R.t("eq", [128, NT0, NE], F32)
    m2 = R.t("m2", [128, NT0], F32)
    P.op("act", lambda e: e.activation(out=s_all[:, 0:T, :], in_=C.ps[RB][:, 0:T * NE].rearrange("p (t e) -> p t e", e=NE), func=AF.Sigmoid),
         reads=[C.psB[RB]], writes=[rtB])
    P.op("dve", lambda e: e.tensor_tensor(out=sbb[:, 0:T, :], in0=s_all[:, 0:T, :], in1=C.rb[:].unsqueeze(1).to_broadcast([128, T, NE]), op=ALU.add),
         reads=[rtB, C.rbB], writes=[rtB])
    sb4 = sbb[:, 0:T, :].rearrange("p t (g i) -> p (t g) i", i=4)
    G4 = T * 4
    P.op("dve", lambda e: e.tensor_tensor(out=t6[:, 0:G4, 0:3], in0=sb4[:, :, 0:3], in1=sb4[:, :, 1:4], op=ALU.add), reads=[rtB], writes=[rtB])
    P.op("dve", lambda e: e.tensor_tensor(out=t6[:, 0:G4, 3:5], in0=sb4[:, :, 0:2], in1=sb4[:, :, 2:4], op=ALU.add), reads=[rtB], writes=[rtB])
    P.op("dve", lambda e: e.tensor_tensor(out=t6[:, 0:G4, 5:6], in0=sb4[:, :, 0:1], in1=sb4[:, :, 3:4], op=ALU.add), reads=[rtB], writes=[rtB])
    P.op("dve", lambda e: e.tensor_reduce(out=gsc[:, 0:G4], in_=t6[:, 0:G4, :], axis=AX.X, op=ALU.max), reads=[rtB], writes=[rtB])
    P.op("dve", lambda e: e.tensor_reduce(out=gmx[:, 0:T], in_=gsc[:, 0:G4].rearrange("p (t g) -> p t g", g=4), axis=AX.X, op=ALU.max),
         reads=[rtB], writes=[rtB])
    P.op("dve", lambda e: e.tensor_tensor(out=pen[:, 0:G4].rearrange("p (t g) -> p t g", g=4), in0=gsc[:, 0:G4].rearrange("p (t g) -> p t g", g=4),
                                          in1=gmx[:, 0:T].unsqueeze(2).to_broadcast([128, T, 4]), op=ALU.is_ge), reads=[rtB], writes=[rtB])
    P.op("dve", lambda e: e.tensor_scalar(out=pen[:, 0:G4], in0=pen[:, 0:G4], scalar1=100.0, scalar2=-100.0, op0=ALU.mult, op1=ALU.add),
         reads=[rtB], writes=[rtB])
    msk4 = msk[:, 0:T, :].rearrange("p t (g i) -> p (t g) i", i=4)
    P.op("dve", lambda e: e.tensor_tensor(out=msk4, in0=sb4, in1=pen[:, 0:G4].unsqueeze(2).to_broadcast([128, G4, 4]), op=ALU.add),
         reads=[rtB], writes=[rtB])
    P.op("dve", lambda e: e.tensor_reduce(out=gmx[:, 0:T], in_=msk[:, 0:T, :], axis=AX.X, op=ALU.max), reads=[rtB], writes=[rtB])
    P.op("dve", lambda e: e.tensor_tensor(out=eq[:, 0:T, :], in0=msk[:, 0:T, :], in1=gmx[:, 0:T].unsqueeze(2).to_broadcast([128, T, NE]), op=ALU.is_ge),
         reads=[rtB], writes=[rtB])
    P.op("dve", lambda e: e.scalar_tensor_tensor(out=eq[:, 0:T, :], in0=eq[:, 0:T, :], scalar=-200.0, in1=msk[:, 0:T, :], op0=ALU.mult, op1=ALU.add),
         reads=[rtB], writes=[rtB])
    P.op("dve", lambda e: e.tensor_reduce(out=m2[:, 0:T], in_=eq[:, 0:T, :], axis=AX.X, op=ALU.max), reads=[rtB], writes=[rtB])
    P.op("dve", lambda e: e.tensor_tensor(out=eq[:, 0:T, :], in0=msk[:, 0:T, :], in1=m2[:, 0:T].unsqueeze(2).to_broadcast([128, T, NE]), op=ALU.is_ge),
         reads=[rtB], writes=[rtB])
    P.op("dve", lambda e: e.tensor_tensor(out=eq[:, 0:T, :], in0=eq[:, 0:T, :], in1=s_all[:, 0:T, :], op=ALU.mult), reads=[rtB], writes=[rtB])
    P.op("dve", lambda e: e.tensor_reduce(out=m2[:, 0:T], in_=eq[:, 0:T, :], axis=AX.X, op=ALU.add), reads=[rtB], writes=[rtB])
    P.op("dve", lambda e: e.reciprocal(out=m2[:, 0:T], in_=m2[:, 0:T]), reads=[rtB], writes=[rtB])
    P.op("dve", lambda e: e.tensor_tensor(out=C.gatesv[:, t0:t0 + T, :], in0=eq[:, 0:T, :], in1=m2[:, 0:T].unsqueeze(2).to_broadcast([128, T, NE]), op=ALU.mult),
         reads=[rtB], writes=[C.gatesB[t] for t in tiles])

    npieces = 0
    if layer == 0:
        P.barrier()
        RO = Region(nc, ov_off, SB_END, "pcov")
        wa2 = RO.t("wa2", [128, 8, 256], BF16)
        adabp = RO.t("adabp", [2, 256], F32)
        mp = RO.t("mp", [2, 256], F32)
        wa2B, adabpB, mpB = P.buf("wa2"), P.buf("adabp"), P.buf("mp")
        npieces = 24
    blocks = [tiles[i:i + 4] for i in range(0, len(tiles), 4)]
    AB = P.bufs("A", len(blocks))
    fk = 0
    dk = 0

    def gu_block(e, bi):
        nonlocal fk
        s = e % 2
        blk = blocks[bi]
        N = 128 * len(blk)
        tok0 = blk[0] * 128
        hb = [hTB[t] for t in blk]
        for f in range(4):
            q = fk % 2
            fk += 1
            pg, pu = 2 * q, 2 * q + 1
            for k in range(8):
                P.op("pe", lambda en, k=k, f=f, pg=pg: en.matmul(C.ps[pg][:, 0:N], Wg[s][:, k, f * 128:(f + 1) * 128],
                                                                 C.hT[:, k, tok0:tok0 + N], start=(k == 0), stop=(k == 7)),
                     reads=[WgB[s]] + hb, writes=[C.psB[pg]])
            for k in range(8):
                P.op("pe", lambda en, k=k, f=f, pu=pu: en.matmul(C.ps[pu][:, 0:N], Wu[s][:, k, f * 128:(f + 1) * 128],
                                                                 C.hT[:, k, tok0:tok0 + N], start=(k == 0), stop=(k == 7)),
                     reads=[WuB[s]] + hb, writes=[C.psB[pu]])
            P.op("act", lambda en, q=q, pg=pg: en.activation(out=sg[q][:, 0:N], in_=C.ps[pg][:, 0:N], func=AF.Silu),
                 reads=[C.psB[pg]], writes=[sgB[q]])
            P.op("dve", lambda en, f=f, q=q, pu=pu: en.tensor_tensor(out=A[:, f, tok0:tok0 + N], in0=C.ps[pu][:, 0:N], in1=sg[q][:, 0:N], op=ALU.mult),
                 reads=[C.psB[pu], sgB[q]], writes=[AB[bi]])

    def down_block(e, bi):
        nonlocal dk
        s = e % 2
        for t in blocks[bi]:
            who = 1 if t < 2 else 0
            q = dk % 2
            dk += 1
            for half in range(2):
                pb = 4 + 2 * q + half
                for f in range(4):
                    P.op("pe", lambda en, f=f, half=half, pb=pb, t=t: en.matmul(
                        C.ps[pb][:, :], A[:, f, t * 128:(t + 1) * 128], Wd[s][:, f, half * 512:(half + 1) * 512],
                        start=(f == 0), stop=(f == 3)),
                        reads=[AB[bi], WdB[s]], writes=[C.psB[pb]])
                P.op("dve", lambda en, half=half, pb=pb, t=t, who=who, q=q: en.scalar_tensor_tensor(
                    out=tmpf[q][:, half * 512:(half + 1) * 512], in0=C.ps[pb][:, :], scalar=C.gatesv[:, t, e:e + 1],
                    in1=C.gbc[:, who, half * 512:(half + 1) * 512], op0=ALU.mult, op1=ALU.mult),
                    reads=[C.psB[pb], C.gatesB[t], C.gbcB[who]], writes=[tmpfB[q]])
            P.op("pool", lambda en, t=t, q=q: en.tensor_tensor(out=C.x_sb[:, t, :], in0=C.x_sb[:, t, :], in1=tmpf[q][:], op=ALU.add),
                 reads=[tmpfB[q], C.xB[t]], writes=[C.xB[t]])

    nb = len(blocks)
    for e in range(NE):
        for bi in range(nb):
            gu_block(e, bi)
            if bi >= 1:
                down_block(e, bi - 1)
        down_block(e, nb - 1)
        if e + 2 < NE:
            load_expert(e + 2)
        for q in range(e * npieces // NE, (e + 1) * npieces // NE):
            adaln_piece(C, q, wa2, wa2B, adabp, adabpB, mp, mpB)


def phase_final(C):
    nc, P, I = C.nc, C.P, C.I
    P.barrier()
    R = Region(nc, TMP_OFF, SB_END, "pf")
    fg = R.t("fg", [128, D], F32)
    fgB = P.buf("fg")
    junk = R.t("junk", [128, D], BF16)
    junkB = P.buf("junk")
    eps = R.t("eps", [128, 1], F32)
    epsB = P.buf("eps")
    ss = [R.t("ss%d" % i, [128, 2], F32) for i in range(2)]
    ssB = P.bufs("ss", 2)
    ob = [R.t("ob%d" % i, [128, D], F32) for i in range(2)]
    obB = P.bufs("ob", 2)
    P.op("dve", lambda e: e.memset(eps[:], EPS), writes=[epsB])
    P.op("sp", lambda e: e.dma_start(out=fg[:], in_=I.fg_bc), writes=[fgB], dma=True)
    for i, t in enumerate(range(2, NT0)):
        q = i % 2
        P.op("dve", lambda e, q=q: e.memset(ss[q][:], 0.0), writes=[ssB[q]])
        P.op("act", lambda e, t=t, q=q: e.activation(out=junk[:], in_=C.x_sb[:, t, :], func=AF.Square, accum_out=ss[q][:, 0:1]),
             reads=[C.xB[t], ssB[q]], writes=[ssB[q], junkB])
        P.op("act", lambda e, q=q: e.activation(out=ss[q][:, 0:1], in_=ss[q][:, 0:1], func=AF.Sqrt, scale=1.0 / D, bias=eps[:, 0:1]),
             reads=[ssB[q], epsB], writes=[ssB[q]])
        P.op("dve", lambda e, q=q: e.reciprocal(out=ss[q][:, 0:1], in_=ss[q][:, 0:1]), reads=[ssB[q]], writes=[ssB[q]])
        P.op("dve", lambda e, t=t, q=q: e.scalar_tensor_tensor(out=ob[q][:], in0=C.x_sb[:, t, :], scalar=ss[q][:, 0:1], in1=fg[:],
                                                              op0=ALU.mult, op1=ALU.mult),
             reads=[C.xB[t], ssB[q], fgB], writes=[obB[q]])
        P.op("sp", lambda e, t=t, q=q: e.dma_start(out=C.out[(t - 2) * 128:(t - 1) * 128, :], in_=ob[q][:]),
             reads=[obB[q]], dma=True)


def phase_attn(C):
    nc, P, I = C.nc, C.P, C.I
    layer = 1
    C.attn_done = True
    P.barrier()
    R = Region(nc, TMP_OFF, SB_END, "pd")
    qT = R.t("qT", [128, 8, NX], BF16)
    qTB = P.bufs("qT", 4)
    kT = R.t("kT", [128, 4, 2304], BF16)
    kTB = P.bufs("kT", 5)
    v_sb = R.t("v", [128, NT0, 4, 65], BF16)
    vB = P.bufs("v", NT0)
    cos_off = R.cur
    cosT = R.t("cosT", [128, NX], F32)
    sinT = R.t("sinT", [128, NX], F32)
    ropeB = P.buf("rope")
    sub_off = R.cur
    wv = R.t("wv", [128, 8, 256], BF16)
    wvB = P.buf("wv")
    sts = []
    for i in range(2):
        st = {}
        st["xn"] = R.t("xn%d" % i, [128, D], BF16)
        st["xnB"] = P.buf("xn%d" % i)
        st["junk"], st["junkB"] = st["xn"], st["xnB"]
        st["ss"] = R.t("ss%d" % i, [128, 2], F32)
        st["ssB"] = P.buf("ss%d" % i)
        if i == 0:
            st["eps"] = R.t("eps", [128, 1], F32)
            st["epsB"] = P.buf("eps")
        else:
            st["eps"], st["epsB"] = sts[0]["eps"], sts[0]["epsB"]
        sts.append(st)
    st = sts[0]
    hTB = P.bufs("h1T", NT0)

    P.op("dve", lambda e: e.memset(st["eps"][:], EPS), writes=[st["epsB"]])
    P.op("pool", lambda e: e.dma_start(out=wv[:], in_=I.wvv.rearrange("(c p) f -> p c f", p=128)),
         writes=[wvB], dma=True)
    P.op("sp", lambda e: e.dma_start(out=cosT[:], in_=I.rope[0]), writes=[ropeB], dma=True)
    P.op("sp", lambda e: e.dma_start(out=sinT[:], in_=I.rope[1]), writes=[ropeB], dma=True)
    adaln_cols(C, 1)
    load_gate_bc(C, layer, 0, 0, 0)
    for t in range(NT0):
        P.op("pool", lambda e, t=t: e.memset(v_sb[:, t, :, :], 1.0), writes=[vB[t]])

    ssall = R.t("ssall", [128, NT0], F32)
    ssBt = P.bufs("ssall", NT0)
    rstd = R.t("rstd", [128, NT0], F32)
    rstdB = P.buf("rstd")
    norm_stats(C, list(range(NT0)), ssall, ssBt, rstd, rstdB, [sts[0]["xn"], sts[1]["xn"]], [sts[0]["xnB"], sts[1]["xnB"]],
               st["eps"], st["epsB"])
    for t in range(NT0):
        who = 1 if t < 2 else 0
        norm_transpose_tile(C, R, C.x_sb[:, t, :], C.xB[t], layer, 0, who, C.hT, hTB[t], t * 128, sts, (6, 7),
                            rstd_ap=rstd[:, t:t + 1], rstdB=rstdB)
    for t in range(NT0):
        pb = 4 + (t % 2)
        for k in range(8):
            P.op("pe", lambda e, k=k, t=t, pb=pb: e.matmul(C.ps[pb][:, 0:256], C.hT[:, k, t * 128:(t + 1) * 128], wv[:, k, :],
                                                           start=(k == 0), stop=(k == 7)),
                 reads=[hTB[t], wvB], writes=[C.psB[pb]])
        P.op("act", lambda e, t=t, pb=pb: e.activation(out=v_sb[:, t, :, 0:64],
                                                       in_=C.ps[pb][:, 0:256].rearrange("p (h d) -> p h d", d=64), func=AF.Copy),
             reads=[C.psB[pb]], writes=[vB[t]])

    import os
    astop = int(os.environ.get("ATTN_STOP", "9"))
    if astop <= 1:
        return
    P.barrier()
    R2d = Region(nc, sub_off, SB_END, "pd2")
    wq = [R2d.t("wq%d" % i, [128, 8, 128], BF16) for i in range(3)]
    wqB = P.bufs("wq", 3)
    perm = R2d.t("perm", [128, 128], BF16)
    permB = P.buf("perm")
    t1 = R2d.t("t1", [128, 512], F32)
    t2 = R2d.t("t2", [128, 512], F32)
    raw = [R2d.t("raw%d" % i, [128, 512], BF16) for i in range(2)]
    t1B, t2B = P.buf("t1"), P.buf("t2")
    rawB = P.bufs("raw", 2)
    P.op("pool", lambda e: e.dma_start(out=perm[:], in_=I.perm), writes=[permB], dma=True)

    uk = 0
    rk = 0
    for ch in range(12):
        s = ch % 3
        P.op("pool", lambda e, ch=ch, s=s: e.dma_start(out=wq[s][:], in_=I.wqk[ch].rearrange("(c p) f -> p c f", p=128)),
             writes=[wqB[s]], dma=True)
        hk = ch - 8
        if ch >= 8:
            pb = rk % 3
            rk += 1
            for k in range(8):
                P.op("pe", lambda e, k=k, s=s, pb=pb: e.matmul(C.ps[pb][:, 0:256], wq[s][:, k, :], C.hT[:, k, 0:256],
                                                             start=(k == 0), stop=(k == 7)),
                     reads=[wqB[s], hTB[0], hTB[1]], writes=[C.psB[pb]])
            P.op("act", lambda e, hk=hk, pb=pb: e.activation(out=kT[:, hk, 0:256], in_=C.ps[pb][:, 0:256], func=AF.Copy),
                 reads=[C.psB[pb]], writes=[kTB[0]])
        for tg in range(4):
            pb = rk % 3
            rk += 1
            c0 = 256 + tg * 512
            hb = [hTB[2 + tg * 4 + j] for j in range(4)]
            for k in range(8):
                P.op("pe", lambda e, k=k, s=s, pb=pb, c0=c0: e.matmul(C.ps[pb][:, :], wq[s][:, k, :], C.hT[:, k, c0:c0 + 512],
                                                                    start=(k == 0), stop=(k == 7)),
                     reads=[wqB[s]] + hb, writes=[C.psB[pb]])
            q = uk % 2
            uk += 1
            if ch < 8:
                dst, dB = qT[:, ch, tg * 512:(tg + 1) * 512], qTB[tg]
            else:
                dst, dB = kT[:, ch - 8, c0:c0 + 512], kTB[1 + tg]
            if os.environ.get("ATTN_NOROPE"):
                P.op("act", lambda e, dst=dst, pb=pb: e.activation(out=dst, in_=C.ps[pb][:, :], func=AF.Copy),
                     reads=[C.psB[pb]], writes=[dB])
                continue
            P.op("act", lambda e, q=q, pb=pb: e.activation(out=raw[q][:], in_=C.ps[pb][:, :], func=AF.Copy),
                 reads=[C.psB[pb]], writes=[rawB[q]])
            pb2 = 3 + q
            P.op("pe", lambda e, q=q, pb2=pb2: e.matmul(C.ps[pb2][:, :], perm[:], raw[q][:], start=True, stop=True),
                 reads=[rawB[q], permB], writes=[C.psB[pb2]])
            P.op("dve", lambda e, pb=pb, tg=tg: e.tensor_tensor(out=t1[:], in0=C.ps[pb][:, :], in1=cosT[:, tg * 512:(tg + 1) * 512], op=ALU.mult),
                 reads=[C.psB[pb], ropeB], writes=[t1B])
            P.op("dve", lambda e, pb2=pb2, tg=tg: e.tensor_tensor(out=t2[:], in0=C.ps[pb2][:, :], in1=sinT[:, tg * 512:(tg + 1) * 512], op=ALU.mult),
                 reads=[C.psB[pb2], ropeB], writes=[t2B])
            if ch < 8:
                dst, dB = qT[:, ch, tg * 512:(tg + 1) * 512], qTB[tg]
            else:
                dst, dB = kT[:, ch - 8, c0:c0 + 512], kTB[1 + tg]
            P.op("dve", lambda e, dst=dst: e.tensor_tensor(out=dst, in0=t1[:], in1=t2[:], op=ALU.add),
                 reads=[t1B, t2B], writes=[dB])

    if astop <= 2:
        return
    P.barrier()
    R2 = Region(nc, HT_OFF, GBC_OFF, "pd3")
    w_o = R2.t("wo", [128, 8, D], BF16)
    woB = P.buf("wo")
    PT = [R2.t("PT%d" % i, [128, 5, 512], BF16) for i in range(2)]
    PTB = [P.bufs("PT%d_" % i, 5) for i in range(2)]
    o_tok0 = R2.t("otok", [128, D], BF16)
    oT0 = R2.t("oT", [128, 8, 128], BF16)
    tmpf = R2.t("tmpf", [128, D], F32)
    tmpfB = P.buf("tmpf")
    maskT = R2.t("maskT", [128, 2, 128], F32)
    maskB = P.buf("maskT")
    esink = R2.t("esink", [128, NE], F32)
    esB = P.buf("esink")
    rsum = R2.t("rsum", [128, 4], F32)
    rsB = P.buf("rsum")
    for h in range(2):
        P.op("pool", lambda e, h=h: e.dma_start(out=w_o[:, h * 4:(h + 1) * 4, :],
                                               in_=I.b_w_o[h * 512:(h + 1) * 512, :].rearrange("(c p) f -> p c f", p=128)),
             writes=[woB], dma=True)
    P.op("sp", lambda e: e.dma_start(out=maskT[:], in_=I.mask.rearrange("p (a b) -> p a b", a=3)[:, 0:2, :]), writes=[maskB], dma=True)
    P.op("sp", lambda e: e.dma_start(out=esink[:], in_=I.sink_bc), writes=[esB], dma=True)
    P.op("act", lambda e: e.activation(out=esink[:], in_=esink[:], func=AF.Exp), reads=[esB], writes=[esB])
    R3 = Region(nc, cos_off, cos_off + 16384, "pd3b")
    qz = [R3.t("qz%d" % i, [128, 4, 128], BF16) for i in range(2)]
    qzB = P.bufs("qz", 2)
    mask4 = R3.t("mask4", [128, 2, 4, 128], BF16)
    mask4B = P.buf("mask4")
    o_toks = [o_tok0, R3.t("otok1", [128, D], BF16)]
    otBs = P.bufs("otok", 2)
    oTs = [oT0, R3.t("oT1", [128, 8, 128], BF16)]
    oTBs = P.bufs("oT", 2)
    for j in range(2):
        P.op("pool", lambda e, j=j: e.memset(qz[j][:], 0.0), writes=[qzB[j]])
    P.op("dve", lambda e: e.tensor_copy(out=mask4[:], in_=maskT[:].unsqueeze(2).to_broadcast([128, 2, 4, 128])),
         reads=[maskB], writes=[mask4B])
    gk = 0

    def attn_tail(i):
        t = 2 + i
        o_tok, otB, oT, oTB = o_toks[i % 2], otBs[i % 2], oTs[i % 2], oTBs[i % 2]
        pt = ps_bf16(C, 6)
        for k in range(8):
            P.op("pe", lambda e, k=k: e.transpose(out=pt[:, k * 128:(k + 1) * 128], in_=o_tok[:, k * 128:(k + 1) * 128], identity=C.ident_b[:]),
                 reads=[otB, C.identB], writes=[C.psB[6]])
        P.op("act", lambda e: e.activation(out=oT[:].rearrange("p k t -> p (k t)"), in_=pt[:, :], func=AF.Copy),
             reads=[C.psB[6]], writes=[oTB])
        for half in range(2):
            pb = 6 + half
            for k in range(8):
                P.op("pe", lambda e, k=k, half=half, pb=pb: e.matmul(C.ps[pb][:, :], oT[:, k, :], w_o[:, k, half * 512:(half + 1) * 512],
                                                                   start=(k == 0), stop=(k == 7)),
                     reads=[oTB, woB], writes=[C.psB[pb]])
            P.op("dve", lambda e, half=half, pb=pb: e.tensor_tensor(out=tmpf[:, half * 512:(half + 1) * 512], in0=C.ps[pb][:, :],
                                                                    in1=C.gbc[:, 0, half * 512:(half + 1) * 512], op=ALU.mult),
                 reads=[C.psB[pb], C.gbcB[0]], writes=[tmpfB])
        P.op("pool", lambda e, t=t: e.tensor_tensor(out=C.x_sb[:, t, :], in0=C.x_sb[:, t, :], in1=tmpf[:], op=ALU.add),
             reads=[tmpfB, C.xB[t]], writes=[C.xB[t]])

    for i in range(16):
        t = 2 + i
        o_tok, otB = o_toks[i % 2], otBs[i % 2]
        blocks = [(0, 0, None), (128, 1, None)]
        for d in (-1, 0, 1):
            j = i + d
            if j < 0 or j > 15:
                continue
            blocks.append((256 + j * 128, 2 + j, {-1: 0, 0: None, 1: 1}[d]))
        tgq = i // 4
        for hk in range(4):
            pq = gk % 2
            gk += 1
            P.op("pool", lambda e, pq=pq, hk=hk, i=i: e.tensor_copy(out=qz[pq][0:64, 0:2, :], in_=qT[0:64, 2 * hk:2 * hk + 2, i * 128:(i + 1) * 128]),
                 reads=[qTB[tgq]], writes=[qzB[pq]])
            P.op("pool", lambda e, pq=pq, hk=hk, i=i: e.tensor_copy(out=qz[pq][64:128, 2:4, :], in_=qT[64:128, 2 * hk:2 * hk + 2, i * 128:(i + 1) * 128]),
                 reads=[qTB[tgq]], writes=[qzB[pq]])
            for bi, (kc, vt, mi) in enumerate(blocks):
                kb = kTB[0] if bi < 2 else kTB[1 + (vt - 2) // 4]
                P.op("pe", lambda e, bi=bi, kc=kc, hk=hk, pq=pq, mi=mi: e.matmul(
                    C.ps[bi][:, :], kT[:, hk, kc:kc + 128], qz[pq][:].rearrange("p a b -> p (a b)"),
                    start=True, stop=(mi is None)),
                    reads=[kb, qzB[pq]], writes=[C.psB[bi]])
                if mi is not None:
                    P.op("pe", lambda e, bi=bi, mi=mi: e.matmul(
                        C.ps[bi][:, :], C.ident_b[:], mask4[:, mi, :, :].rearrange("p a b -> p (a b)"), start=False, stop=True),
                        reads=[C.identB, mask4B], writes=[C.psB[bi]])
                P.op("act", lambda e, bi=bi, pq=pq: e.activation(out=PT[pq][:, bi, :], in_=C.ps[bi][:, :], func=AF.Exp, scale=0.125),
                     reads=[C.psB[bi]], writes=[PTB[pq][bi]])
            nb = len(blocks)
            for slot in range(4):
                for bi, (kc, vt, mi) in enumerate(blocks):
                    P.op("pe", lambda e, slot=slot, bi=bi, vt=vt, pq=pq, hk=hk, nb=nb: e.matmul(
                        C.ps[5][:, slot * 65:(slot + 1) * 65], PT[pq][:, bi, slot * 128:(slot + 1) * 128], v_sb[:, vt, hk, :],
                        start=(bi == 0), stop=(bi == nb - 1)),
                        reads=[PTB[pq][bi], vB[vt]], writes=[C.psB[5]])
            O = C.ps[5][:, 0:260].rearrange("p (b c d) -> p b c d", b=2, c=2)
            es = esink[:, hk * 4:(hk + 1) * 4].rearrange("p (c b) -> p b c", b=2)
            P.op("dve", lambda e, O=O, es=es: e.tensor_tensor(out=rsum[:].rearrange("p (b c) -> p b c", b=2), in0=O[:, :, :, 64], in1=es, op=ALU.add),
                 reads=[C.psB[5], esB], writes=[rsB])
            P.op("dve", lambda e: e.reciprocal(out=rsum[:], in_=rsum[:]), reads=[rsB], writes=[rsB])
            od = o_tok[:, hk * 256:(hk + 1) * 256].rearrange("p (c b d) -> p b c d", c=2, b=2)
            P.op("dve", lambda e, O=O, od=od: e.tensor_tensor(
                out=od, in0=O[:, :, :, 0:64], in1=rsum[:].rearrange("p (b c) -> p b c", b=2).unsqueeze(3).to_broadcast([128, 2, 2, 64]),
                op=ALU.mult),
                reads=[C.psB[5], rsB], writes=[otB])
            if hk == 0 and i >= 1:
                attn_tail(i - 1)
    attn_tail(15)
```

```python
import numpy as np
import concourse.bass as bass
import concourse.mybir as mybir
from concourse.bass_utils import run_bass_kernel_spmd

F32 = mybir.dt.float32
BF16 = mybir.dt.bfloat16
AF = mybir.ActivationFunctionType
ALU = mybir.AluOpType
AX = mybir.AxisListType


class Buf:
    __slots__ = ("name", "writers", "readers", "excl")

    def __init__(self, name):
        self.name = name
        self.writers = []
        self.readers = []
        self.excl = False


class Op:
    __slots__ = ("eng", "fn", "deps", "dma", "sem", "cnt", "need_inc", "k")

    def __init__(self, eng, fn, dma):
        self.eng = eng
        self.fn = fn
        self.deps = []
        self.dma = dma
        self.sem = None
        self.cnt = 0
        self.need_inc = False
        self.k = -1


class Prog:
    ENGS = ("pe", "act", "dve", "pool", "sp")
    NDMA = 16
    LIMIT = 8000

    def __init__(self, nc):
        self.nc = nc
        self.ops = {e: [] for e in self.ENGS}
        self.ndma = 0
        self.dma_ops = []
        self.dma_q = {}
        self.all_bufs = []
        self.barrier_marks = []

    def buf(self, name):
        b = Buf(name)
        b.writers = list(self.barrier_marks)
        self.all_bufs.append(b)
        return b

    def bufs(self, name, n):
        return [self.buf("%s%d" % (name, i)) for i in range(n)]

    def op(self, eng, fn, reads=(), writes=(), dma=False, extra=()):
        o = Op(eng, fn, dma)
        deps = o.deps
        xr = [b for b in reads if b.excl and b not in writes]
        if xr:
            writes = list(writes) + xr
        for b in reads:
            for w in b.writers:
                deps.append(w)
        for b in writes:
            if b.readers:
                for r in b.readers:
                    if r is o:
                        continue
                    if r.eng == eng and eng == "pe" and not r.dma and not dma:
                        continue
                    deps.append(r)
                b.writers = []
                b.readers = []
            else:
                for w in b.writers:
                    if w.eng != eng or w.dma or dma or eng != "pe":
                        deps.append(w)
        for d in extra:
            deps.append(d)
        for b in reads:
            b.readers.append(o)
        for b in writes:
            b.writers.append(o)
        if dma:
            q = self.dma_q.setdefault(eng, [])
            o.k = len(q)
            if len(q) >= self.NDMA:
                deps.append(q[len(q) - self.NDMA])
            q.append(o)
            self.ndma += 1
            self.dma_ops.append(o)
        self.ops[eng].append(o)
        return o

    def barrier(self):
        marks = []
        for q in self.dma_q.values():
            marks.extend(q[-self.NDMA:])
        for e in self.ENGS:
            for o in reversed(self.ops[e]):
                if not o.dma:
                    marks.append(o)
                    break
        self.barrier_marks = marks
        for b in self.all_bufs:
            b.writers = list(marks)
            b.readers = []

    def emit(self):
        nc = self.nc
        for e in self.ENGS:
            for o in self.ops[e]:
                for d in o.deps:
                    if not d.dma:
                        d.need_inc = True
        self._sem_ctx = []
        for qn, q in self.dma_q.items():
            dsems = []
            for i in range(min(self.NDMA, len(q))):
                c = nc.semaphore("dq_%s_%d" % (qn, i))
                dsems.append(c.__enter__())
                self._sem_ctx.append(c)
            dcount = [0] * self.NDMA
            for o in q:
                s = o.k % self.NDMA
                dcount[s] += 16
                o.sem = dsems[s]
                o.cnt = dcount[s]
        for e in self.ENGS:
            cur = None
            n = self.LIMIT
            si = 0
            for o in self.ops[e]:
                if o.dma or not o.need_inc:
                    continue
                if n >= self.LIMIT:
                    c = nc.semaphore("e_%s_%d" % (e, si))
                    cur = c.__enter__()
                    self._sem_ctx.append(c)
                    si += 1
                    n = 0
                n += 1
                o.sem = cur
                o.cnt = n
        engmap = {"pe": nc.tensor, "act": nc.scalar, "dve": nc.vector, "pool": nc.gpsimd, "sp": nc.sync}

        def run(ename, eng):
            waited = {}
            for o in self.ops[ename]:
                need = {}
                for d in o.deps:
                    if d.sem is None:
                        continue
                    key = d.sem
                    if need.get(key, (0,))[0] < d.cnt:
                        need[key] = (d.cnt, d.sem)
                for key, (cnt, sem) in need.items():
                    if waited.get(key, 0) >= cnt:
                        continue
                    eng.wait_ge(sem, cnt)
                    waited[key] = cnt
                inst = o.fn(eng)
                if o.dma:
                    inst.then_inc(o.sem, 16)
                elif o.need_inc:
                    inst.then_inc(o.sem, 1)

        with nc.Block() as block:
            @block.tensor
            def _(e):
                run("pe", e)

            @block.scalar
            def _(e):
                run("act", e)

            @block.vector
            def _(e):
                run("dve", e)

            @block.gpsimd
            def _(e):
                run("pool", e)

            @block.sync
            def _(e):
                run("sp", e)
        for c in reversed(self._sem_ctx):
            c.__exit__(None, None, None)


def _nop_fn(ename):
    def f(eng):
        return eng.nop()
    return f


D = 1024
NCTX = 256
NX = 2048
NT0 = 18
EPS = 1e-6
NE = 16
DE = 512
BASE = 16640
X_OFF = BASE
HT_OFF = X_OFF + 18 * 4096
GBC_OFF = HT_OFF + 8 * 2304 * 2
SM_OFF = GBC_OFF + 8192
TMP_OFF = SM_OFF + 4096
SB_END = 229376
_DT_SIZE = {F32: 4, BF16: 2}


class Ctx:
    pass


class Region:
    def __init__(self, nc, lo, hi, tag):
        self.nc, self.lo, self.hi, self.cur, self.tag = nc, lo, hi, lo, tag
        self.n = 0

    def t(self, name, shape, dtype):
        nbytes = _DT_SIZE[dtype]
        for s in shape[1:]:
            nbytes *= s
        nbytes = (nbytes + 31) // 32 * 32
        assert self.cur + nbytes <= self.hi, (self.tag, name, self.cur + nbytes - self.hi)
        h = self.nc.alloc_sbuf_tensor_at("%s_%s" % (self.tag, name), list(shape), dtype, offset=self.cur)
        self.cur += nbytes
        return h


def build_program(stop_after=None, dbg=False):
    nc = bass.Bass("TRN2", target_bir_lowering=False)
    P = Prog(nc)
    C = Ctx()
    C.nc, C.P = nc, P

    def din(name, shape):
        return nc.dram_tensor(name, list(shape), F32, kind="ExternalInput").ap()

    I = Ctx()
    I.x = din("x", [NX, D])
    I.ctx = din("ctx", [NCTX, D])
    I.ccol = din("ccol", [128, 8, 2])
    I.ada_w = din("ada_w", [2, 12, D, 512])
    I.adab2 = din("adab2", [2, 2, 6 * D])
    I.ncol = din("ncol", [128, 2, 2, 8])
    I.a_w_in = din("a_w_in", [8, D, 512])
    I.a_w_out = din("a_w_out", [2048, D])
    I.gv_bc = din("gv_bc", [128, 2048])
    I.wsT = din("wsT", [128, 8, 128])
    I.bs_bc = din("bs_bc", [128, 8, 128])
    I.wqk = din("wqk", [12, D, 128])
    I.wvv = din("wvv", [D, 256])
    I.b_w_o = din("b_w_o", [D, D])
    I.sink_bc = din("sink_bc", [128, 16])
    I.wr = din("wr", [128, 8, 16])
    I.rb_bc = din("rb_bc", [128, 16])
    I.w_gate = din("moe_w_gate", [2, NE, D, DE])
    I.w_up = din("moe_w_up", [2, NE, D, DE])
    I.w_down = din("moe_w_down", [2, NE, DE, D])
    I.fg_bc = din("fg_bc", [128, D])
    I.rope = din("rope", [2, 128, NX])
    I.perm = din("perm", [128, 128])
    I.mask = din("mask", [128, 384])
    C.I = I
    C.out = nc.dram_tensor("out", [NX, D], F32, kind="ExternalOutput").ap()
    C.msc = nc.dram_tensor("msc", [2, 2, 6 * D], F32, kind="Internal").ap()
    C.mscB = P.buf("msc")
    C.dbg = dbg
    if dbg:
        C.dbg_x = nc.dram_tensor("dbg_x", [NT0 * 128, D], F32, kind="ExternalOutput").ap()
        C.dbg_m = nc.dram_tensor("dbg_m", [2, 2, 6 * D], F32, kind="ExternalOutput").ap()

    C.x_sb = nc.alloc_sbuf_tensor_at("x_sb", [128, NT0, D], F32, offset=X_OFF)
    C.xB = P.bufs("x", NT0)
    C.hT = nc.alloc_sbuf_tensor_at("hT", [128, 8, 2304], BF16, offset=HT_OFF)
    C.gbc = nc.alloc_sbuf_tensor_at("gbc", [128, 2, D], F32, offset=GBC_OFF)
    C.gbcB = P.bufs("gbc", 2)
    sm = Region(nc, SM_OFF, TMP_OFF, "sm")
    C.cols = sm.t("cols", [128, 2, 4, 8, 2], F32)
    C.colsB = P.buf("cols")
    C.gatesv = sm.t("gatesv", [128, NT0, NE], F32)
    C.gatesB = P.bufs("gates", NT0)
    C.ident_f = sm.t("ident_f", [128, 128], F32)
    C.ident_b = sm.t("ident_b", [128, 128], BF16)
    C.identB = P.buf("ident")
    C.rb = sm.t("rb", [128, NE], F32)
    C.rbB = P.buf("rb")
    C.ncol = sm.t("ncol", [128, 2, 2, 8], F32)
    C.ncolB = P.buf("ncol")
    C.sclhs = sm.t("sclhs", [128, 8, 2], BF16)
    C.scB = P.buf("sclhs")
    C.ps = [nc.alloc_psum_tensor("ps%d" % i, [128, 512], F32) for i in range(8)]
    C.psB = P.bufs("ps", 8)
    for b in C.psB:
        b.excl = True
    C.ntt = 0

    P.op("pool", lambda e: e.memset(C.ident_f[:], 1.0), writes=[C.identB])
    P.op("pool", lambda e: e.affine_select(out=C.ident_f[:], in_=C.ident_f[:], pattern=[[-1, 128]],
                                           compare_op=ALU.is_equal, fill=0.0, base=0, channel_multiplier=1),
         reads=[C.identB], writes=[C.identB])
    P.op("dve", lambda e: e.tensor_copy(out=C.ident_b[:], in_=C.ident_f[:]), reads=[C.identB], writes=[C.identB])
    P.op("sp", lambda e: e.dma_start(out=C.rb[:], in_=I.rb_bc), writes=[C.rbB], dma=True)
    P.op("sp", lambda e: e.dma_start(out=C.ncol[:], in_=I.ncol), writes=[C.ncolB], dma=True)

    phases = stop_after if (stop_after and stop_after.startswith("!")) else None
    if phases is not None:
        for ph in phases[1:]:
            if ph == "A":
                phase_adaln(C)
            elif ph == "L":
                for t in range(NT0):
                    src = C.I.ctx[t * 128:(t + 1) * 128, :] if t < 2 else C.I.x[(t - 2) * 128:(t - 1) * 128, :]
                    P.op("sp", lambda e, t=t, src=src: e.dma_start(out=C.x_sb[:, t, :], in_=src), writes=[C.xB[t]], dma=True)
            elif ph == "B":
                phase_gmlp(C)
            elif ph == "C":
                phase_moe(C, 0)
            elif ph == "D":
                phase_attn(C)
            elif ph == "E":
                phase_moe(C, 1)
            elif ph == "F":
                phase_final(C)
        return finish(C)
    phase_adaln(C)
    if stop_after == "A":
        return finish(C)
    phase_gmlp(C)
    if stop_after == "B":
        return finish(C)
    phase_moe(C, 0)
    if stop_after == "C":
        return finish(C)
    phase_attn(C)
    if stop_after == "D":
        return finish(C)
    phase_moe(C, 1)
    phase_final(C)
    return finish(C)


def finish(C):
    P = C.P
    if C.dbg:
        for t in range(2 if getattr(C, "attn_done", False) else 0, NT0):
            P.op("sp", lambda e, t=t: e.dma_start(out=C.dbg_x[t * 128:(t + 1) * 128, :], in_=C.x_sb[:, t, :]),
                 reads=[C.xB[t]], dma=True)
        P.op("sp", lambda e: e.dma_start(out=C.dbg_m, in_=C.msc), reads=[C.mscB], dma=True)
    P.barrier()
    P.op("sp", lambda e: e.nop(), reads=[C.mscB])
    P.emit()
    return C.nc


def adaln_cols(C, layer):
    P = C.P
    for vi, vec in enumerate((0, 1, 3, 4)):
        for who in range(2):
            P.op("sp", lambda e, vi=vi, vec=vec, who=who: e.dma_start(
                out=C.cols[:, layer, vi, :, who],
                in_=C.msc[layer, who, vec * 1024:(vec + 1) * 1024].rearrange("(k p) -> p k", p=128),
                allow_slow_non_contiguous=True),
                reads=[C.mscB], writes=[C.colsB], dma=True)
    for vi, wn in ((1, 0), (3, 1)):
        for who in range(2):
            P.op("dve", lambda e, vi=vi, wn=wn, who=who: e.scalar_tensor_tensor(
                out=C.cols[:, layer, vi, :, who], in0=C.cols[:, layer, vi, :, who], scalar=1.0,
                in1=C.ncol[:, layer, wn, :], op0=ALU.add, op1=ALU.mult),
                reads=[C.colsB, C.ncolB], writes=[C.colsB])


def phase_adaln(C):
    nc, P, I = C.nc, C.P, C.I
    P.barrier()
    R = Region(nc, TMP_OFF, SB_END, "pa")
    m_sb = R.t("m", [2, 6 * D], F32)
    mB = P.buf("m")
    adab = R.t("adab", [2, 6 * D], F32)
    adabB = P.buf("adab")
    wa = [R.t("wa%d" % i, [128, 8, 512], BF16) for i in range(3)]
    waB = P.bufs("wa", 3)
    ccol = R.t("ccol", [128, 8, 2], F32)
    ccB = P.buf("ccol")
    P.op("sp", lambda e: e.dma_start(out=ccol[:], in_=I.ccol), writes=[ccB], dma=True)
    P.op("act", lambda e: e.activation(out=C.sclhs[:], in_=ccol[:], func=AF.Silu), reads=[ccB], writes=[C.scB])
    layer = 0
    P.op("sp", lambda e: e.dma_start(out=adab[:], in_=I.adab2[layer]), writes=[adabB], dma=True)
    for nb in range(12):
        j = nb % 3
        P.op("pool", lambda e, nb=nb, j=j: e.dma_start(out=wa[j][:], in_=I.ada_w[layer, nb].rearrange("(c p) f -> p c f", p=128)),
             writes=[waB[j]], dma=True)
        pb = nb % 2
        for kk in range(8):
            P.op("pe", lambda e, j=j, kk=kk, pb=pb: e.matmul(C.ps[pb][0:2, :], C.sclhs[:, kk, :], wa[j][:, kk, :],
                                                           start=(kk == 0), stop=(kk == 7)),
                 reads=[waB[j], C.scB], writes=[C.psB[pb]])
        P.op("dve", lambda e, nb=nb, pb=pb: e.tensor_tensor(out=m_sb[:, nb * 512:(nb + 1) * 512], in0=C.ps[pb][0:2, :],
                                                            in1=adab[:, nb * 512:(nb + 1) * 512], op=ALU.add),
             reads=[C.psB[pb], adabB], writes=[mB])
    P.op("sp", lambda e: e.dma_start(out=C.msc[layer], in_=m_sb[:]), reads=[mB], writes=[C.mscB], dma=True)
    adaln_cols(C, layer)


def adaln_piece_load(C, q, wa2, wa2B, adabp, adabpB):
    P, I = C.P, C.I
    nb, h = q // 2, q % 2
    c0 = nb * 512 + h * 256
    P.op("pool", lambda e: e.dma_start(out=wa2[:], in_=I.ada_w[1, nb][:, h * 256:(h + 1) * 256].rearrange("(c p) f -> p c f", p=128)),
         writes=[wa2B], dma=True)
    P.op("sp", lambda e: e.dma_start(out=adabp[:], in_=I.adab2[1][:, c0:c0 + 256]), writes=[adabpB], dma=True)


def adaln_piece(C, q, wa2, wa2B, adabp, adabpB, mp, mpB):
    P, I = C.P, C.I
    nb, h = q // 2, q % 2
    c0 = nb * 512 + h * 256
    for kk in range(8):
        P.op("pe", lambda e, kk=kk: e.matmul(C.ps[0][0:2, 0:256], C.sclhs[:, kk, :], wa2[:, kk, :], start=(kk == 0), stop=(kk == 7)),
             reads=[wa2B, C.scB], writes=[C.psB[0]])
    P.op("dve", lambda e: e.tensor_tensor(out=mp[:], in0=C.ps[0][0:2, 0:256], in1=adabp[:], op=ALU.add),
         reads=[C.psB[0], adabpB], writes=[mpB])
    P.op("sp", lambda e: e.dma_start(out=C.msc[1][:, c0:c0 + 256], in_=mp[:]), reads=[mpB], writes=[C.mscB], dma=True)


def load_gate_bc(C, layer, gi, who, slot):
    vec = 2 if gi == 0 else 5
    src = C.msc[layer, who:who + 1, vec * 1024:(vec + 1) * 1024]
    C.P.op("sp", lambda e: e.dma_start(out=C.gbc[:, slot, :], in_=src.partition_broadcast(128)),
           reads=[C.mscB], writes=[C.gbcB[slot]], dma=True)


def ps_bf16(C, b):
    return C.ps[b][:].bitcast(BF16)


def norm_stats(C, tiles, ssall, ssBt, rstd, rstdB, junks, junkBs, eps, epsB):
    P = C.P
    T = len(tiles)
    for i, t in enumerate(tiles):
        P.op("dve", lambda e, i=i: e.memset(ssall[:, i:i + 1], 0.0), writes=[ssBt[i]])
    for i, t in enumerate(tiles):
        j = i % len(junks)
        P.op("act", lambda e, i=i, t=t, j=j: e.activation(out=junks[j][:, 0:D], in_=C.x_sb[:, t, :], func=AF.Square, accum_out=ssall[:, i:i + 1]),
             reads=[C.xB[t], ssBt[i]], writes=[ssBt[i], junkBs[j]])
    P.op("act", lambda e: e.activation(out=rstd[:, 0:T], in_=ssall[:, 0:T], func=AF.Sqrt, scale=1.0 / D, bias=eps[:, 0:1]),
         reads=list(ssBt[:T]) + [epsB], writes=[rstdB])
    P.op("dve", lambda e: e.reciprocal(out=rstd[:, 0:T], in_=rstd[:, 0:T]), reads=[rstdB], writes=[rstdB])


def norm_transpose_tile(C, R, src_ap, srcB, layer, which, who, dstT, dstB, tok0, st, ps_bank, rstd_ap=None, rstdB=None):
    P = C.P
    if isinstance(st, list):
        st = st[C.ntt % len(st)]
    if isinstance(ps_bank, (list, tuple)):
        ps_bank = ps_bank[C.ntt % len(ps_bank)]
    sh_v, gm_v = (0, 1) if which == 0 else (2, 3)
    ss, ssB = st["ss"], st["ssB"]
    if rstd_ap is not None:
        P.op("dve", lambda e: e.tensor_scalar(out=st["xn"][:], in0=src_ap, scalar1=rstd_ap, scalar2=None, op0=ALU.mult),
             reads=[srcB, rstdB], writes=[st["xnB"]])
    else:
        _norm_stats_single(C, src_ap, srcB, st)
    _transpose_evac(C, layer, which, who, dstT, dstB, tok0, st, ps_bank)


def _norm_stats_single(C, src_ap, srcB, st):
    P = C.P
    ss, ssB = st["ss"], st["ssB"]
    P.op("dve", lambda e: e.memset(ss[:], 0.0), writes=[ssB])
    P.op("act", lambda e: e.activation(out=st["junk"][:, 0:D], in_=src_ap, func=AF.Square, accum_out=ss[:, 0:1]),
         reads=[srcB, ssB], writes=[ssB, st["junkB"]])
    P.op("act", lambda e: e.activation(out=ss[:, 0:1], in_=ss[:, 0:1], func=AF.Sqrt, scale=1.0 / D, bias=st["eps"][:, 0:1]),
         reads=[ssB, st["epsB"]], writes=[ssB])
    P.op("dve", lambda e: e.reciprocal(out=ss[:, 0:1], in_=ss[:, 0:1]), reads=[ssB], writes=[ssB])
    P.op("dve", lambda e: e.tensor_scalar(out=st["xn"][:], in0=src_ap, scalar1=ss[:, 0:1], scalar2=None, op0=ALU.mult),
         reads=[srcB, ssB], writes=[st["xnB"]])


def _transpose_evac(C, layer, which, who, dstT, dstB, tok0, st, ps_bank):
    P = C.P
    sh_v, gm_v = (0, 1) if which == 0 else (2, 3)
    pt = ps_bf16(C, ps_bank)
    use_act = (C.ntt % 2 == 0)
    C.ntt += 1
    for k in range(8):
        P.op("pe", lambda e, k=k: e.transpose(out=pt[:, k * 128:(k + 1) * 128], in_=st["xn"][:, k * 128:(k + 1) * 128],
                                              identity=C.ident_b[:]),
             reads=[st["xnB"], C.identB], writes=[C.psB[ps_bank]])
    for k in range(8):
        gm = C.cols[:, layer, gm_v, k, who:who + 1]
        sh = C.cols[:, layer, sh_v, k, who:who + 1]
        if use_act:
            P.op("act", lambda e, k=k, gm=gm, sh=sh: e.activation(out=dstT[:, k, tok0:tok0 + 128], in_=pt[:, k * 128:(k + 1) * 128],
                                                                 func=AF.Identity, scale=gm, bias=sh),
                 reads=[C.psB[ps_bank], C.colsB], writes=[dstB])
        else:
            P.op("dve", lambda e, k=k, gm=gm, sh=sh: e.tensor_scalar(out=dstT[:, k, tok0:tok0 + 128], in0=pt[:, k * 128:(k + 1) * 128],
                                                                    scalar1=gm, scalar2=sh, op0=ALU.mult, op1=ALU.add),
                 reads=[C.psB[ps_bank], C.colsB], writes=[dstB])


def make_norm_scratch(C, R, tag):
    P = C.P
    st = {}
    st["junk"] = R.t(tag + "junk", [128, 2048], BF16)
    st["junkB"] = P.buf(tag + "junk")
    st["ss"] = R.t(tag + "ss", [128, 2], F32)
    st["ssB"] = P.buf(tag + "ss")
    st["xn"] = R.t(tag + "xn", [128, D], BF16)
    st["xnB"] = P.buf(tag + "xn")
    st["eps"] = R.t(tag + "eps", [128, 1], F32)
    epsB = P.buf(tag + "eps")
    P.op("dve", lambda e: e.memset(st["eps"][:], EPS), writes=[epsB])
    st["epsB"] = epsB
    return st


def phase_gmlp(C):
    nc, P, I = C.nc, C.P, C.I
    layer = 0
    P.barrier()
    R = Region(nc, TMP_OFF, SB_END, "pb")
    wi = [R.t("wi%d" % i, [128, 8, 512], BF16) for i in range(2)]
    wiB = P.bufs("wi", 2)
    hTg = R.t("hTg", [128, 8, 512], BF16)
    hTgB = P.buf("hTg")
    uT = R.t("uT", [128, 16, 512], BF16)
    uTB = P.buf("uT")
    vraw4 = R.t("vraw", [128, 4, 2048], BF16)
    vrawB4 = P.bufs("vraw", 4)
    prodT = [R.t("prodT%d" % i, [128, 16, 128], BF16) for i in range(2)]
    prodB = P.bufs("prodT", 2)
    sts = []
    for i in range(2):
        st = {}
        st["xn"] = R.t("xn%d" % i, [128, D], BF16)
        st["xnB"] = P.buf("xn%d" % i)
        st["junk"], st["junkB"] = st["xn"], st["xnB"]
        st["ss"] = R.t("ss%d" % i, [128, 2], F32)
        st["ssB"] = P.buf("ss%d" % i)
        sts.append(st)
    eps = R.t("eps", [128, 1], F32)
    epsB = P.buf("eps")
    ssall = R.t("ssall", [128, 4], F32)
    ssBt = P.bufs("ssall", 4)
    rstd = R.t("rstd", [128, 4], F32)
    rstdB = P.buf("rstd")
    ssv = R.t("ssv", [128, 4, 2], F32)
    ssvBt = P.bufs("ssv", 4)
    rstdv = R.t("rstdv", [128, 4], F32)
    rstdvB = P.buf("rstdv")
    tmpf = [R.t("tmpf%d" % i, [128, D], F32) for i in range(2)]
    tmpfB = P.bufs("tmpf", 2)
    gv = R.t("gv", [128, 2048], BF16)
    gvB = P.buf("gv")
    bsrow = R.t("bsrow", [1, 8, 128], BF16)
    ones1 = R.t("ones1", [1, 128], BF16)
    bsB = P.buf("bs")
    wsT = R.t("wsT", [128, 8, 128], BF16)
    wsB = P.buf("ws")
    w_out = nc.alloc_sbuf_tensor_at("pb_wout", [128, 16, D], BF16, offset=HT_OFF)
    woB = P.buf("wout")

    P.op("dve", lambda e: e.memset(eps[:], EPS), writes=[epsB])
    P.op("dve", lambda e: e.memset(ones1[:], 1.0), writes=[bsB])
    P.op("pool", lambda e: e.dma_start(out=gv[:], in_=I.gv_bc), writes=[gvB], dma=True)
    P.op("pool", lambda e: e.dma_start(out=wsT[:], in_=I.wsT), writes=[wsB], dma=True)
    P.op("pool", lambda e: e.dma_start(out=bsrow[:], in_=I.bs_bc[0:1]), writes=[bsB], dma=True)
    load_gate_bc(C, layer, 0, 0, 0)
    load_gate_bc(C, layer, 0, 1, 1)
    for h in range(2):
        P.op("pool", lambda e, h=h: e.dma_start(out=w_out[:, h * 8:(h + 1) * 8, :],
                                               in_=I.a_w_out[h * 1024:(h + 1) * 1024, :].rearrange("(c p) f -> p c f", p=128)),
             writes=[woB], dma=True)

    groups = [[0, 1, 2, 3], [4, 5, 6, 7], [8, 9, 10, 11], [12, 13, 14, 15], [16, 17]]
    ring = [1, 2, 3]
    rk = 0
    pmk = 0
    wk = 0
    tk = 0
    st8 = {"rk": 0, "pmk": 0, "wk": 0, "tk": 0}
    wsc = nc.dram_tensor("w_in_bf16", [8, 128, 8 * 512], BF16, kind="Internal").ap()
    wscB = P.bufs("wsc", 8)

    def load_piece(pc, s, gi):
        if gi == 0:
            P.op("pool", lambda e: e.dma_start(out=wi[s][:], in_=I.a_w_in[pc].rearrange("(c p) f -> p c f", p=128)),
                 writes=[wiB[s]], dma=True)
            P.op("sp", lambda e: e.dma_start(out=wsc[pc], in_=wi[s][:].rearrange("p c f -> p (c f)")),
                 reads=[wiB[s]], writes=[wscB[pc]], dma=True)
        else:
            P.op("sp", lambda e: e.dma_start(out=wi[s][:].rearrange("p c f -> p (c f)"), in_=wsc[pc]),
                 reads=[wscB[pc]], writes=[wiB[s]], dma=True)

    def stage_a(g):
        n = len(g)
        N = 128 * n
        for i, t in enumerate(g):
            src = I.ctx[t * 128:(t + 1) * 128, :] if t < 2 else I.x[(t - 2) * 128:(t - 1) * 128, :]
            P.op("sp", lambda e, t=t, src=src: e.dma_start(out=C.x_sb[:, t, :], in_=src), writes=[C.xB[t]], dma=True)
        norm_stats(C, g, ssall, ssBt, rstd, rstdB, [sts[0]["xn"], sts[1]["xn"]], [sts[0]["xnB"], sts[1]["xnB"]], eps, epsB)
        for i, t in enumerate(g):
            who = 1 if t < 2 else 0
            norm_transpose_tile(C, R, C.x_sb[:, t, :], C.xB[t], layer, 0, who, hTg, hTgB, i * 128, sts, 0,
                                rstd_ap=rstd[:, i:i + 1], rstdB=rstdB)

    def stage_bc(g, gi):
        n = len(g)
        N = 128 * n
        rk, wk = st8["rk"], st8["wk"]
        for jj in range(4):
            s = wk % 2
            wk += 1
            load_piece(jj, s, gi)
            for j4 in range(4):
                j = jj * 4 + j4
                pb = ring[rk % 3]
                rk += 1
                for k in range(8):
                    P.op("pe", lambda e, s=s, j4=j4, k=k, pb=pb, N=N: e.matmul(
                        C.ps[pb][:, 0:N], wi[s][:, k, j4 * 128:(j4 + 1) * 128], hTg[:, k, 0:N], start=(k == 0), stop=(k == 7)),
                        reads=[wiB[s], hTgB], writes=[C.psB[pb]])
                P.op("act", lambda e, j=j, pb=pb, N=N: e.activation(out=uT[:, j, 0:N], in_=C.ps[pb][:, 0:N], func=AF.Gelu_apprx_tanh),
                     reads=[C.psB[pb]], writes=[uTB])
        for sv in range(4):
            s = wk % 2
            wk += 1
            load_piece(4 + sv, s, gi)
            for i, t in enumerate(g):
                pb = ring[rk % 3]
                rk += 1
                for k in range(8):
                    P.op("pe", lambda e, s=s, k=k, pb=pb, i=i: e.matmul(
                        C.ps[pb][:, :], hTg[:, k, i * 128:(i + 1) * 128], wi[s][:, k, :], start=(k == 0), stop=(k == 7)),
                        reads=[wiB[s], hTgB], writes=[C.psB[pb]])
                P.op("act", lambda e, sv=sv, pb=pb, i=i: e.activation(out=vraw4[:, i, sv * 512:(sv + 1) * 512], in_=C.ps[pb][:, :],
                                                                      func=AF.Gelu_apprx_tanh),
                     reads=[C.psB[pb]], writes=[vrawB4[i]])
        for i in range(n):
            P.op("dve", lambda e, i=i: e.memset(ssv[:, i, :], 0.0), writes=[ssvBt[i]])
        for i in range(n):
            for hf in range(2):
                jb = sts[(2 * i + hf) % 2]
                P.op("act", lambda e, i=i, hf=hf, jb=jb: e.activation(out=jb["xn"][:, 0:1024], in_=vraw4[:, i, hf * 1024:(hf + 1) * 1024],
                                                                    func=AF.Square, accum_out=ssv[:, i, hf:hf + 1]),
                     reads=[vrawB4[i], ssvBt[i]], writes=[ssvBt[i], jb["xnB"]])
        P.op("dve", lambda e, n=n: e.tensor_tensor(out=rstdv[:, 0:n], in0=ssv[:, 0:n, 0], in1=ssv[:, 0:n, 1], op=ALU.add),
             reads=list(ssvBt[:n]), writes=[rstdvB])
        P.op("act", lambda e, n=n: e.activation(out=rstdv[:, 0:n], in_=rstdv[:, 0:n], func=AF.Sqrt, scale=1.0 / 2048, bias=eps[:, 0:1]),
             reads=[rstdvB, epsB], writes=[rstdvB])
        P.op("dve", lambda e, n=n: e.reciprocal(out=rstdv[:, 0:n], in_=rstdv[:, 0:n]), reads=[rstdvB], writes=[rstdvB])
        st8["rk"], st8["wk"] = rk, wk

    def stage_d(g):
        n = len(g)
        pmk, tk = st8["pmk"], st8["tk"]
        pps = {}

        def spatial(i, t):
            nonlocal pmk, tk
            pp = tk % 2
            tk += 1
            pps[i] = pp
            P.op("dve", lambda e, i=i: e.scalar_tensor_tensor(out=vraw4[:, i, :], in0=vraw4[:, i, :], scalar=rstdv[:, i:i + 1], in1=gv[:],
                                                             op0=ALU.mult, op1=ALU.mult),
                 reads=[vrawB4[i], rstdvB, gvB], writes=[vrawB4[i]])
            for b4 in range(4):
                pm = 4 + (pmk % 2)
                pmk += 1
                brow = bsrow[0:1, 2 * b4:2 * b4 + 2, :].unsqueeze(2).to_broadcast([1, 2, 2, 128])
                P.op("pe", lambda e, pm=pm, brow=brow: e.matmul(C.ps[pm][:, :].rearrange("p (a b n) -> p a b n", a=2, b=2), ones1[0:1, :], brow,
                                                                start=True, stop=False),
                     reads=[bsB], writes=[C.psB[pm]])
                for jq in range(4):
                    j = b4 * 4 + jq
                    P.op("pe", lambda e, pm=pm, jq=jq, j=j, i=i: e.matmul(
                        C.ps[pm][:, jq * 128:(jq + 1) * 128], vraw4[:, i, j * 128:(j + 1) * 128], wsT[:, j // 2, :], start=False, stop=(jq == 3)),
                        reads=[vrawB4[i], wsB], writes=[C.psB[pm]])
                P.op("dve", lambda e, pm=pm, b4=b4, i=i, pp=pp: e.tensor_tensor(
                    out=prodT[pp][:, 4 * b4:4 * b4 + 4, :], in0=C.ps[pm][:, :].rearrange("p (a n) -> p a n", a=4),
                    in1=uT[:, 4 * b4:4 * b4 + 4, i * 128:(i + 1) * 128], op=ALU.mult),
                    reads=[C.psB[pm], uTB], writes=[prodB[pp]])

        def outproj(i, t):
            who = 1 if t < 2 else 0
            pp = pps[i]
            for half in range(2):
                pb = 6 + half
                for j in range(16):
                    P.op("pe", lambda e, pb=pb, j=j, half=half, pp=pp: e.matmul(
                        C.ps[pb][:, :], prodT[pp][:, j, :], w_out[:, j, half * 512:(half + 1) * 512], start=(j == 0), stop=(j == 15)),
                        reads=[prodB[pp], woB], writes=[C.psB[pb]])
                P.op("dve", lambda e, pb=pb, half=half, who=who, pp=pp: e.tensor_tensor(
                    out=tmpf[pp][:, half * 512:(half + 1) * 512], in0=C.ps[pb][:, :], in1=C.gbc[:, who, half * 512:(half + 1) * 512], op=ALU.mult),
                    reads=[C.psB[pb], C.gbcB[who]], writes=[tmpfB[pp]])
            P.op("dve", lambda e, t=t, pp=pp: e.tensor_tensor(out=C.x_sb[:, t, :], in0=C.x_sb[:, t, :], in1=tmpf[pp][:], op=ALU.add),
                 reads=[tmpfB[pp], C.xB[t]], writes=[C.xB[t]])

        spatial(0, g[0])
        for i in range(n):
            if i + 1 < n:
                spatial(i + 1, g[i + 1])
            outproj(i, g[i])
        st8["pmk"], st8["tk"] = pmk, tk

    stage_a(groups[0])
    for gi, g in enumerate(groups):
        stage_bc(g, gi)
        if gi + 1 < len(groups):
            stage_a(groups[gi + 1])
        stage_d(g)


def _const_tables():
    t = np.arange(NX)
    row = (t // 64).astype(np.float32)
    col = (t % 64).astype(np.float32)
    inv = (10000.0 ** (-np.arange(16, dtype=np.float32) / 16)).astype(np.float32)
    cosT = np.zeros((128, NX), np.float32)
    sinT = np.zeros((128, NX), np.float32)
    perm = np.zeros((128, 128), np.float32)
    for p in range(128):
        d = p % 64
        axis = d // 32
        half = (d % 32) // 16
        f = d % 16
        ang = (row if axis == 0 else col) * inv[f]
        cosT[p] = np.cos(ang)
        sinT[p] = -np.sin(ang) if half == 0 else np.sin(ang)
        partner = p + 16 if half == 0 else p - 16
        perm[partner, p] = 1.0
    qi = np.arange(128)[:, None]
    kj = np.arange(128)[None, :]
    NEG = -30000.0
    mask = np.zeros((128, 384), np.float32)
    mask[:, 0:128] = np.where(qi >= kj, 0.0, NEG)
    mask[:, 128:256] = np.where(qi <= kj, 0.0, NEG)
    return np.stack([cosT, sinT]), perm, mask


def _wqk_layout(w):
    chunks = [w[:, c * 128:(c + 1) * 128] for c in range(8)]
    for hk in range(4):
        kh = w[:, 1024 + hk * 64:1024 + (hk + 1) * 64]
        chunks.append(np.concatenate([kh, kh], axis=1))
    return np.stack(chunks, axis=0)


def _prep(inputs):
    f = lambda a: np.ascontiguousarray(np.asarray(a, dtype=np.float32))
    g = {k: np.asarray(v) for k, v in inputs.items()}
    rope, perm, mask = _const_tables()
    ncol = np.stack([g["norm1_g"], g["norm2_g"]], axis=0)
    ncol = ncol.reshape(2, 2, 8, 128).transpose(3, 1, 0, 2)
    common = {
        "ada_w": f(g["ada_w"].reshape(2, D, 12, 512).transpose(0, 2, 1, 3)),
        "adab2": f(np.repeat(g["ada_b"][:, None, :], 2, axis=1)),
        "ncol": f(ncol),
        "a_w_in": f(g["a_w_in"][0].reshape(D, 8, 512).transpose(1, 0, 2)),
        "a_w_out": f(g["a_w_out"][0]),
        "gv_bc": f(np.broadcast_to(g["a_g_v"][0][None, :], (128, 2048))),
        "wsT": f(g["a_w_s"][0].transpose(2, 0, 1)),
        "bs_bc": f(np.broadcast_to(g["a_b_s"][0][None, :, :], (128, 8, 128))),
        "wqk": f(_wqk_layout(g["b_w_qkv"][0])),
        "wvv": f(g["b_w_qkv"][0][:, 1280:1536]),
        "b_w_o": f(g["b_w_o"][0]),
        "sink_bc": f(np.broadcast_to(g["b_sink"][0][None, :], (128, 16))),
        "wr": f(g["router_w"].reshape(8, 128, 16).transpose(1, 0, 2)),
        "rb_bc": f(np.broadcast_to(g["router_b"][None, :], (128, 16))),
        "moe_w_gate": f(g["moe_w_gate"]),
        "moe_w_up": f(g["moe_w_up"]),
        "moe_w_down": f(g["moe_w_down"]),
        "fg_bc": f(np.broadcast_to(g["final_g"][None, :], (128, D))),
        "rope": f(rope), "perm": f(perm), "mask": f(mask),
    }
    maps = []
    for b in range(8):
        ccol = np.stack([g["c"][b].reshape(8, 128).T, g["c_ctx"].reshape(8, 128).T], axis=-1)
        m = dict(common)
        m["x"] = f(g["x"][b])
        m["ctx"] = f(g["ctx"][b])
        m["ccol"] = f(ccol)
        maps.append(m)
    return maps


def kernel(**inputs):
    maps = _prep(inputs)
    nc = build_program()
    res = run_bass_kernel_spmd(nc, maps, core_ids=list(range(8)))
    out = np.stack([np.asarray(r["out"]) for r in res.results], axis=0)
    return out.astype(np.float32)


def phase_moe(C, layer):
    nc, P, I = C.nc, C.P, C.I
    tiles = list(range(NT0)) if layer == 0 else list(range(2, NT0))
    P.barrier()
    R = Region(nc, TMP_OFF, SB_END, "pc%d" % layer)
    Wg = [R.t("wg%d" % i, [128, 8, DE], BF16) for i in range(2)]
    Wu = [R.t("wu%d" % i, [128, 8, DE], BF16) for i in range(2)]
    Wd = [R.t("wd%d" % i, [128, 4, D], BF16) for i in range(2)]
    WgB, WuB, WdB = P.bufs("wg", 2), P.bufs("wu", 2), P.bufs("wd", 2)
    A = R.t("A", [128, 4, 2304], BF16)
    sg = [R.t("sg%d" % i, [128, 512], BF16) for i in range(2)]
    sgB = P.bufs("sg", 2)
    tmpf = [R.t("tmpf%d" % i, [128, D], F32) for i in range(2)]
    tmpfB = P.bufs("tmpf", 2)
    wr = R.t("wr", [128, 8, NE], BF16)
    wrB = P.buf("wr")
    ov_off = R.cur
    sts = []
    for i in range(2):
        st = {}
        st["xn"] = R.t("xn%d" % i, [128, D], BF16)
        st["xnB"] = P.buf("xn%d" % i)
        st["junk"], st["junkB"] = st["xn"], st["xnB"]
        st["ss"] = R.t("ss%d" % i, [128, 2], F32)
        st["ssB"] = P.buf("ss%d" % i)
        if i == 0:
            st["eps"] = R.t("eps", [128, 1], F32)
            st["epsB"] = P.buf("eps")
        else:
            st["eps"], st["epsB"] = sts[0]["eps"], sts[0]["epsB"]
        sts.append(st)
    st = sts[0]
    rtB = P.buf("rt")
    hTB = P.bufs("h2T", NT0)
    C.hTB = hTB

    P.op("dve", lambda e: e.memset(st["eps"][:], EPS), writes=[st["epsB"]])
    P.op("pool", lambda e: e.dma_start(out=wr[:], in_=I.wr), writes=[wrB], dma=True)
    load_gate_bc(C, layer, 1, 0, 0)
    if layer == 0:
        load_gate_bc(C, layer, 1, 1, 1)

    def load_expert(e):
        s = e % 2
        P.op("pool", lambda en: en.dma_start(out=Wg[s][:], in_=I.w_gate[layer, e].rearrange("(c p) f -> p c f", p=128)),
             writes=[WgB[s]], dma=True)
        P.op("pool", lambda en: en.dma_start(out=Wu[s][:], in_=I.w_up[layer, e].rearrange("(c p) f -> p c f", p=128)),
             writes=[WuB[s]], dma=True)
        P.op("pool", lambda en: en.dma_start(out=Wd[s][:], in_=I.w_down[layer, e].rearrange("(c p) f -> p c f", p=128)),
             writes=[WdB[s]], dma=True)

    load_expert(0)
    load_expert(1)

    T = len(tiles)
    t0 = tiles[0]
    ssall = R.t("ssall", [128, NT0], F32)
    ssBt = P.bufs("ssall", NT0)
    rstd = R.t("rstd", [128, NT0], F32)
    rstdB = P.buf("rstd")
    norm_stats(C, tiles, ssall, ssBt, rstd, rstdB, [sts[0]["xn"], sts[1]["xn"]], [sts[0]["xnB"], sts[1]["xnB"]], st["eps"], st["epsB"])
    RB = 5
    for i, t in enumerate(tiles):
        who = 1 if t < 2 else 0
        norm_transpose_tile(C, R, C.x_sb[:, t, :], C.xB[t], layer, 1, who, C.hT, hTB[t], t * 128, sts, (6, 7),
                            rstd_ap=rstd[:, i:i + 1], rstdB=rstdB)
        for k in range(8):
            P.op("pe", lambda e, k=k, t=t, i=i: e.matmul(C.ps[RB][:, i * NE:(i + 1) * NE], C.hT[:, k, t * 128:(t + 1) * 128], wr[:, k, :],
                                                         start=(k == 0), stop=(k == 7)),
                 reads=[hTB[t], wrB], writes=[C.psB[RB]])
    s_all = R.t("s_all", [128, NT0, NE], F32)
    sbb = R.t("sbb", [128, NT0, NE], F32)
    t6 = R.t("t6", [128, NT0 * 4, 6], F32)
    gsc = R.t("gsc", [128, NT0 * 4], F32)
    gmx = R.t("gmx", [128, NT0], F32)
    pen = R.t("pen", [128, NT0 * 4], F32)
    msk = R.t("msk", [128, NT0, NE], F32)
    eq = R.t("eq", [128, NT0, NE], F32)
    m2 = R.t("m2", [128, NT0], F32)
    P.op("act", lambda e: e.activation(out=s_all[:, 0:T, :], in_=C.ps[RB][:, 0:T * NE].rearrange("p (t e) -> p t e", e=NE), func=AF.Sigmoid),
         reads=[C.psB[RB]], writes=[rtB])
    P.op("dve", lambda e: e.tensor_tensor(out=sbb[:, 0:T, :], in0=s_all[:, 0:T, :], in1=C.rb[:].unsqueeze(1).to_broadcast([128, T, NE]), op=ALU.add),
         reads=[rtB, C.rbB], writes=[rtB])
    sb4 = sbb[:, 0:T, :].rearrange("p t (g i) -> p (t g) i", i=4)
    G4 = T * 4
    P.op("dve", lambda e: e.tensor_tensor(out=t6[:, 0:G4, 0:3], in0=sb4[:, :, 0:3], in1=sb4[:, :, 1:4], op=ALU.add), reads=[rtB], writes=[rtB])
    P.op("dve", lambda e: e.tensor_tensor(out=t6[:, 0:G4, 3:5], in0=sb4[:, :, 0:2], in1=sb4[:, :, 2:4], op=ALU.add), reads=[rtB], writes=[rtB])
    P.op("dve", lambda e: e.tensor_tensor(out=t6[:, 0:G4, 5:6], in0=sb4[:, :, 0:1], in1=sb4[:, :, 3:4], op=ALU.add), reads=[rtB], writes=[rtB])
    P.op("dve", lambda e: e.tensor_reduce(out=gsc[:, 0:G4], in_=t6[:, 0:G4, :], axis=AX.X, op=ALU.max), reads=[rtB], writes=[rtB])
    P.op("dve", lambda e: e.tensor_reduce(out=gmx[:, 0:T], in_=gsc[:, 0:G4].rearrange("p (t g) -> p t g", g=4), axis=AX.X, op=ALU.max),
         reads=[rtB], writes=[rtB])
    P.op("dve", lambda e: e.tensor_tensor(out=pen[:, 0:G4].rearrange("p (t g) -> p t g", g=4), in0=gsc[:, 0:G4].rearrange("p (t g) -> p t g", g=4),
                                          in1=gmx[:, 0:T].unsqueeze(2).to_broadcast([128, T, 4]), op=ALU.is_ge), reads=[rtB], writes=[rtB])
    P.op("dve", lambda e: e.tensor_scalar(out=pen[:, 0:G4], in0=pen[:, 0:G4], scalar1=100.0, scalar2=-100.0, op0=ALU.mult, op1=ALU.add),
         reads=[rtB], writes=[rtB])
    msk4 = msk[:, 0:T, :].rearrange("p t (g i) -> p (t g) i", i=4)
    P.op("dve", lambda e: e.tensor_tensor(out=msk4, in0=sb4, in1=pen[:, 0:G4].unsqueeze(2).to_broadcast([128, G4, 4]), op=ALU.add),
         reads=[rtB], writes=[rtB])
    P.op("dve", lambda e: e.tensor_reduce(out=gmx[:, 0:T], in_=msk[:, 0:T, :], axis=AX.X, op=ALU.max), reads=[rtB], writes=[rtB])
    P.op("dve", lambda e: e.tensor_tensor(out=eq[:, 0:T, :], in0=msk[:, 0:T, :], in1=gmx[:, 0:T].unsqueeze(2).to_broadcast([128, T, NE]), op=ALU.is_ge),
         reads=[rtB], writes=[rtB])
    P.op("dve", lambda e: e.scalar_tensor_tensor(out=eq[:, 0:T, :], in0=eq[:, 0:T, :], scalar=-200.0, in1=msk[:, 0:T, :], op0=ALU.mult, op1=ALU.add),
         reads=[rtB], writes=[rtB])
    P.op("dve", lambda e: e.tensor_reduce(out=m2[:, 0:T], in_=eq[:, 0:T, :], axis=AX.X, op=ALU.max), reads=[rtB], writes=[rtB])
    P.op("dve", lambda e: e.tensor_tensor(out=eq[:, 0:T, :], in0=msk[:, 0:T, :], in1=m2[:, 0:T].unsqueeze(2).to_broadcast([128, T, NE]), op=ALU.is_ge),
         reads=[rtB], writes=[rtB])
    P.op("dve", lambda e: e.tensor_tensor(out=eq[:, 0:T, :], in0=eq[:, 0:T, :], in1=s_all[:, 0:T, :], op=ALU.mult), reads=[rtB], writes=[rtB])
    P.op("dve", lambda e: e.tensor_reduce(out=m2[:, 0:T], in_=eq[:, 0:T, :], axis=AX.X, op=ALU.add), reads=[rtB], writes=[rtB])
    P.op("dve", lambda e: e.reciprocal(out=m2[:, 0:T], in_=m2[:, 0:T]), reads=[rtB], writes=[rtB])
    P.op("dve", lambda e: e.tensor_tensor(out=C.gatesv[:, t0:t0 + T, :], in0=eq[:, 0:T, :], in1=m2[:, 0:T].unsqueeze(2).to_broadcast([128, T, NE]), op=ALU.mult),
         reads=[rtB], writes=[C.gatesB[t] for t in tiles])

    npieces = 0
    if layer == 0:
        P.barrier()
        RO = Region(nc, ov_off, SB_END, "pcov")
        wa2 = [RO.t("wa2_%d" % i, [128, 8, 256], BF16) for i in range(2)]
        adabp = [RO.t("adabp%d" % i, [2, 256], F32) for i in range(2)]
        mp0 = RO.t("mp0", [2, 256], F32)
        mp = [mp0, mp0]
        mpB0 = P.buf("mp")
        wa2B, adabpB, mpB = P.bufs("wa2", 2), P.bufs("adabp", 2), [mpB0, mpB0]
        npieces = 24
        for q in range(2):
            adaln_piece_load(C, q, wa2[q], wa2B[q], adabp[q], adabpB[q])
    blocks = [tiles[i:i + 4] for i in range(0, len(tiles), 4)]
    AB = P.bufs("A", len(blocks))
    fk = 0
    dk = 0

    def gu_block(e, bi):
        nonlocal fk
        s = e % 2
        blk = blocks[bi]
        N = 128 * len(blk)
        tok0 = blk[0] * 128
        hb = [hTB[t] for t in blk]
        for f in range(4):
            q = fk % 2
            fk += 1
            pg, pu = 2 * q, 2 * q + 1
            for k in range(8):
                P.op("pe", lambda en, k=k, f=f, pg=pg: en.matmul(C.ps[pg][:, 0:N], Wg[s][:, k, f * 128:(f + 1) * 128],
                                                                 C.hT[:, k, tok0:tok0 + N], start=(k == 0), stop=(k == 7)),
                     reads=[WgB[s]] + hb, writes=[C.psB[pg]])
            for k in range(8):
                P.op("pe", lambda en, k=k, f=f, pu=pu: en.matmul(C.ps[pu][:, 0:N], Wu[s][:, k, f * 128:(f + 1) * 128],
                                                                 C.hT[:, k, tok0:tok0 + N], start=(k == 0), stop=(k == 7)),
                     reads=[WuB[s]] + hb, writes=[C.psB[pu]])
            P.op("act", lambda en, q=q, pg=pg: en.activation(out=sg[q][:, 0:N], in_=C.ps[pg][:, 0:N], func=AF.Silu),
                 reads=[C.psB[pg]], writes=[sgB[q]])
            P.op("dve", lambda en, f=f, q=q, pu=pu: en.tensor_tensor(out=A[:, f, tok0:tok0 + N], in0=C.ps[pu][:, 0:N], in1=sg[q][:, 0:N], op=ALU.mult),
                 reads=[C.psB[pu], sgB[q]], writes=[AB[bi]])

    def down_block(e, bi):
        nonlocal dk
        s = e % 2
        for t in blocks[bi]:
            who = 1 if t < 2 else 0
            q = dk % 2
            dk += 1
            for half in range(2):
                pb = 4 + 2 * q + half
                for f in range(4):
                    P.op("pe", lambda en, f=f, half=half, pb=pb, t=t: en.matmul(
                        C.ps[pb][:, :], A[:, f, t * 128:(t + 1) * 128], Wd[s][:, f, half * 512:(half + 1) * 512],
                        start=(f == 0), stop=(f == 3)),
                        reads=[AB[bi], WdB[s]], writes=[C.psB[pb]])
                P.op("dve", lambda en, half=half, pb=pb, t=t, who=who, q=q: en.scalar_tensor_tensor(
                    out=tmpf[q][:, half * 512:(half + 1) * 512], in0=C.ps[pb][:, :], scalar=C.gatesv[:, t, e:e + 1],
                    in1=C.gbc[:, who, half * 512:(half + 1) * 512], op0=ALU.mult, op1=ALU.mult),
                    reads=[C.psB[pb], C.gatesB[t], C.gbcB[who]], writes=[tmpfB[q]])
            P.op("pool", lambda en, t=t, q=q: en.tensor_tensor(out=C.x_sb[:, t, :], in0=C.x_sb[:, t, :], in1=tmpf[q][:], op=ALU.add),
                 reads=[tmpfB[q], C.xB[t]], writes=[C.xB[t]])

    nb = len(blocks)
    for e in range(NE):
        for bi in range(nb):
            gu_block(e, bi)
            if bi >= 1:
                down_block(e, bi - 1)
        down_block(e, nb - 1)
        if e + 2 < NE:
            load_expert(e + 2)
        for q in range(e * npieces // NE, (e + 1) * npieces // NE):
            j = q % 2
            adaln_piece(C, q, wa2[j], wa2B[j], adabp[j], adabpB[j], mp[j], mpB[j])
            if q + 2 < npieces:
                adaln_piece_load(C, q + 2, wa2[j], wa2B[j], adabp[j], adabpB[j])


def phase_final(C):
    nc, P, I = C.nc, C.P, C.I
    P.barrier()
    R = Region(nc, TMP_OFF, SB_END, "pf")
    fg = R.t("fg", [128, D], F32)
    fgB = P.buf("fg")
    junk = R.t("junk", [128, D], BF16)
    junkB = P.buf("junk")
    eps = R.t("eps", [128, 1], F32)
    epsB = P.buf("eps")
    ss = [R.t("ss%d" % i, [128, 2], F32) for i in range(2)]
    ssB = P.bufs("ss", 2)
    ob = [R.t("ob%d" % i, [128, D], F32) for i in range(2)]
    obB = P.bufs("ob", 2)
    P.op("dve", lambda e: e.memset(eps[:], EPS), writes=[epsB])
    P.op("sp", lambda e: e.dma_start(out=fg[:], in_=I.fg_bc), writes=[fgB], dma=True)
    for i, t in enumerate(range(2, NT0)):
        q = i % 2
        P.op("dve", lambda e, q=q: e.memset(ss[q][:], 0.0), writes=[ssB[q]])
        P.op("act", lambda e, t=t, q=q: e.activation(out=junk[:], in_=C.x_sb[:, t, :], func=AF.Square, accum_out=ss[q][:, 0:1]),
             reads=[C.xB[t], ssB[q]], writes=[ssB[q], junkB])
        P.op("act", lambda e, q=q: e.activation(out=ss[q][:, 0:1], in_=ss[q][:, 0:1], func=AF.Sqrt, scale=1.0 / D, bias=eps[:, 0:1]),
             reads=[ssB[q], epsB], writes=[ssB[q]])
        P.op("dve", lambda e, q=q: e.reciprocal(out=ss[q][:, 0:1], in_=ss[q][:, 0:1]), reads=[ssB[q]], writes=[ssB[q]])
        P.op("dve", lambda e, t=t, q=q: e.scalar_tensor_tensor(out=ob[q][:], in0=C.x_sb[:, t, :], scalar=ss[q][:, 0:1], in1=fg[:],
                                                              op0=ALU.mult, op1=ALU.mult),
             reads=[C.xB[t], ssB[q], fgB], writes=[obB[q]])
        P.op("sp", lambda e, t=t, q=q: e.dma_start(out=C.out[(t - 2) * 128:(t - 1) * 128, :], in_=ob[q][:]),
             reads=[obB[q]], dma=True)


def phase_attn(C):
    nc, P, I = C.nc, C.P, C.I
    layer = 1
    C.attn_done = True
    P.barrier()
    R = Region(nc, TMP_OFF, SB_END, "pd")
    qT = R.t("qT", [128, 8, NX], BF16)
    qTB = P.bufs("qT", 4)
    kT = R.t("kT", [128, 4, 2304], BF16)
    kTB = P.bufs("kT", 5)
    v_sb = R.t("v", [128, NT0, 4, 65], BF16)
    vB = P.bufs("v", NT0)
    cos_off = R.cur
    cosT = R.t("cosT", [128, NX], F32)
    sinT = R.t("sinT", [128, NX], F32)
    ropeB = P.buf("rope")
    sub_off = R.cur
    wv = R.t("wv", [128, 8, 256], BF16)
    wvB = P.buf("wv")
    sts = []
    for i in range(2):
        st = {}
        st["xn"] = R.t("xn%d" % i, [128, D], BF16)
        st["xnB"] = P.buf("xn%d" % i)
        st["junk"], st["junkB"] = st["xn"], st["xnB"]
        st["ss"] = R.t("ss%d" % i, [128, 2], F32)
        st["ssB"] = P.buf("ss%d" % i)
        if i == 0:
            st["eps"] = R.t("eps", [128, 1], F32)
            st["epsB"] = P.buf("eps")
        else:
            st["eps"], st["epsB"] = sts[0]["eps"], sts[0]["epsB"]
        sts.append(st)
    st = sts[0]
    hTB = P.bufs("h1T", NT0)

    P.op("dve", lambda e: e.memset(st["eps"][:], EPS), writes=[st["epsB"]])
    P.op("pool", lambda e: e.dma_start(out=wv[:], in_=I.wvv.rearrange("(c p) f -> p c f", p=128)),
         writes=[wvB], dma=True)
    P.op("sp", lambda e: e.dma_start(out=cosT[:], in_=I.rope[0]), writes=[ropeB], dma=True)
    P.op("sp", lambda e: e.dma_start(out=sinT[:], in_=I.rope[1]), writes=[ropeB], dma=True)
    adaln_cols(C, 1)
    load_gate_bc(C, layer, 0, 0, 0)
    for t in range(NT0):
        P.op("pool", lambda e, t=t: e.memset(v_sb[:, t, :, :], 1.0), writes=[vB[t]])

    ssall = R.t("ssall", [128, NT0], F32)
    ssBt = P.bufs("ssall", NT0)
    rstd = R.t("rstd", [128, NT0], F32)
    rstdB = P.buf("rstd")
    norm_stats(C, list(range(NT0)), ssall, ssBt, rstd, rstdB, [sts[0]["xn"], sts[1]["xn"]], [sts[0]["xnB"], sts[1]["xnB"]],
               st["eps"], st["epsB"])
    for t in range(NT0):
        who = 1 if t < 2 else 0
        norm_transpose_tile(C, R, C.x_sb[:, t, :], C.xB[t], layer, 0, who, C.hT, hTB[t], t * 128, sts, (6, 7),
                            rstd_ap=rstd[:, t:t + 1], rstdB=rstdB)
    for t in range(NT0):
        pb = 4 + (t % 2)
        for k in range(8):
            P.op("pe", lambda e, k=k, t=t, pb=pb: e.matmul(C.ps[pb][:, 0:256], C.hT[:, k, t * 128:(t + 1) * 128], wv[:, k, :],
                                                           start=(k == 0), stop=(k == 7)),
                 reads=[hTB[t], wvB], writes=[C.psB[pb]])
        P.op("act", lambda e, t=t, pb=pb: e.activation(out=v_sb[:, t, :, 0:64],
                                                       in_=C.ps[pb][:, 0:256].rearrange("p (h d) -> p h d", d=64), func=AF.Copy),
             reads=[C.psB[pb]], writes=[vB[t]])

    import os
    astop = int(os.environ.get("ATTN_STOP", "9"))
    if astop <= 1:
        return
    P.barrier()
    R2d = Region(nc, sub_off, SB_END, "pd2")
    wq = [R2d.t("wq%d" % i, [128, 8, 128], BF16) for i in range(3)]
    wqB = P.bufs("wq", 3)
    perm = R2d.t("perm", [128, 128], BF16)
    permB = P.buf("perm")
    t1 = R2d.t("t1", [128, 512], F32)
    t2 = R2d.t("t2", [128, 512], F32)
    raw = [R2d.t("raw%d" % i, [128, 512], BF16) for i in range(2)]
    t1B, t2B = P.buf("t1"), P.buf("t2")
    rawB = P.bufs("raw", 2)
    P.op("pool", lambda e: e.dma_start(out=perm[:], in_=I.perm), writes=[permB], dma=True)

    uk = 0
    rk = 0
    for ch in range(12):
        s = ch % 3
        P.op("pool", lambda e, ch=ch, s=s: e.dma_start(out=wq[s][:], in_=I.wqk[ch].rearrange("(c p) f -> p c f", p=128)),
             writes=[wqB[s]], dma=True)
        hk = ch - 8
        if ch >= 8:
            pb = rk % 3
            rk += 1
            for k in range(8):
                P.op("pe", lambda e, k=k, s=s, pb=pb: e.matmul(C.ps[pb][:, 0:256], wq[s][:, k, :], C.hT[:, k, 0:256],
                                                             start=(k == 0), stop=(k == 7)),
                     reads=[wqB[s], hTB[0], hTB[1]], writes=[C.psB[pb]])
            P.op("act", lambda e, hk=hk, pb=pb: e.activation(out=kT[:, hk, 0:256], in_=C.ps[pb][:, 0:256], func=AF.Copy),
                 reads=[C.psB[pb]], writes=[kTB[0]])
        for tg in range(4):
            pb = rk % 3
            rk += 1
            c0 = 256 + tg * 512
            hb = [hTB[2 + tg * 4 + j] for j in range(4)]
            for k in range(8):
                P.op("pe", lambda e, k=k, s=s, pb=pb, c0=c0: e.matmul(C.ps[pb][:, :], wq[s][:, k, :], C.hT[:, k, c0:c0 + 512],
                                                                    start=(k == 0), stop=(k == 7)),
                     reads=[wqB[s]] + hb, writes=[C.psB[pb]])
            q = uk % 2
            uk += 1
            if ch < 8:
                dst, dB = qT[:, ch, tg * 512:(tg + 1) * 512], qTB[tg]
            else:
                dst, dB = kT[:, ch - 8, c0:c0 + 512], kTB[1 + tg]
            if os.environ.get("ATTN_NOROPE"):
                P.op("act", lambda e, dst=dst, pb=pb: e.activation(out=dst, in_=C.ps[pb][:, :], func=AF.Copy),
                     reads=[C.psB[pb]], writes=[dB])
                continue
            P.op("act", lambda e, q=q, pb=pb: e.activation(out=raw[q][:], in_=C.ps[pb][:, :], func=AF.Copy),
                 reads=[C.psB[pb]], writes=[rawB[q]])
            pb2 = 3 + q
            P.op("pe", lambda e, q=q, pb2=pb2: e.matmul(C.ps[pb2][:, :], perm[:], raw[q][:], start=True, stop=True),
                 reads=[rawB[q], permB], writes=[C.psB[pb2]])
            P.op("dve", lambda e, pb=pb, tg=tg: e.tensor_tensor(out=t1[:], in0=C.ps[pb][:, :], in1=cosT[:, tg * 512:(tg + 1) * 512], op=ALU.mult),
                 reads=[C.psB[pb], ropeB], writes=[t1B])
            P.op("dve", lambda e, pb2=pb2, tg=tg: e.tensor_tensor(out=t2[:], in0=C.ps[pb2][:, :], in1=sinT[:, tg * 512:(tg + 1) * 512], op=ALU.mult),
                 reads=[C.psB[pb2], ropeB], writes=[t2B])
            if ch < 8:
                dst, dB = qT[:, ch, tg * 512:(tg + 1) * 512], qTB[tg]
            else:
                dst, dB = kT[:, ch - 8, c0:c0 + 512], kTB[1 + tg]
            P.op("dve", lambda e, dst=dst: e.tensor_tensor(out=dst, in0=t1[:], in1=t2[:], op=ALU.add),
                 reads=[t1B, t2B], writes=[dB])

    if astop <= 2:
        return
    P.barrier()
    R2 = Region(nc, HT_OFF, GBC_OFF, "pd3")
    w_o = R2.t("wo", [128, 8, D], BF16)
    woB = P.buf("wo")
    PT = [R2.t("PT%d" % i, [128, 5, 512], BF16) for i in range(2)]
    PTB = [P.bufs("PT%d_" % i, 5) for i in range(2)]
    o_tok0 = R2.t("otok", [128, D], BF16)
    oT0 = R2.t("oT", [128, 8, 128], BF16)
    tmpf = R2.t("tmpf", [128, D], F32)
    tmpfB = P.buf("tmpf")
    maskT = R2.t("maskT", [128, 2, 128], F32)
    maskB = P.buf("maskT")
    esink = R2.t("esink", [128, NE], F32)
    esB = P.buf("esink")
    rsum = R2.t("rsum", [128, 4], F32)
    rsB = P.buf("rsum")
    for h in range(2):
        P.op("pool", lambda e, h=h: e.dma_start(out=w_o[:, h * 4:(h + 1) * 4, :],
                                               in_=I.b_w_o[h * 512:(h + 1) * 512, :].rearrange("(c p) f -> p c f", p=128)),
             writes=[woB], dma=True)
    P.op("sp", lambda e: e.dma_start(out=maskT[:], in_=I.mask.rearrange("p (a b) -> p a b", a=3)[:, 0:2, :]), writes=[maskB], dma=True)
    P.op("sp", lambda e: e.dma_start(out=esink[:], in_=I.sink_bc), writes=[esB], dma=True)
    P.op("act", lambda e: e.activation(out=esink[:], in_=esink[:], func=AF.Exp), reads=[esB], writes=[esB])
    R3 = Region(nc, cos_off, cos_off + 16384, "pd3b")
    qz = [R3.t("qz%d" % i, [128, 4, 128], BF16) for i in range(2)]
    qzB = P.bufs("qz", 2)
    mask4 = R3.t("mask4", [128, 2, 4, 128], BF16)
    mask4B = P.buf("mask4")
    o_toks = [o_tok0, R3.t("otok1", [128, D], BF16)]
    otBs = P.bufs("otok", 2)
    oTs = [oT0, R3.t("oT1", [128, 8, 128], BF16)]
    oTBs = P.bufs("oT", 2)
    for j in range(2):
        P.op("pool", lambda e, j=j: e.memset(qz[j][:], 0.0), writes=[qzB[j]])
    P.op("dve", lambda e: e.tensor_copy(out=mask4[:], in_=maskT[:].unsqueeze(2).to_broadcast([128, 2, 4, 128])),
         reads=[maskB], writes=[mask4B])
    gk = 0

    def attn_tail(i):
        t = 2 + i
        o_tok, otB, oT, oTB = o_toks[i % 2], otBs[i % 2], oTs[i % 2], oTBs[i % 2]
        pt = ps_bf16(C, 6)
        for k in range(8):
            P.op("pe", lambda e, k=k: e.transpose(out=pt[:, k * 128:(k + 1) * 128], in_=o_tok[:, k * 128:(k + 1) * 128], identity=C.ident_b[:]),
                 reads=[otB, C.identB], writes=[C.psB[6]])
        P.op("act", lambda e: e.activation(out=oT[:].rearrange("p k t -> p (k t)"), in_=pt[:, :], func=AF.Copy),
             reads=[C.psB[6]], writes=[oTB])
        for half in range(2):
            pb = 6 + half
            for k in range(8):
                P.op("pe", lambda e, k=k, half=half, pb=pb: e.matmul(C.ps[pb][:, :], oT[:, k, :], w_o[:, k, half * 512:(half + 1) * 512],
                                                                   start=(k == 0), stop=(k == 7)),
                     reads=[oTB, woB], writes=[C.psB[pb]])
            P.op("dve", lambda e, half=half, pb=pb: e.tensor_tensor(out=tmpf[:, half * 512:(half + 1) * 512], in0=C.ps[pb][:, :],
                                                                    in1=C.gbc[:, 0, half * 512:(half + 1) * 512], op=ALU.mult),
                 reads=[C.psB[pb], C.gbcB[0]], writes=[tmpfB])
        P.op("pool", lambda e, t=t: e.tensor_tensor(out=C.x_sb[:, t, :], in0=C.x_sb[:, t, :], in1=tmpf[:], op=ALU.add),
             reads=[tmpfB, C.xB[t]], writes=[C.xB[t]])

    for i in range(16):
        t = 2 + i
        o_tok, otB = o_toks[i % 2], otBs[i % 2]
        blocks = [(0, 0, None), (128, 1, None)]
        for d in (-1, 0, 1):
            j = i + d
            if j < 0 or j > 15:
                continue
            blocks.append((256 + j * 128, 2 + j, {-1: 0, 0: None, 1: 1}[d]))
        tgq = i // 4
        for hk in range(4):
            pq = gk % 2
            gk += 1
            P.op("pool", lambda e, pq=pq, hk=hk, i=i: e.tensor_copy(out=qz[pq][0:64, 0:2, :], in_=qT[0:64, 2 * hk:2 * hk + 2, i * 128:(i + 1) * 128]),
                 reads=[qTB[tgq]], writes=[qzB[pq]])
            P.op("pool", lambda e, pq=pq, hk=hk, i=i: e.tensor_copy(out=qz[pq][64:128, 2:4, :], in_=qT[64:128, 2 * hk:2 * hk + 2, i * 128:(i + 1) * 128]),
                 reads=[qTB[tgq]], writes=[qzB[pq]])
            for bi, (kc, vt, mi) in enumerate(blocks):
                kb = kTB[0] if bi < 2 else kTB[1 + (vt - 2) // 4]
                P.op("pe", lambda e, bi=bi, kc=kc, hk=hk, pq=pq, mi=mi: e.matmul(
                    C.ps[bi][:, :], kT[:, hk, kc:kc + 128], qz[pq][:].rearrange("p a b -> p (a b)"),
                    start=True, stop=(mi is None)),
                    reads=[kb, qzB[pq]], writes=[C.psB[bi]])
                if mi is not None:
                    P.op("pe", lambda e, bi=bi, mi=mi: e.matmul(
                        C.ps[bi][:, :], C.ident_b[:], mask4[:, mi, :, :].rearrange("p a b -> p (a b)"), start=False, stop=True),
                        reads=[C.identB, mask4B], writes=[C.psB[bi]])
                P.op("act", lambda e, bi=bi, pq=pq: e.activation(out=PT[pq][:, bi, :], in_=C.ps[bi][:, :], func=AF.Exp, scale=0.125),
                     reads=[C.psB[bi]], writes=[PTB[pq][bi]])
            nb = len(blocks)
            for slot in range(4):
                for bi, (kc, vt, mi) in enumerate(blocks):
                    P.op("pe", lambda e, slot=slot, bi=bi, vt=vt, pq=pq, hk=hk, nb=nb: e.matmul(
                        C.ps[5][:, slot * 65:(slot + 1) * 65], PT[pq][:, bi, slot * 128:(slot + 1) * 128], v_sb[:, vt, hk, :],
                        start=(bi == 0), stop=(bi == nb - 1)),
                        reads=[PTB[pq][bi], vB[vt]], writes=[C.psB[5]])
            O = C.ps[5][:, 0:260].rearrange("p (b c d) -> p b c d", b=2, c=2)
            es = esink[:, hk * 4:(hk + 1) * 4].rearrange("p (c b) -> p b c", b=2)
            P.op("dve", lambda e, O=O, es=es: e.tensor_tensor(out=rsum[:].rearrange("p (b c) -> p b c", b=2), in0=O[:, :, :, 64], in1=es, op=ALU.add),
                 reads=[C.psB[5], esB], writes=[rsB])
            P.op("dve", lambda e: e.reciprocal(out=rsum[:], in_=rsum[:]), reads=[rsB], writes=[rsB])
            od = o_tok[:, hk * 256:(hk + 1) * 256].rearrange("p (c b d) -> p b c d", c=2, b=2)
            P.op("dve", lambda e, O=O, od=od: e.tensor_tensor(
                out=od, in0=O[:, :, :, 0:64], in1=rsum[:].rearrange("p (b c) -> p b c", b=2).unsqueeze(3).to_broadcast([128, 2, 2, 64]),
                op=ALU.mult),
                reads=[C.psB[5], rsB], writes=[otB])
            if hk == 0 and i >= 1:
                attn_tail(i - 1)
    attn_tail(15)
```

```python
import numpy as np
import concourse.bass as bass
import concourse.mybir as mybir
from concourse.bass_utils import run_bass_kernel_spmd

F32 = mybir.dt.float32
BF16 = mybir.dt.bfloat16
AF = mybir.ActivationFunctionType
ALU = mybir.AluOpType
AX = mybir.AxisListType


class Buf:
    __slots__ = ("name", "writers", "readers", "excl")

    def __init__(self, name):
        self.name = name
        self.writers = []
        self.readers = []
        self.excl = False


class Op:
    __slots__ = ("eng", "fn", "deps", "dma", "sem", "cnt", "need_inc", "k")

    def __init__(self, eng, fn, dma):
        self.eng = eng
        self.fn = fn
        self.deps = []
        self.dma = dma
        self.sem = None
        self.cnt = 0
        self.need_inc = False
        self.k = -1


class Prog:
    ENGS = ("pe", "act", "dve", "pool", "sp")
    NDMA = 16
    LIMIT = 8000

    def __init__(self, nc):
        self.nc = nc
        self.ops = {e: [] for e in self.ENGS}
        self.ndma = 0
        self.dma_ops = []
        self.dma_q = {}
        self.all_bufs = []
        self.barrier_marks = []

    def buf(self, name):
        b = Buf(name)
        b.writers = list(self.barrier_marks)
        self.all_bufs.append(b)
        return b

    def bufs(self, name, n):
        return [self.buf("%s%d" % (name, i)) for i in range(n)]

    def op(self, eng, fn, reads=(), writes=(), dma=False, extra=()):
        o = Op(eng, fn, dma)
        deps = o.deps
        xr = [b for b in reads if b.excl and b not in writes]
        if xr:
            writes = list(writes) + xr
        for b in reads:
            for w in b.writers:
                deps.append(w)
        for b in writes:
            if b.readers:
                for r in b.readers:
                    if r is o:
                        continue
                    if r.eng == eng and eng == "pe" and not r.dma and not dma:
                        continue
                    deps.append(r)
                b.writers = []
                b.readers = []
            else:
                for w in b.writers:
                    if w.eng != eng or w.dma or dma or eng != "pe":
                        deps.append(w)
        for d in extra:
            deps.append(d)
        for b in reads:
            b.readers.append(o)
        for b in writes:
            b.writers.append(o)
        if dma:
            q = self.dma_q.setdefault(eng, [])
            o.k = len(q)
            if len(q) >= self.NDMA:
                deps.append(q[len(q) - self.NDMA])
            q.append(o)
            self.ndma += 1
            self.dma_ops.append(o)
        self.ops[eng].append(o)
        return o

    def barrier(self):
        marks = []
        for q in self.dma_q.values():
            marks.extend(q[-self.NDMA:])
        for e in self.ENGS:
            for o in reversed(self.ops[e]):
                if not o.dma:
                    marks.append(o)
                    break
        self.barrier_marks = marks
        for b in self.all_bufs:
            b.writers = list(marks)
            b.readers = []

    def emit(self):
        nc = self.nc
        for e in self.ENGS:
            for o in self.ops[e]:
                for d in o.deps:
                    if not d.dma:
                        d.need_inc = True
        self._sem_ctx = []
        for qn, q in self.dma_q.items():
            dsems = []
            for i in range(min(self.NDMA, len(q))):
                c = nc.semaphore("dq_%s_%d" % (qn, i))
                dsems.append(c.__enter__())
                self._sem_ctx.append(c)
            dcount = [0] * self.NDMA
            for o in q:
                s = o.k % self.NDMA
                dcount[s] += 16
                o.sem = dsems[s]
                o.cnt = dcount[s]
        for e in self.ENGS:
            cur = None
            n = self.LIMIT
            si = 0
            for o in self.ops[e]:
                if o.dma or not o.need_inc:
                    continue
                if n >= self.LIMIT:
                    c = nc.semaphore("e_%s_%d" % (e, si))
                    cur = c.__enter__()
                    self._sem_ctx.append(c)
                    si += 1
                    n = 0
                n += 1
                o.sem = cur
                o.cnt = n
        engmap = {"pe": nc.tensor, "act": nc.scalar, "dve": nc.vector, "pool": nc.gpsimd, "sp": nc.sync}

        def run(ename, eng):
            waited = {}
            for o in self.ops[ename]:
                need = {}
                for d in o.deps:
                    if d.sem is None:
                        continue
                    key = d.sem
                    if need.get(key, (0,))[0] < d.cnt:
                        need[key] = (d.cnt, d.sem)
                for key, (cnt, sem) in need.items():
                    if waited.get(key, 0) >= cnt:
                        continue
                    eng.wait_ge(sem, cnt)
                    waited[key] = cnt
                inst = o.fn(eng)
                if o.dma:
                    inst.then_inc(o.sem, 16)
                elif o.need_inc:
                    inst.then_inc(o.sem, 1)

        with nc.Block() as block:
            @block.tensor
            def _(e):
                run("pe", e)

            @block.scalar
            def _(e):
                run("act", e)

            @block.vector
            def _(e):
                run("dve", e)

            @block.gpsimd
            def _(e):
                run("pool", e)

            @block.sync
            def _(e):
                run("sp", e)
        for c in reversed(self._sem_ctx):
            c.__exit__(None, None, None)


def _nop_fn(ename):
    def f(eng):
        return eng.nop()
    return f


D = 1024
NCTX = 256
NX = 2048
NT0 = 18
EPS = 1e-6
NE = 16
DE = 512
BASE = 16640
X_OFF = BASE
HT_OFF = X_OFF + 18 * 4096
GBC_OFF = HT_OFF + 8 * 2304 * 2
SM_OFF = GBC_OFF + 8192
TMP_OFF = SM_OFF + 4096
SB_END = 229376
_DT_SIZE = {F32: 4, BF16: 2}


class Ctx:
    pass


class Region:
    def __init__(self, nc, lo, hi, tag):
        self.nc, self.lo, self.hi, self.cur, self.tag = nc, lo, hi, lo, tag
        self.n = 0

    def t(self, name, shape, dtype):
        nbytes = _DT_SIZE[dtype]
        for s in shape[1:]:
            nbytes *= s
        nbytes = (nbytes + 31) // 32 * 32
        assert self.cur + nbytes <= self.hi, (self.tag, name, self.cur + nbytes - self.hi)
        h = self.nc.alloc_sbuf_tensor_at("%s_%s" % (self.tag, name), list(shape), dtype, offset=self.cur)
        self.cur += nbytes
        return h


def build_program(stop_after=None, dbg=False):
    nc = bass.Bass("TRN2", target_bir_lowering=False)
    P = Prog(nc)
    C = Ctx()
    C.nc, C.P = nc, P

    def din(name, shape):
        return nc.dram_tensor(name, list(shape), F32, kind="ExternalInput").ap()

    I = Ctx()
    I.x = din("x", [NX, D])
    I.ctx = din("ctx", [NCTX, D])
    I.ccol = din("ccol", [128, 8, 2])
    I.ada_w = din("ada_w", [2, 12, D, 512])
    I.adab2 = din("adab2", [2, 2, 6 * D])
    I.ncol = din("ncol", [128, 2, 2, 8])
    I.a_w_in = din("a_w_in", [8, D, 512])
    I.a_w_out = din("a_w_out", [2048, D])
    I.gv_bc = din("gv_bc", [128, 2048])
    I.wsT = din("wsT", [128, 8, 128])
    I.bs_bc = din("bs_bc", [128, 8, 128])
    I.wqk = din("wqk", [12, D, 128])
    I.wvv = din("wvv", [D, 256])
    I.b_w_o = din("b_w_o", [D, D])
    I.sink_bc = din("sink_bc", [128, 16])
    I.wr = din("wr", [128, 8, 16])
    I.rb_bc = din("rb_bc", [128, 16])
    I.w_gate = din("moe_w_gate", [2, NE, D, DE])
    I.w_up = din("moe_w_up", [2, NE, D, DE])
    I.w_down = din("moe_w_down", [2, NE, DE, D])
    I.fg_bc = din("fg_bc", [128, D])
    I.rope = din("rope", [2, 128, NX])
    I.perm = din("perm", [128, 128])
    I.mask = din("mask", [128, 384])
    C.I = I
    C.out = nc.dram_tensor("out", [NX, D], F32, kind="ExternalOutput").ap()
    C.msc = nc.dram_tensor("msc", [2, 2, 6 * D], F32, kind="Internal").ap()
    C.mscB = P.buf("msc")
    C.dbg = dbg
    if dbg:
        C.dbg_x = nc.dram_tensor("dbg_x", [NT0 * 128, D], F32, kind="ExternalOutput").ap()
        C.dbg_m = nc.dram_tensor("dbg_m", [2, 2, 6 * D], F32, kind="ExternalOutput").ap()

    C.x_sb = nc.alloc_sbuf_tensor_at("x_sb", [128, NT0, D], F32, offset=X_OFF)
    C.xB = P.bufs("x", NT0)
    C.hT = nc.alloc_sbuf_tensor_at("hT", [128, 8, 2304], BF16, offset=HT_OFF)
    C.gbc = nc.alloc_sbuf_tensor_at("gbc", [128, 2, D], F32, offset=GBC_OFF)
    C.gbcB = P.bufs("gbc", 2)
    sm = Region(nc, SM_OFF, TMP_OFF, "sm")
    C.cols = sm.t("cols", [128, 2, 4, 8, 2], F32)
    C.colsB = P.buf("cols")
    C.gatesv = sm.t("gatesv", [128, NT0, NE], F32)
    C.gatesB = P.bufs("gates", NT0)
    C.ident_f = sm.t("ident_f", [128, 128], F32)
    C.ident_b = sm.t("ident_b", [128, 128], BF16)
    C.identB = P.buf("ident")
    C.rb = sm.t("rb", [128, NE], F32)
    C.rbB = P.buf("rb")
    C.ncol = sm.t("ncol", [128, 2, 2, 8], F32)
    C.ncolB = P.buf("ncol")
    C.sclhs = sm.t("sclhs", [128, 8, 2], BF16)
    C.scB = P.buf("sclhs")
    C.ps = [nc.alloc_psum_tensor("ps%d" % i, [128, 512], F32) for i in range(8)]
    C.psB = P.bufs("ps", 8)
    for b in C.psB:
        b.excl = True
    C.ntt = 0

    P.op("pool", lambda e: e.memset(C.ident_f[:], 1.0), writes=[C.identB])
    P.op("pool", lambda e: e.affine_select(out=C.ident_f[:], in_=C.ident_f[:], pattern=[[-1, 128]],
                                           compare_op=ALU.is_equal, fill=0.0, base=0, channel_multiplier=1),
         reads=[C.identB], writes=[C.identB])
    P.op("dve", lambda e: e.tensor_copy(out=C.ident_b[:], in_=C.ident_f[:]), reads=[C.identB], writes=[C.identB])
    P.op("sp", lambda e: e.dma_start(out=C.rb[:], in_=I.rb_bc), writes=[C.rbB], dma=True)
    P.op("sp", lambda e: e.dma_start(out=C.ncol[:], in_=I.ncol), writes=[C.ncolB], dma=True)

    phases = stop_after if (stop_after and stop_after.startswith("!")) else None
    if phases is not None:
        for ph in phases[1:]:
            if ph == "A":
                phase_adaln(C)
            elif ph == "L":
                for t in range(NT0):
                    src = C.I.ctx[t * 128:(t + 1) * 128, :] if t < 2 else C.I.x[(t - 2) * 128:(t - 1) * 128, :]
                    P.op("sp", lambda e, t=t, src=src: e.dma_start(out=C.x_sb[:, t, :], in_=src), writes=[C.xB[t]], dma=True)
            elif ph == "B":
                phase_gmlp(C)
            elif ph == "C":
                phase_moe(C, 0)
            elif ph == "D":
                phase_attn(C)
            elif ph == "E":
                phase_moe(C, 1)
            elif ph == "F":
                phase_final(C)
        return finish(C)
    phase_adaln(C)
    if stop_after == "A":
        return finish(C)
    phase_gmlp(C)
    if stop_after == "B":
        return finish(C)
    phase_moe(C, 0)
    if stop_after == "C":
        return finish(C)
    phase_attn(C)
    if stop_after == "D":
        return finish(C)
    phase_moe(C, 1)
    phase_final(C)
    return finish(C)


def finish(C):
    P = C.P
    if C.dbg:
        for t in range(2 if getattr(C, "attn_done", False) else 0, NT0):
            P.op("sp", lambda e, t=t: e.dma_start(out=C.dbg_x[t * 128:(t + 1) * 128, :], in_=C.x_sb[:, t, :]),
                 reads=[C.xB[t]], dma=True)
        P.op("sp", lambda e: e.dma_start(out=C.dbg_m, in_=C.msc), reads=[C.mscB], dma=True)
    P.barrier()
    P.op("sp", lambda e: e.nop(), reads=[C.mscB])
    P.emit()
    return C.nc


def adaln_cols(C, layer):
    P = C.P
    for vi, vec in enumerate((0, 1, 3, 4)):
        for who in range(2):
            P.op("sp", lambda e, vi=vi, vec=vec, who=who: e.dma_start(
                out=C.cols[:, layer, vi, :, who],
                in_=C.msc[layer, who, vec * 1024:(vec + 1) * 1024].rearrange("(k p) -> p k", p=128),
                allow_slow_non_contiguous=True),
                reads=[C.mscB], writes=[C.colsB], dma=True)
    for vi, wn in ((1, 0), (3, 1)):
        for who in range(2):
            P.op("dve", lambda e, vi=vi, wn=wn, who=who: e.scalar_tensor_tensor(
                out=C.cols[:, layer, vi, :, who], in0=C.cols[:, layer, vi, :, who], scalar=1.0,
                in1=C.ncol[:, layer, wn, :], op0=ALU.add, op1=ALU.mult),
                reads=[C.colsB, C.ncolB], writes=[C.colsB])


def phase_adaln(C):
    nc, P, I = C.nc, C.P, C.I
    P.barrier()
    R = Region(nc, TMP_OFF, SB_END, "pa")
    m_sb = R.t("m", [2, 6 * D], F32)
    mB = P.buf("m")
    adab = R.t("adab", [2, 6 * D], F32)
    adabB = P.buf("adab")
    wa = [R.t("wa%d" % i, [128, 8, 512], BF16) for i in range(3)]
    waB = P.bufs("wa", 3)
    ccol = R.t("ccol", [128, 8, 2], F32)
    ccB = P.buf("ccol")
    P.op("sp", lambda e: e.dma_start(out=ccol[:], in_=I.ccol), writes=[ccB], dma=True)
    P.op("act", lambda e: e.activation(out=C.sclhs[:], in_=ccol[:], func=AF.Silu), reads=[ccB], writes=[C.scB])
    layer = 0
    P.op("sp", lambda e: e.dma_start(out=adab[:], in_=I.adab2[layer]), writes=[adabB], dma=True)
    for nb in range(12):
        j = nb % 3
        P.op("pool", lambda e, nb=nb, j=j: e.dma_start(out=wa[j][:], in_=I.ada_w[layer, nb].rearrange("(c p) f -> p c f", p=128)),
             writes=[waB[j]], dma=True)
        pb = nb % 2
        for kk in range(8):
            P.op("pe", lambda e, j=j, kk=kk, pb=pb: e.matmul(C.ps[pb][0:2, :], C.sclhs[:, kk, :], wa[j][:, kk, :],
                                                           start=(kk == 0), stop=(kk == 7)),
                 reads=[waB[j], C.scB], writes=[C.psB[pb]])
        P.op("dve", lambda e, nb=nb, pb=pb: e.tensor_tensor(out=m_sb[:, nb * 512:(nb + 1) * 512], in0=C.ps[pb][0:2, :],
                                                            in1=adab[:, nb * 512:(nb + 1) * 512], op=ALU.add),
             reads=[C.psB[pb], adabB], writes=[mB])
    P.op("sp", lambda e: e.dma_start(out=C.msc[layer], in_=m_sb[:]), reads=[mB], writes=[C.mscB], dma=True)
    adaln_cols(C, layer)


def adaln_piece_load(C, q, wa2, wa2B, adabp, adabpB):
    P, I = C.P, C.I
    nb, h = q // 2, q % 2
    c0 = nb * 512 + h * 256
    P.op("pool", lambda e: e.dma_start(out=wa2[:], in_=I.ada_w[1, nb][:, h * 256:(h + 1) * 256].rearrange("(c p) f -> p c f", p=128)),
         writes=[wa2B], dma=True)
    P.op("sp", lambda e: e.dma_start(out=adabp[:], in_=I.adab2[1][:, c0:c0 + 256]), writes=[adabpB], dma=True)


def adaln_piece(C, q, wa2, wa2B, adabp, adabpB, mp, mpB):
    P, I = C.P, C.I
    nb, h = q // 2, q % 2
    c0 = nb * 512 + h * 256
    for kk in range(8):
        P.op("pe", lambda e, kk=kk: e.matmul(C.ps[0][0:2, 0:256], C.sclhs[:, kk, :], wa2[:, kk, :], start=(kk == 0), stop=(kk == 7)),
             reads=[wa2B, C.scB], writes=[C.psB[0]])
    P.op("dve", lambda e: e.tensor_tensor(out=mp[:], in0=C.ps[0][0:2, 0:256], in1=adabp[:], op=ALU.add),
         reads=[C.psB[0], adabpB], writes=[mpB])
    P.op("sp", lambda e: e.dma_start(out=C.msc[1][:, c0:c0 + 256], in_=mp[:]), reads=[mpB], writes=[C.mscB], dma=True)


def load_gate_bc(C, layer, gi, who, slot):
    vec = 2 if gi == 0 else 5
    src = C.msc[layer, who:who + 1, vec * 1024:(vec + 1) * 1024]
    C.P.op("sp", lambda e: e.dma_start(out=C.gbc[:, slot, :], in_=src.partition_broadcast(128)),
           reads=[C.mscB], writes=[C.gbcB[slot]], dma=True)


def ps_bf16(C, b):
    return C.ps[b][:].bitcast(BF16)


def norm_stats(C, tiles, ssall, ssBt, rstd, rstdB, junks, junkBs, eps, epsB):
    P = C.P
    T = len(tiles)
    for i, t in enumerate(tiles):
        P.op("dve", lambda e, i=i: e.memset(ssall[:, i:i + 1], 0.0), writes=[ssBt[i]])
    for i, t in enumerate(tiles):
        j = i % len(junks)
        P.op("act", lambda e, i=i, t=t, j=j: e.activation(out=junks[j][:, 0:D], in_=C.x_sb[:, t, :], func=AF.Square, accum_out=ssall[:, i:i + 1]),
             reads=[C.xB[t], ssBt[i]], writes=[ssBt[i], junkBs[j]])
    P.op("act", lambda e: e.activation(out=rstd[:, 0:T], in_=ssall[:, 0:T], func=AF.Sqrt, scale=1.0 / D, bias=eps[:, 0:1]),
         reads=list(ssBt[:T]) + [epsB], writes=[rstdB])
    P.op("dve", lambda e: e.reciprocal(out=rstd[:, 0:T], in_=rstd[:, 0:T]), reads=[rstdB], writes=[rstdB])


def norm_transpose_tile(C, R, src_ap, srcB, layer, which, who, dstT, dstB, tok0, st, ps_bank, rstd_ap=None, rstdB=None):
    P = C.P
    if isinstance(st, list):
        st = st[C.ntt % len(st)]
    if isinstance(ps_bank, (list, tuple)):
        ps_bank = ps_bank[C.ntt % len(ps_bank)]
    sh_v, gm_v = (0, 1) if which == 0 else (2, 3)
    ss, ssB = st["ss"], st["ssB"]
    if rstd_ap is not None:
        P.op("dve", lambda e: e.tensor_scalar(out=st["xn"][:], in0=src_ap, scalar1=rstd_ap, scalar2=None, op0=ALU.mult),
             reads=[srcB, rstdB], writes=[st["xnB"]])
    else:
        _norm_stats_single(C, src_ap, srcB, st)
    _transpose_evac(C, layer, which, who, dstT, dstB, tok0, st, ps_bank)


def _norm_stats_single(C, src_ap, srcB, st):
    P = C.P
    ss, ssB = st["ss"], st["ssB"]
    P.op("dve", lambda e: e.memset(ss[:], 0.0), writes=[ssB])
    P.op("act", lambda e: e.activation(out=st["junk"][:, 0:D], in_=src_ap, func=AF.Square, accum_out=ss[:, 0:1]),
         reads=[srcB, ssB], writes=[ssB, st["junkB"]])
    P.op("act", lambda e: e.activation(out=ss[:, 0:1], in_=ss[:, 0:1], func=AF.Sqrt, scale=1.0 / D, bias=st["eps"][:, 0:1]),
         reads=[ssB, st["epsB"]], writes=[ssB])
    P.op("dve", lambda e: e.reciprocal(out=ss[:, 0:1], in_=ss[:, 0:1]), reads=[ssB], writes=[ssB])
    P.op("dve", lambda e: e.tensor_scalar(out=st["xn"][:], in0=src_ap, scalar1=ss[:, 0:1], scalar2=None, op0=ALU.mult),
         reads=[srcB, ssB], writes=[st["xnB"]])


def _transpose_evac(C, layer, which, who, dstT, dstB, tok0, st, ps_bank):
    P = C.P
    sh_v, gm_v = (0, 1) if which == 0 else (2, 3)
    pt = ps_bf16(C, ps_bank)
    use_act = (C.ntt % 2 == 0)
    C.ntt += 1
    for k in range(8):
        P.op("pe", lambda e, k=k: e.transpose(out=pt[:, k * 128:(k + 1) * 128], in_=st["xn"][:, k * 128:(k + 1) * 128],
                                              identity=C.ident_b[:]),
             reads=[st["xnB"], C.identB], writes=[C.psB[ps_bank]])
    for k in range(8):
        gm = C.cols[:, layer, gm_v, k, who:who + 1]
        sh = C.cols[:, layer, sh_v, k, who:who + 1]
        if use_act:
            P.op("act", lambda e, k=k, gm=gm, sh=sh: e.activation(out=dstT[:, k, tok0:tok0 + 128], in_=pt[:, k * 128:(k + 1) * 128],
                                                                 func=AF.Identity, scale=gm, bias=sh),
                 reads=[C.psB[ps_bank], C.colsB], writes=[dstB])
        else:
            P.op("dve", lambda e, k=k, gm=gm, sh=sh: e.tensor_scalar(out=dstT[:, k, tok0:tok0 + 128], in0=pt[:, k * 128:(k + 1) * 128],
                                                                    scalar1=gm, scalar2=sh, op0=ALU.mult, op1=ALU.add),
                 reads=[C.psB[ps_bank], C.colsB], writes=[dstB])


def make_norm_scratch(C, R, tag):
    P = C.P
    st = {}
    st["junk"] = R.t(tag + "junk", [128, 2048], BF16)
    st["junkB"] = P.buf(tag + "junk")
    st["ss"] = R.t(tag + "ss", [128, 2], F32)
    st["ssB"] = P.buf(tag + "ss")
    st["xn"] = R.t(tag + "xn", [128, D], BF16)
    st["xnB"] = P.buf(tag + "xn")
    st["eps"] = R.t(tag + "eps", [128, 1], F32)
    epsB = P.buf(tag + "eps")
    P.op("dve", lambda e: e.memset(st["eps"][:], EPS), writes=[epsB])
    st["epsB"] = epsB
    return st


def phase_gmlp(C):
    nc, P, I = C.nc, C.P, C.I
    layer = 0
    P.barrier()
    R = Region(nc, TMP_OFF, SB_END, "pb")
    wi = [R.t("wi%d" % i, [128, 8, 512], BF16) for i in range(2)]
    wiB = P.bufs("wi", 2)
    hTg = R.t("hTg", [128, 8, 512], BF16)
    hTgB = P.buf("hTg")
    uT = R.t("uT", [128, 16, 512], BF16)
    uTB = P.buf("uT")
    vraw4 = R.t("vraw", [128, 4, 2048], BF16)
    vrawB4 = P.bufs("vraw", 4)
    prodT = [R.t("prodT%d" % i, [128, 16, 128], BF16) for i in range(2)]
    prodB = P.bufs("prodT", 2)
    sts = []
    for i in range(2):
        st = {}
        st["xn"] = R.t("xn%d" % i, [128, D], BF16)
        st["xnB"] = P.buf("xn%d" % i)
        st["junk"], st["junkB"] = st["xn"], st["xnB"]
        st["ss"] = R.t("ss%d" % i, [128, 2], F32)
        st["ssB"] = P.buf("ss%d" % i)
        sts.append(st)
    eps = R.t("eps", [128, 1], F32)
    epsB = P.buf("eps")
    ssall = R.t("ssall", [128, 4], F32)
    ssBt = P.bufs("ssall", 4)
    rstd = R.t("rstd", [128, 4], F32)
    rstdB = P.buf("rstd")
    ssv = R.t("ssv", [128, 4, 4], F32)
    ssvBt = P.bufs("ssv", 4)
    rstdv = R.t("rstdv", [128, 4], F32)
    rstdvB = P.buf("rstdv")
    tmpf = [R.t("tmpf%d" % i, [128, D], F32) for i in range(2)]
    tmpfB = P.bufs("tmpf", 2)
    gv = R.t("gv", [128, 2048], BF16)
    gvB = P.buf("gv")
    bsrow = R.t("bsrow", [1, 8, 128], BF16)
    ones1 = R.t("ones1", [1, 128], BF16)
    bsB = P.buf("bs")
    wsT = R.t("wsT", [128, 8, 128], BF16)
    wsB = P.buf("ws")
    w_out = nc.alloc_sbuf_tensor_at("pb_wout", [128, 16, D], BF16, offset=HT_OFF)
    woB = P.buf("wout")

    P.op("dve", lambda e: e.memset(eps[:], EPS), writes=[epsB])
    P.op("dve", lambda e: e.memset(ones1[:], 1.0), writes=[bsB])
    P.op("pool", lambda e: e.dma_start(out=gv[:], in_=I.gv_bc), writes=[gvB], dma=True)
    P.op("pool", lambda e: e.dma_start(out=wsT[:], in_=I.wsT), writes=[wsB], dma=True)
    P.op("pool", lambda e: e.dma_start(out=bsrow[:], in_=I.bs_bc[0:1]), writes=[bsB], dma=True)
    load_gate_bc(C, layer, 0, 0, 0)
    load_gate_bc(C, layer, 0, 1, 1)
    for h in range(2):
        P.op("pool", lambda e, h=h: e.dma_start(out=w_out[:, h * 8:(h + 1) * 8, :],
                                               in_=I.a_w_out[h * 1024:(h + 1) * 1024, :].rearrange("(c p) f -> p c f", p=128)),
             writes=[woB], dma=True)

    groups = [[0, 1, 2, 3], [4, 5, 6, 7], [8, 9, 10, 11], [12, 13, 14, 15], [16, 17]]
    ring = [1, 2, 3]
    rk = 0
    pmk = 0
    wk = 0
    tk = 0
    st8 = {"rk": 0, "pmk": 0, "wk": 0, "tk": 0}
    wsc = nc.dram_tensor("w_in_bf16", [8, 128, 8 * 512], BF16, kind="Internal").ap()
    wscB = P.bufs("wsc", 8)

    def load_piece(pc, s, gi):
        if gi == 0:
            P.op("pool", lambda e: e.dma_start(out=wi[s][:], in_=I.a_w_in[pc].rearrange("(c p) f -> p c f", p=128)),
                 writes=[wiB[s]], dma=True)
            P.op("sp", lambda e: e.dma_start(out=wsc[pc], in_=wi[s][:].rearrange("p c f -> p (c f)")),
                 reads=[wiB[s]], writes=[wscB[pc]], dma=True)
        else:
            P.op("sp", lambda e: e.dma_start(out=wi[s][:].rearrange("p c f -> p (c f)"), in_=wsc[pc]),
                 reads=[wscB[pc]], writes=[wiB[s]], dma=True)

    def stage_a(g):
        n = len(g)
        N = 128 * n
        for i, t in enumerate(g):
            src = I.ctx[t * 128:(t + 1) * 128, :] if t < 2 else I.x[(t - 2) * 128:(t - 1) * 128, :]
            P.op("sp", lambda e, t=t, src=src: e.dma_start(out=C.x_sb[:, t, :], in_=src), writes=[C.xB[t]], dma=True)
        norm_stats(C, g, ssall, ssBt, rstd, rstdB, [sts[0]["xn"], sts[1]["xn"]], [sts[0]["xnB"], sts[1]["xnB"]], eps, epsB)
        def xn_part(i, t):
            stx = sts[i % 2]
            P.op("dve", lambda e, i=i, t=t, stx=stx: e.tensor_scalar(out=stx["xn"][:], in0=C.x_sb[:, t, :], scalar1=rstd[:, i:i + 1], scalar2=None,
                                                                    op0=ALU.mult),
                 reads=[C.xB[t], rstdB], writes=[stx["xnB"]])

        def tr_part(i, t):
            who = 1 if t < 2 else 0
            _transpose_evac(C, layer, 0, who, hTg, hTgB, i * 128, sts[i % 2], 0)

        xn_part(0, g[0])
        for i, t in enumerate(g):
            if i + 1 < n:
                xn_part(i + 1, g[i + 1])
            tr_part(i, t)

    def stage_bc(g, gi):
        n = len(g)
        N = 128 * n
        rk, wk = st8["rk"], st8["wk"]
        for jj in range(4):
            s = wk % 2
            wk += 1
            load_piece(jj, s, gi)
            for j4 in range(4):
                j = jj * 4 + j4
                pb = ring[rk % 3]
                rk += 1
                for k in range(8):
                    P.op("pe", lambda e, s=s, j4=j4, k=k, pb=pb, N=N: e.matmul(
                        C.ps[pb][:, 0:N], wi[s][:, k, j4 * 128:(j4 + 1) * 128], hTg[:, k, 0:N], start=(k == 0), stop=(k == 7)),
                        reads=[wiB[s], hTgB], writes=[C.psB[pb]])
                P.op("act", lambda e, j=j, pb=pb, N=N: e.activation(out=uT[:, j, 0:N], in_=C.ps[pb][:, 0:N], func=AF.Gelu_apprx_tanh),
                     reads=[C.psB[pb]], writes=[uTB])
        for sv in range(4):
            s = wk % 2
            wk += 1
            load_piece(4 + sv, s, gi)
            for i, t in enumerate(g):
                pb = ring[rk % 3]
                rk += 1
                for k in range(8):
                    P.op("pe", lambda e, s=s, k=k, pb=pb, i=i: e.matmul(
                        C.ps[pb][:, :], hTg[:, k, i * 128:(i + 1) * 128], wi[s][:, k, :], start=(k == 0), stop=(k == 7)),
                        reads=[wiB[s], hTgB], writes=[C.psB[pb]])
                P.op("act", lambda e, sv=sv, pb=pb, i=i: e.activation(out=vraw4[:, i, sv * 512:(sv + 1) * 512], in_=C.ps[pb][:, :],
                                                                      func=AF.Gelu_apprx_tanh),
                     reads=[C.psB[pb]], writes=[vrawB4[i]])
                if sv == 0:
                    P.op("dve", lambda e, i=i: e.memset(ssv[:, i, :], 0.0), writes=[ssvBt[i]])
                jb = sts[(i + sv) % 2]
                P.op("act", lambda e, sv=sv, i=i, jb=jb: e.activation(out=jb["xn"][:, 0:512], in_=vraw4[:, i, sv * 512:(sv + 1) * 512],
                                                                    func=AF.Square, accum_out=ssv[:, i, sv:sv + 1]),
                     reads=[vrawB4[i], ssvBt[i]], writes=[ssvBt[i], jb["xnB"]])
        P.op("dve", lambda e, n=n: e.tensor_reduce(out=rstdv[:, 0:n], in_=ssv[:, 0:n, :], axis=AX.X, op=ALU.add),
             reads=list(ssvBt[:n]), writes=[rstdvB])
        P.op("act", lambda e, n=n: e.activation(out=rstdv[:, 0:n], in_=rstdv[:, 0:n], func=AF.Sqrt, scale=1.0 / 2048, bias=eps[:, 0:1]),
             reads=[rstdvB, epsB], writes=[rstdvB])
        P.op("dve", lambda e, n=n: e.reciprocal(out=rstdv[:, 0:n], in_=rstdv[:, 0:n]), reads=[rstdvB], writes=[rstdvB])
        st8["rk"], st8["wk"] = rk, wk

    def stage_d(g):
        n = len(g)
        pmk, tk = st8["pmk"], st8["tk"]
        pps = {}

        def spatial(i, t):
            nonlocal pmk, tk
            pp = tk % 2
            tk += 1
            pps[i] = pp
            P.op("dve", lambda e, i=i: e.scalar_tensor_tensor(out=vraw4[:, i, :], in0=vraw4[:, i, :], scalar=rstdv[:, i:i + 1], in1=gv[:],
                                                             op0=ALU.mult, op1=ALU.mult),
                 reads=[vrawB4[i], rstdvB, gvB], writes=[vrawB4[i]])
            for b4 in range(4):
                pm = 4 + (pmk % 2)
                pmk += 1
                brow = bsrow[0:1, 2 * b4:2 * b4 + 2, :].unsqueeze(2).to_broadcast([1, 2, 2, 128])
                P.op("pe", lambda e, pm=pm, brow=brow: e.matmul(C.ps[pm][:, :].rearrange("p (a b n) -> p a b n", a=2, b=2), ones1[0:1, :], brow,
                                                                start=True, stop=False),
                     reads=[bsB], writes=[C.psB[pm]])
                for jq in range(4):
                    j = b4 * 4 + jq
                    P.op("pe", lambda e, pm=pm, jq=jq, j=j, i=i: e.matmul(
                        C.ps[pm][:, jq * 128:(jq + 1) * 128], vraw4[:, i, j * 128:(j + 1) * 128], wsT[:, j // 2, :], start=False, stop=(jq == 3)),
                        reads=[vrawB4[i], wsB], writes=[C.psB[pm]])
                P.op("dve", lambda e, pm=pm, b4=b4, i=i, pp=pp: e.tensor_tensor(
                    out=prodT[pp][:, 4 * b4:4 * b4 + 4, :], in0=C.ps[pm][:, :].rearrange("p (a n) -> p a n", a=4),
                    in1=uT[:, 4 * b4:4 * b4 + 4, i * 128:(i + 1) * 128], op=ALU.mult),
                    reads=[C.psB[pm], uTB], writes=[prodB[pp]])

        def outproj(i, t):
            who = 1 if t < 2 else 0
            pp = pps[i]
            for half in range(2):
                pb = 6 + half
                for j in range(16):
                    P.op("pe", lambda e, pb=pb, j=j, half=half, pp=pp: e.matmul(
                        C.ps[pb][:, :], prodT[pp][:, j, :], w_out[:, j, half * 512:(half + 1) * 512], start=(j == 0), stop=(j == 15)),
                        reads=[prodB[pp], woB], writes=[C.psB[pb]])
                P.op("dve", lambda e, pb=pb, half=half, who=who, pp=pp: e.tensor_tensor(
                    out=tmpf[pp][:, half * 512:(half + 1) * 512], in0=C.ps[pb][:, :], in1=C.gbc[:, who, half * 512:(half + 1) * 512], op=ALU.mult),
                    reads=[C.psB[pb], C.gbcB[who]], writes=[tmpfB[pp]])
            P.op("dve", lambda e, t=t, pp=pp: e.tensor_tensor(out=C.x_sb[:, t, :], in0=C.x_sb[:, t, :], in1=tmpf[pp][:], op=ALU.add),
                 reads=[tmpfB[pp], C.xB[t]], writes=[C.xB[t]])

        spatial(0, g[0])
        for i in range(n):
            if i + 1 < n:
                spatial(i + 1, g[i + 1])
            outproj(i, g[i])
        st8["pmk"], st8["tk"] = pmk, tk

    stage_a(groups[0])
    for gi, g in enumerate(groups):
        stage_bc(g, gi)
        if gi + 1 < len(groups):
            stage_a(groups[gi + 1])
        stage_d(g)


def _const_tables():
    t = np.arange(NX)
    row = (t // 64).astype(np.float32)
    col = (t % 64).astype(np.float32)
    inv = (10000.0 ** (-np.arange(16, dtype=np.float32) / 16)).astype(np.float32)
    cosT = np.zeros((128, NX), np.float32)
    sinT = np.zeros((128, NX), np.float32)
    perm = np.zeros((128, 128), np.float32)
    for p in range(128):
        d = p % 64
        axis = d // 32
        half = (d % 32) // 16
        f = d % 16
        ang = (row if axis == 0 else col) * inv[f]
        cosT[p] = np.cos(ang)
        sinT[p] = -np.sin(ang) if half == 0 else np.sin(ang)
        partner = p + 16 if half == 0 else p - 16
        perm[partner, p] = 1.0
    qi = np.arange(128)[:, None]
    kj = np.arange(128)[None, :]
    NEG = -30000.0
    mask = np.zeros((128, 384), np.float32)
    mask[:, 0:128] = np.where(qi >= kj, 0.0, NEG)
    mask[:, 128:256] = np.where(qi <= kj, 0.0, NEG)
    return np.stack([cosT, sinT]), perm, mask


def _wqk_layout(w):
    chunks = [w[:, c * 128:(c + 1) * 128] for c in range(8)]
    for hk in range(4):
        kh = w[:, 1024 + hk * 64:1024 + (hk + 1) * 64]
        chunks.append(np.concatenate([kh, kh], axis=1))
    return np.stack(chunks, axis=0)


def _prep(inputs):
    f = lambda a: np.ascontiguousarray(np.asarray(a, dtype=np.float32))
    g = {k: np.asarray(v) for k, v in inputs.items()}
    rope, perm, mask = _const_tables()
    ncol = np.stack([g["norm1_g"], g["norm2_g"]], axis=0)
    ncol = ncol.reshape(2, 2, 8, 128).transpose(3, 1, 0, 2)
    common = {
        "ada_w": f(g["ada_w"].reshape(2, D, 12, 512).transpose(0, 2, 1, 3)),
        "adab2": f(np.repeat(g["ada_b"][:, None, :], 2, axis=1)),
        "ncol": f(ncol),
        "a_w_in": f(g["a_w_in"][0].reshape(D, 8, 512).transpose(1, 0, 2)),
        "a_w_out": f(g["a_w_out"][0]),
        "gv_bc": f(np.broadcast_to(g["a_g_v"][0][None, :], (128, 2048))),
        "wsT": f(g["a_w_s"][0].transpose(2, 0, 1)),
        "bs_bc": f(np.broadcast_to(g["a_b_s"][0][None, :, :], (128, 8, 128))),
        "wqk": f(_wqk_layout(g["b_w_qkv"][0])),
        "wvv": f(g["b_w_qkv"][0][:, 1280:1536]),
        "b_w_o": f(g["b_w_o"][0]),
        "sink_bc": f(np.broadcast_to(g["b_sink"][0][None, :], (128, 16))),
        "wr": f(g["router_w"].reshape(8, 128, 16).transpose(1, 0, 2)),
        "rb_bc": f(np.broadcast_to(g["router_b"][None, :], (128, 16))),
        "moe_w_gate": f(g["moe_w_gate"]),
        "moe_w_up": f(g["moe_w_up"]),
        "moe_w_down": f(g["moe_w_down"]),
        "fg_bc": f(np.broadcast_to(g["final_g"][None, :], (128, D))),
        "rope": f(rope), "perm": f(perm), "mask": f(mask),
    }
    maps = []
    for b in range(8):
        ccol = np.stack([g["c"][b].reshape(8, 128).T, g["c_ctx"].reshape(8, 128).T], axis=-1)
        m = dict(common)
        m["x"] = f(g["x"][b])
        m["ctx"] = f(g["ctx"][b])
        m["ccol"] = f(ccol)
        maps.append(m)
    return maps


def kernel(**inputs):
    maps = _prep(inputs)
    nc = build_program()
    res = run_bass_kernel_spmd(nc, maps, core_ids=list(range(8)))
    out = np.stack([np.asarray(r["out"]) for r in res.results], axis=0)
    return out.astype(np.float32)


def phase_moe(C, layer):
    nc, P, I = C.nc, C.P, C.I
    tiles = list(range(NT0)) if layer == 0 else list(range(2, NT0))
    P.barrier()
    R = Region(nc, TMP_OFF, SB_END, "pc%d" % layer)
    Wg = [R.t("wg%d" % i, [128, 8, DE], BF16) for i in range(2)]
    Wu = [R.t("wu%d" % i, [128, 8, DE], BF16) for i in range(2)]
    Wd = [R.t("wd%d" % i, [128, 4, D], BF16) for i in range(2)]
    WgB, WuB, WdB = P.bufs("wg", 2), P.bufs("wu", 2), P.bufs("wd", 2)
    A = R.t("A", [128, 4, 2304], BF16)
    sg = [R.t("sg%d" % i, [128, 512], BF16) for i in range(2)]
    sgB = P.bufs("sg", 2)
    tmpf = [R.t("tmpf%d" % i, [128, D], F32) for i in range(2)]
    tmpfB = P.bufs("tmpf", 2)
    wr = R.t("wr", [128, 8, NE], BF16)
    wrB = P.buf("wr")
    ov_off = R.cur
    sts = []
    for i in range(2):
        st = {}
        st["xn"] = R.t("xn%d" % i, [128, D], BF16)
        st["xnB"] = P.buf("xn%d" % i)
        st["junk"], st["junkB"] = st["xn"], st["xnB"]
        st["ss"] = R.t("ss%d" % i, [128, 2], F32)
        st["ssB"] = P.buf("ss%d" % i)
        if i == 0:
            st["eps"] = R.t("eps", [128, 1], F32)
            st["epsB"] = P.buf("eps")
        else:
            st["eps"], st["epsB"] = sts[0]["eps"], sts[0]["epsB"]
        sts.append(st)
    st = sts[0]
    rtB = P.buf("rt")
    hTB = P.bufs("h2T", NT0)
    C.hTB = hTB

    P.op("dve", lambda e: e.memset(st["eps"][:], EPS), writes=[st["epsB"]])
    P.op("pool", lambda e: e.dma_start(out=wr[:], in_=I.wr), writes=[wrB], dma=True)
    load_gate_bc(C, layer, 1, 0, 0)
    if layer == 0:
        load_gate_bc(C, layer, 1, 1, 1)

    def load_expert(e):
        s = e % 2
        P.op("pool", lambda en: en.dma_start(out=Wg[s][:], in_=I.w_gate[layer, e].rearrange("(c p) f -> p c f", p=128)),
             writes=[WgB[s]], dma=True)
        P.op("pool", lambda en: en.dma_start(out=Wu[s][:], in_=I.w_up[layer, e].rearrange("(c p) f -> p c f", p=128)),
             writes=[WuB[s]], dma=True)
        P.op("pool", lambda en: en.dma_start(out=Wd[s][:], in_=I.w_down[layer, e].rearrange("(c p) f -> p c f", p=128)),
             writes=[WdB[s]], dma=True)

    load_expert(0)
    load_expert(1)

    T = len(tiles)
    t0 = tiles[0]
    ssall = R.t("ssall", [128, NT0], F32)
    ssBt = P.bufs("ssall", NT0)
    rstd = R.t("rstd", [128, NT0], F32)
    rstdB = P.buf("rstd")
    norm_stats(C, tiles, ssall, ssBt, rstd, rstdB, [sts[0]["xn"], sts[1]["xn"]], [sts[0]["xnB"], sts[1]["xnB"]], st["eps"], st["epsB"])
    RB = 5
    for i, t in enumerate(tiles):
        who = 1 if t < 2 else 0
        norm_transpose_tile(C, R, C.x_sb[:, t, :], C.xB[t], layer, 1, who, C.hT, hTB[t], t * 128, sts, (6, 7),
                            rstd_ap=rstd[:, i:i + 1], rstdB=rstdB)
        for k in range(8):
            P.op("pe", lambda e, k=k, t=t, i=i: e.matmul(C.ps[RB][:, i * NE:(i + 1) * NE], C.hT[:, k, t * 128:(t + 1) * 128], wr[:, k, :],
                                                         start=(k == 0), stop=(k == 7)),
                 reads=[hTB[t], wrB], writes=[C.psB[RB]])
    s_all = R.t("s_all", [128, NT0, NE], F32)
    sbb = R.t("sbb", [128, NT0, NE], F32)
    t6 = R.t("t6", [128, NT0 * 4, 6], F32)
    gsc = R.t("gsc", [128, NT0 * 4], F32)
    gmx = R.t("gmx", [128, NT0], F32)
    pen = R.t("pen", [128, NT0 * 4], F32)
    msk = R.t("msk", [128, NT0, NE], F32)
    eq = R.t("eq", [128, NT0, NE], F32)
    m2 = R.t("m2", [128, NT0], F32)
    P.op("act", lambda e: e.activation(out=s_all[:, 0:T, :], in_=C.ps[RB][:, 0:T * NE].rearrange("p (t e) -> p t e", e=NE), func=AF.Sigmoid),
         reads=[C.psB[RB]], writes=[rtB])
    P.op("dve", lambda e: e.tensor_tensor(out=sbb[:, 0:T, :], in0=s_all[:, 0:T, :], in1=C.rb[:].unsqueeze(1).to_broadcast([128, T, NE]), op=ALU.add),
         reads=[rtB, C.rbB], writes=[rtB])
    sb4 = sbb[:, 0:T, :].rearrange("p t (g i) -> p (t g) i", i=4)
    G4 = T * 4
    P.op("dve", lambda e: e.tensor_tensor(out=t6[:, 0:G4, 0:3], in0=sb4[:, :, 0:3], in1=sb4[:, :, 1:4], op=ALU.add), reads=[rtB], writes=[rtB])
    P.op("dve", lambda e: e.tensor_tensor(out=t6[:, 0:G4, 3:5], in0=sb4[:, :, 0:2], in1=sb4[:, :, 2:4], op=ALU.add), reads=[rtB], writes=[rtB])
    P.op("dve", lambda e: e.tensor_tensor(out=t6[:, 0:G4, 5:6], in0=sb4[:, :, 0:1], in1=sb4[:, :, 3:4], op=ALU.add), reads=[rtB], writes=[rtB])
    P.op("dve", lambda e: e.tensor_reduce(out=gsc[:, 0:G4], in_=t6[:, 0:G4, :], axis=AX.X, op=ALU.max), reads=[rtB], writes=[rtB])
    P.op("dve", lambda e: e.tensor_reduce(out=gmx[:, 0:T], in_=gsc[:, 0:G4].rearrange("p (t g) -> p t g", g=4), axis=AX.X, op=ALU.max),
         reads=[rtB], writes=[rtB])
    P.op("dve", lambda e: e.tensor_tensor(out=pen[:, 0:G4].rearrange("p (t g) -> p t g", g=4), in0=gsc[:, 0:G4].rearrange("p (t g) -> p t g", g=4),
                                          in1=gmx[:, 0:T].unsqueeze(2).to_broadcast([128, T, 4]), op=ALU.is_ge), reads=[rtB], writes=[rtB])
    P.op("dve", lambda e: e.tensor_scalar(out=pen[:, 0:G4], in0=pen[:, 0:G4], scalar1=100.0, scalar2=-100.0, op0=ALU.mult, op1=ALU.add),
         reads=[rtB], writes=[rtB])
    msk4 = msk[:, 0:T, :].rearrange("p t (g i) -> p (t g) i", i=4)
    P.op("dve", lambda e: e.tensor_tensor(out=msk4, in0=sb4, in1=pen[:, 0:G4].unsqueeze(2).to_broadcast([128, G4, 4]), op=ALU.add),
         reads=[rtB], writes=[rtB])
    P.op("dve", lambda e: e.tensor_reduce(out=gmx[:, 0:T], in_=msk[:, 0:T, :], axis=AX.X, op=ALU.max), reads=[rtB], writes=[rtB])
    P.op("dve", lambda e: e.tensor_tensor(out=eq[:, 0:T, :], in0=msk[:, 0:T, :], in1=gmx[:, 0:T].unsqueeze(2).to_broadcast([128, T, NE]), op=ALU.is_ge),
         reads=[rtB], writes=[rtB])
    P.op("dve", lambda e: e.scalar_tensor_tensor(out=eq[:, 0:T, :], in0=eq[:, 0:T, :], scalar=-200.0, in1=msk[:, 0:T, :], op0=ALU.mult, op1=ALU.add),
         reads=[rtB], writes=[rtB])
    P.op("dve", lambda e: e.tensor_reduce(out=m2[:, 0:T], in_=eq[:, 0:T, :], axis=AX.X, op=ALU.max), reads=[rtB], writes=[rtB])
    P.op("dve", lambda e: e.tensor_tensor(out=eq[:, 0:T, :], in0=msk[:, 0:T, :], in1=m2[:, 0:T].unsqueeze(2).to_broadcast([128, T, NE]), op=ALU.is_ge),
         reads=[rtB], writes=[rtB])
    P.op("dve", lambda e: e.tensor_tensor(out=eq[:, 0:T, :], in0=eq[:, 0:T, :], in1=s_all[:, 0:T, :], op=ALU.mult), reads=[rtB], writes=[rtB])
    P.op("dve", lambda e: e.tensor_reduce(out=m2[:, 0:T], in_=eq[:, 0:T, :], axis=AX.X, op=ALU.add), reads=[rtB], writes=[rtB])
    P.op("dve", lambda e: e.reciprocal(out=m2[:, 0:T], in_=m2[:, 0:T]), reads=[rtB], writes=[rtB])
    P.op("dve", lambda e: e.tensor_tensor(out=C.gatesv[:, t0:t0 + T, :], in0=eq[:, 0:T, :], in1=m2[:, 0:T].unsqueeze(2).to_broadcast([128, T, NE]), op=ALU.mult),
         reads=[rtB], writes=[C.gatesB[t] for t in tiles])

    npieces = 0
    if layer == 0:
        P.barrier()
        RO = Region(nc, ov_off, SB_END, "pcov")
        wa2 = [RO.t("wa2_%d" % i, [128, 8, 256], BF16) for i in range(2)]
        adabp = [RO.t("adabp%d" % i, [2, 256], F32) for i in range(2)]
        mp0 = RO.t("mp0", [2, 256], F32)
        mp = [mp0, mp0]
        mpB0 = P.buf("mp")
        wa2B, adabpB, mpB = P.bufs("wa2", 2), P.bufs("adabp", 2), [mpB0, mpB0]
        npieces = 24
        for q in range(2):
            adaln_piece_load(C, q, wa2[q], wa2B[q], adabp[q], adabpB[q])
    blocks = [tiles[i:i + 4] for i in range(0, len(tiles), 4)]
    AB = P.bufs("A", len(blocks))
    fk = 0
    dk = 0

    def gu_block(e, bi):
        nonlocal fk
        s = e % 2
        blk = blocks[bi]
        N = 128 * len(blk)
        tok0 = blk[0] * 128
        hb = [hTB[t] for t in blk]
        for f in range(4):
            q = fk % 2
            fk += 1
            pg, pu = 2 * q, 2 * q + 1
            for k in range(8):
                P.op("pe", lambda en, k=k, f=f, pg=pg: en.matmul(C.ps[pg][:, 0:N], Wg[s][:, k, f * 128:(f + 1) * 128],
                                                                 C.hT[:, k, tok0:tok0 + N], start=(k == 0), stop=(k == 7)),
                     reads=[WgB[s]] + hb, writes=[C.psB[pg]])
            for k in range(8):
                P.op("pe", lambda en, k=k, f=f, pu=pu: en.matmul(C.ps[pu][:, 0:N], Wu[s][:, k, f * 128:(f + 1) * 128],
                                                                 C.hT[:, k, tok0:tok0 + N], start=(k == 0), stop=(k == 7)),
                     reads=[WuB[s]] + hb, writes=[C.psB[pu]])
            P.op("act", lambda en, q=q, pg=pg: en.activation(out=sg[q][:, 0:N], in_=C.ps[pg][:, 0:N], func=AF.Silu),
                 reads=[C.psB[pg]], writes=[sgB[q]])
            P.op("dve", lambda en, f=f, q=q, pu=pu: en.tensor_tensor(out=A[:, f, tok0:tok0 + N], in0=C.ps[pu][:, 0:N], in1=sg[q][:, 0:N], op=ALU.mult),
                 reads=[C.psB[pu], sgB[q]], writes=[AB[bi]])

    def down_block(e, bi):
        nonlocal dk
        s = e % 2
        for t in blocks[bi]:
            who = 1 if t < 2 else 0
            q = dk % 2
            dk += 1
            for half in range(2):
                pb = 4 + 2 * q + half
                for f in range(4):
                    P.op("pe", lambda en, f=f, half=half, pb=pb, t=t: en.matmul(
                        C.ps[pb][:, :], A[:, f, t * 128:(t + 1) * 128], Wd[s][:, f, half * 512:(half + 1) * 512],
                        start=(f == 0), stop=(f == 3)),
                        reads=[AB[bi], WdB[s]], writes=[C.psB[pb]])
                P.op("dve", lambda en, half=half, pb=pb, t=t, who=who, q=q: en.scalar_tensor_tensor(
                    out=tmpf[q][:, half * 512:(half + 1) * 512], in0=C.ps[pb][:, :], scalar=C.gatesv[:, t, e:e + 1],
                    in1=C.gbc[:, who, half * 512:(half + 1) * 512], op0=ALU.mult, op1=ALU.mult),
                    reads=[C.psB[pb], C.gatesB[t], C.gbcB[who]], writes=[tmpfB[q]])
            P.op("pool", lambda en, t=t, q=q: en.tensor_tensor(out=C.x_sb[:, t, :], in0=C.x_sb[:, t, :], in1=tmpf[q][:], op=ALU.add),
                 reads=[tmpfB[q], C.xB[t]], writes=[C.xB[t]])

    nb = len(blocks)
    for e in range(NE):
        for bi in range(nb):
            gu_block(e, bi)
            if bi >= 1:
                down_block(e, bi - 1)
        down_block(e, nb - 1)
        if e + 2 < NE:
            load_expert(e + 2)
        for q in range(e * npieces // NE, (e + 1) * npieces // NE):
            j = q % 2
            adaln_piece(C, q, wa2[j], wa2B[j], adabp[j], adabpB[j], mp[j], mpB[j])
            if q + 2 < npieces:
                adaln_piece_load(C, q + 2, wa2[j], wa2B[j], adabp[j], adabpB[j])


def phase_final(C):
    nc, P, I = C.nc, C.P, C.I
    P.barrier()
    R = Region(nc, TMP_OFF, SB_END, "pf")
    fg = R.t("fg", [128, D], F32)
    fgB = P.buf("fg")
    junk = R.t("junk", [128, D], BF16)
    junkB = P.buf("junk")
    eps = R.t("eps", [128, 1], F32)
    epsB = P.buf("eps")
    ss = [R.t("ss%d" % i, [128, 2], F32) for i in range(2)]
    ssB = P.bufs("ss", 2)
    ob = [R.t("ob%d" % i, [128, D], F32) for i in range(2)]
    obB = P.bufs("ob", 2)
    P.op("dve", lambda e: e.memset(eps[:], EPS), writes=[epsB])
    P.op("sp", lambda e: e.dma_start(out=fg[:], in_=I.fg_bc), writes=[fgB], dma=True)
    for i, t in enumerate(range(2, NT0)):
        q = i % 2
        P.op("dve", lambda e, q=q: e.memset(ss[q][:], 0.0), writes=[ssB[q]])
        P.op("act", lambda e, t=t, q=q: e.activation(out=junk[:], in_=C.x_sb[:, t, :], func=AF.Square, accum_out=ss[q][:, 0:1]),
             reads=[C.xB[t], ssB[q]], writes=[ssB[q], junkB])
        P.op("act", lambda e, q=q: e.activation(out=ss[q][:, 0:1], in_=ss[q][:, 0:1], func=AF.Sqrt, scale=1.0 / D, bias=eps[:, 0:1]),
             reads=[ssB[q], epsB], writes=[ssB[q]])
        P.op("dve", lambda e, q=q: e.reciprocal(out=ss[q][:, 0:1], in_=ss[q][:, 0:1]), reads=[ssB[q]], writes=[ssB[q]])
        P.op("dve", lambda e, t=t, q=q: e.scalar_tensor_tensor(out=ob[q][:], in0=C.x_sb[:, t, :], scalar=ss[q][:, 0:1], in1=fg[:],
                                                              op0=ALU.mult, op1=ALU.mult),
             reads=[C.xB[t], ssB[q], fgB], writes=[obB[q]])
        P.op("sp", lambda e, t=t, q=q: e.dma_start(out=C.out[(t - 2) * 128:(t - 1) * 128, :], in_=ob[q][:]),
             reads=[obB[q]], dma=True)


def phase_attn(C):
    nc, P, I = C.nc, C.P, C.I
    layer = 1
    C.attn_done = True
    P.barrier()
    R = Region(nc, TMP_OFF, SB_END, "pd")
    qT = R.t("qT", [128, 8, NX], BF16)
    qTB = P.bufs("qT", 4)
    kT = R.t("kT", [128, 4, 2304], BF16)
    kTB = P.bufs("kT", 5)
    v_sb = R.t("v", [128, NT0, 4, 65], BF16)
    vB = P.bufs("v", NT0)
    cos_off = R.cur
    cosT = R.t("cosT", [128, NX], F32)
    sinT = R.t("sinT", [128, NX], F32)
    ropeB = P.buf("rope")
    sub_off = R.cur
    wv = R.t("wv", [128, 8, 256], BF16)
    wvB = P.buf("wv")
    sts = []
    for i in range(2):
        st = {}
        st["xn"] = R.t("xn%d" % i, [128, D], BF16)
        st["xnB"] = P.buf("xn%d" % i)
        st["junk"], st["junkB"] = st["xn"], st["xnB"]
        st["ss"] = R.t("ss%d" % i, [128, 2], F32)
        st["ssB"] = P.buf("ss%d" % i)
        if i == 0:
            st["eps"] = R.t("eps", [128, 1], F32)
            st["epsB"] = P.buf("eps")
        else:
            st["eps"], st["epsB"] = sts[0]["eps"], sts[0]["epsB"]
        sts.append(st)
    st = sts[0]
    hTB = P.bufs("h1T", NT0)

    P.op("dve", lambda e: e.memset(st["eps"][:], EPS), writes=[st["epsB"]])
    P.op("pool", lambda e: e.dma_start(out=wv[:], in_=I.wvv.rearrange("(c p) f -> p c f", p=128)),
         writes=[wvB], dma=True)
    P.op("sp", lambda e: e.dma_start(out=cosT[:], in_=I.rope[0]), writes=[ropeB], dma=True)
    P.op("sp", lambda e: e.dma_start(out=sinT[:], in_=I.rope[1]), writes=[ropeB], dma=True)
    adaln_cols(C, 1)
    load_gate_bc(C, layer, 0, 0, 0)
    for t in range(NT0):
        P.op("pool", lambda e, t=t: e.memset(v_sb[:, t, :, :], 1.0), writes=[vB[t]])

    ssall = R.t("ssall", [128, NT0], F32)
    ssBt = P.bufs("ssall", NT0)
    rstd = R.t("rstd", [128, NT0], F32)
    rstdB = P.buf("rstd")
    norm_stats(C, list(range(NT0)), ssall, ssBt, rstd, rstdB, [sts[0]["xn"], sts[1]["xn"]], [sts[0]["xnB"], sts[1]["xnB"]],
               st["eps"], st["epsB"])
    for t in range(NT0):
        who = 1 if t < 2 else 0
        norm_transpose_tile(C, R, C.x_sb[:, t, :], C.xB[t], layer, 0, who, C.hT, hTB[t], t * 128, sts, (6, 7),
                            rstd_ap=rstd[:, t:t + 1], rstdB=rstdB)
    for t in range(NT0):
        pb = 4 + (t % 2)
        for k in range(8):
            P.op("pe", lambda e, k=k, t=t, pb=pb: e.matmul(C.ps[pb][:, 0:256], C.hT[:, k, t * 128:(t + 1) * 128], wv[:, k, :],
                                                           start=(k == 0), stop=(k == 7)),
                 reads=[hTB[t], wvB], writes=[C.psB[pb]])
        P.op("act", lambda e, t=t, pb=pb: e.activation(out=v_sb[:, t, :, 0:64],
                                                       in_=C.ps[pb][:, 0:256].rearrange("p (h d) -> p h d", d=64), func=AF.Copy),
             reads=[C.psB[pb]], writes=[vB[t]])

    import os
    astop = int(os.environ.get("ATTN_STOP", "9"))
    if astop <= 1:
        return
    P.barrier()
    R2d = Region(nc, sub_off, SB_END, "pd2")
    wq = [R2d.t("wq%d" % i, [128, 8, 128], BF16) for i in range(3)]
    wqB = P.bufs("wq", 3)
    perm = R2d.t("perm", [128, 128], BF16)
    permB = P.buf("perm")
    t1 = R2d.t("t1", [128, 512], F32)
    t2 = R2d.t("t2", [128, 512], F32)
    raw = [R2d.t("raw%d" % i, [128, 512], BF16) for i in range(2)]
    t1B, t2B = P.buf("t1"), P.buf("t2")
    rawB = P.bufs("raw", 2)
    P.op("pool", lambda e: e.dma_start(out=perm[:], in_=I.perm), writes=[permB], dma=True)

    uk = 0
    rk = 0
    for ch in range(12):
        s = ch % 3
        P.op("pool", lambda e, ch=ch, s=s: e.dma_start(out=wq[s][:], in_=I.wqk[ch].rearrange("(c p) f -> p c f", p=128)),
             writes=[wqB[s]], dma=True)
        hk = ch - 8
        if ch >= 8:
            pb = rk % 3
            rk += 1
            for k in range(8):
                P.op("pe", lambda e, k=k, s=s, pb=pb: e.matmul(C.ps[pb][:, 0:256], wq[s][:, k, :], C.hT[:, k, 0:256],
                                                             start=(k == 0), stop=(k == 7)),
                     reads=[wqB[s], hTB[0], hTB[1]], writes=[C.psB[pb]])
            P.op("act", lambda e, hk=hk, pb=pb: e.activation(out=kT[:, hk, 0:256], in_=C.ps[pb][:, 0:256], func=AF.Copy),
                 reads=[C.psB[pb]], writes=[kTB[0]])
        for tg in range(4):
            pb = rk % 3
            rk += 1
            c0 = 256 + tg * 512
            hb = [hTB[2 + tg * 4 + j] for j in range(4)]
            for k in range(8):
                P.op("pe", lambda e, k=k, s=s, pb=pb, c0=c0: e.matmul(C.ps[pb][:, :], wq[s][:, k, :], C.hT[:, k, c0:c0 + 512],
                                                                    start=(k == 0), stop=(k == 7)),
                     reads=[wqB[s]] + hb, writes=[C.psB[pb]])
            q = uk % 2
            uk += 1
            if ch < 8:
                dst, dB = qT[:, ch, tg * 512:(tg + 1) * 512], qTB[tg]
            else:
                dst, dB = kT[:, ch - 8, c0:c0 + 512], kTB[1 + tg]
            if os.environ.get("ATTN_NOROPE"):
                P.op("act", lambda e, dst=dst, pb=pb: e.activation(out=dst, in_=C.ps[pb][:, :], func=AF.Copy),
                     reads=[C.psB[pb]], writes=[dB])
                continue
            P.op("act", lambda e, q=q, pb=pb: e.activation(out=raw[q][:], in_=C.ps[pb][:, :], func=AF.Copy),
                 reads=[C.psB[pb]], writes=[rawB[q]])
            pb2 = 3 + q
            P.op("pe", lambda e, q=q, pb2=pb2: e.matmul(C.ps[pb2][:, :], perm[:], raw[q][:], start=True, stop=True),
                 reads=[rawB[q], permB], writes=[C.psB[pb2]])
            P.op("dve", lambda e, pb=pb, tg=tg: e.tensor_tensor(out=t1[:], in0=C.ps[pb][:, :], in1=cosT[:, tg * 512:(tg + 1) * 512], op=ALU.mult),
                 reads=[C.psB[pb], ropeB], writes=[t1B])
            P.op("dve", lambda e, pb2=pb2, tg=tg: e.tensor_tensor(out=t2[:], in0=C.ps[pb2][:, :], in1=sinT[:, tg * 512:(tg + 1) * 512], op=ALU.mult),
                 reads=[C.psB[pb2], ropeB], writes=[t2B])
            if ch < 8:
                dst, dB = qT[:, ch, tg * 512:(tg + 1) * 512], qTB[tg]
            else:
                dst, dB = kT[:, ch - 8, c0:c0 + 512], kTB[1 + tg]
            P.op("dve", lambda e, dst=dst: e.tensor_tensor(out=dst, in0=t1[:], in1=t2[:], op=ALU.add),
                 reads=[t1B, t2B], writes=[dB])

    if astop <= 2:
        return
    P.barrier()
    R2 = Region(nc, HT_OFF, GBC_OFF, "pd3")
    w_o = R2.t("wo", [128, 8, D], BF16)
    woB = P.buf("wo")
    PT = [R2.t("PT%d" % i, [128, 5, 512], BF16) for i in range(2)]
    PTB = [P.bufs("PT%d_" % i, 5) for i in range(2)]
    o_tok0 = R2.t("otok", [128, D], BF16)
    oT0 = R2.t("oT", [128, 8, 128], BF16)
    tmpf = R2.t("tmpf", [128, D], F32)
    tmpfB = P.buf("tmpf")
    maskT = R2.t("maskT", [128, 2, 128], F32)
    maskB = P.buf("maskT")
    esink = R2.t("esink", [128, NE], F32)
    esB = P.buf("esink")
    rsum = R2.t("rsum", [128, 4], F32)
    rsB = P.buf("rsum")
    for h in range(2):
        P.op("pool", lambda e, h=h: e.dma_start(out=w_o[:, h * 4:(h + 1) * 4, :],
                                               in_=I.b_w_o[h * 512:(h + 1) * 512, :].rearrange("(c p) f -> p c f", p=128)),
             writes=[woB], dma=True)
    P.op("sp", lambda e: e.dma_start(out=maskT[:], in_=I.mask.rearrange("p (a b) -> p a b", a=3)[:, 0:2, :]), writes=[maskB], dma=True)
    P.op("sp", lambda e: e.dma_start(out=esink[:], in_=I.sink_bc), writes=[esB], dma=True)
    P.op("act", lambda e: e.activation(out=esink[:], in_=esink[:], func=AF.Exp), reads=[esB], writes=[esB])
    R3 = Region(nc, cos_off, cos_off + 16384, "pd3b")
    qz = [R3.t("qz%d" % i, [128, 4, 128], BF16) for i in range(2)]
    qzB = P.bufs("qz", 2)
    mask4 = R3.t("mask4", [128, 2, 4, 128], BF16)
    mask4B = P.buf("mask4")
    o_toks = [o_tok0, R3.t("otok1", [128, D], BF16)]
    otBs = P.bufs("otok", 2)
    oTs = [oT0, R3.t("oT1", [128, 8, 128], BF16)]
    oTBs = P.bufs("oT", 2)
    for j in range(2):
        P.op("pool", lambda e, j=j: e.memset(qz[j][:], 0.0), writes=[qzB[j]])
    P.op("dve", lambda e: e.tensor_copy(out=mask4[:], in_=maskT[:].unsqueeze(2).to_broadcast([128, 2, 4, 128])),
         reads=[maskB], writes=[mask4B])
    gk = 0

    def attn_tail(i):
        t = 2 + i
        o_tok, otB, oT, oTB = o_toks[i % 2], otBs[i % 2], oTs[i % 2], oTBs[i % 2]
        pt = ps_bf16(C, 6)
        for k in range(8):
            P.op("pe", lambda e, k=k: e.transpose(out=pt[:, k * 128:(k + 1) * 128], in_=o_tok[:, k * 128:(k + 1) * 128], identity=C.ident_b[:]),
                 reads=[otB, C.identB], writes=[C.psB[6]])
        P.op("act", lambda e: e.activation(out=oT[:].rearrange("p k t -> p (k t)"), in_=pt[:, :], func=AF.Copy),
             reads=[C.psB[6]], writes=[oTB])
        for half in range(2):
            pb = 6 + half
            for k in range(8):
                P.op("pe", lambda e, k=k, half=half, pb=pb: e.matmul(C.ps[pb][:, :], oT[:, k, :], w_o[:, k, half * 512:(half + 1) * 512],
                                                                   start=(k == 0), stop=(k == 7)),
                     reads=[oTB, woB], writes=[C.psB[pb]])
            P.op("dve", lambda e, half=half, pb=pb: e.tensor_tensor(out=tmpf[:, half * 512:(half + 1) * 512], in0=C.ps[pb][:, :],
                                                                    in1=C.gbc[:, 0, half * 512:(half + 1) * 512], op=ALU.mult),
                 reads=[C.psB[pb], C.gbcB[0]], writes=[tmpfB])
        P.op("pool", lambda e, t=t: e.tensor_tensor(out=C.x_sb[:, t, :], in0=C.x_sb[:, t, :], in1=tmpf[:], op=ALU.add),
             reads=[tmpfB, C.xB[t]], writes=[C.xB[t]])

    for i in range(16):
        t = 2 + i
        o_tok, otB = o_toks[i % 2], otBs[i % 2]
        blocks = [(0, 0, None), (128, 1, None)]
        for d in (-1, 0, 1):
            j = i + d
            if j < 0 or j > 15:
                continue
            blocks.append((256 + j * 128, 2 + j, {-1: 0, 0: None, 1: 1}[d]))
        tgq = i // 4
        for hk in range(4):
            pq = gk % 2
            gk += 1
            P.op("pool", lambda e, pq=pq, hk=hk, i=i: e.tensor_copy(out=qz[pq][0:64, 0:2, :], in_=qT[0:64, 2 * hk:2 * hk + 2, i * 128:(i + 1) * 128]),
                 reads=[qTB[tgq]], writes=[qzB[pq]])
            P.op("pool", lambda e, pq=pq, hk=hk, i=i: e.tensor_copy(out=qz[pq][64:128, 2:4, :], in_=qT[64:128, 2 * hk:2 * hk + 2, i * 128:(i + 1) * 128]),
                 reads=[qTB[tgq]], writes=[qzB[pq]])
            for bi, (kc, vt, mi) in enumerate(blocks):
                kb = kTB[0] if bi < 2 else kTB[1 + (vt - 2) // 4]
                P.op("pe", lambda e, bi=bi, kc=kc, hk=hk, pq=pq, mi=mi: e.matmul(
                    C.ps[bi][:, :], kT[:, hk, kc:kc + 128], qz[pq][:].rearrange("p a b -> p (a b)"),
                    start=True, stop=(mi is None)),
                    reads=[kb, qzB[pq]], writes=[C.psB[bi]])
                if mi is not None:
                    P.op("pe", lambda e, bi=bi, mi=mi: e.matmul(
                        C.ps[bi][:, :], C.ident_b[:], mask4[:, mi, :, :].rearrange("p a b -> p (a b)"), start=False, stop=True),
                        reads=[C.identB, mask4B], writes=[C.psB[bi]])
                P.op("act", lambda e, bi=bi, pq=pq: e.activation(out=PT[pq][:, bi, :], in_=C.ps[bi][:, :], func=AF.Exp, scale=0.125),
                     reads=[C.psB[bi]], writes=[PTB[pq][bi]])
            nb = len(blocks)
            for slot in range(4):
                for bi, (kc, vt, mi) in enumerate(blocks):
                    P.op("pe", lambda e, slot=slot, bi=bi, vt=vt, pq=pq, hk=hk, nb=nb: e.matmul(
                        C.ps[5][:, slot * 65:(slot + 1) * 65], PT[pq][:, bi, slot * 128:(slot + 1) * 128], v_sb[:, vt, hk, :],
                        start=(bi == 0), stop=(bi == nb - 1)),
                        reads=[PTB[pq][bi], vB[vt]], writes=[C.psB[5]])
            O = C.ps[5][:, 0:260].rearrange("p (b c d) -> p b c d", b=2, c=2)
            es = esink[:, hk * 4:(hk + 1) * 4].rearrange("p (c b) -> p b c", b=2)
            P.op("dve", lambda e, O=O, es=es: e.tensor_tensor(out=rsum[:].rearrange("p (b c) -> p b c", b=2), in0=O[:, :, :, 64], in1=es, op=ALU.add),
                 reads=[C.psB[5], esB], writes=[rsB])
            P.op("dve", lambda e: e.reciprocal(out=rsum[:], in_=rsum[:]), reads=[rsB], writes=[rsB])
            od = o_tok[:, hk * 256:(hk + 1) * 256].rearrange("p (c b d) -> p b c d", c=2, b=2)
            P.op("dve", lambda e, O=O, od=od: e.tensor_tensor(
                out=od, in0=O[:, :, :, 0:64], in1=rsum[:].rearrange("p (b c) -> p b c", b=2).unsqueeze(3).to_broadcast([128, 2, 2, 64]),
                op=ALU.mult),
                reads=[C.psB[5], rsB], writes=[otB])
            if hk == 0 and i >= 1:
                attn_tail(i - 1)
    attn_tail(15)
```
